# Optimizing a Trainium2 kernel written in Bass

```python
import jax, jax.numpy as jnp
from jax import lax
import numpy as np

D_MODEL = 1024
BATCH = 1
SEQ = 16384
DEPTH = 2

D_MIX = 2 * D_MODEL
D_MLSTM = D_MIX // 2
NH_MLSTM = 8
DH_MLSTM = D_MLSTM // NH_MLSTM
MLSTM_CHUNK = 128
D_LRU = D_MIX - D_MLSTM
NB_LRU = 8
BS_LRU = D_LRU // NB_LRU
LRU_C = 8.0
CONV_W = 4
N_EXPERTS = 32
N_GROUPS = 8
EXPERTS_PER_GROUP = N_EXPERTS // N_GROUPS
TOP_K = 2
D_FF = D_MODEL // 2
MOE_BLOCK = 128
DEEPNORM_ALPHA = (2 * DEPTH) ** 0.25
DEEPNORM_BETA = (8 * DEPTH) ** -0.25
LN_EPS = 1e-5
N_IN = 4 * D_MLSTM + 2 * NH_MLSTM + 2 * D_LRU
IN_SPLITS = (2 * D_MLSTM, 3 * D_MLSTM, 4 * D_MLSTM, 4 * D_MLSTM + NH_MLSTM,
             4 * D_MLSTM + 2 * NH_MLSTM, 4 * D_MLSTM + 2 * NH_MLSTM + D_LRU)

kernel_name = "hymba_mlstm_rglru_deepnorm_grouped_moe"


def layer_norm(x):
    xf = x.astype(jnp.float32)
    mu = jnp.mean(xf, axis=-1, keepdims=True)
    var = jnp.mean(jnp.square(xf - mu), axis=-1, keepdims=True)
    return ((xf - mu) * lax.rsqrt(var + LN_EPS)).astype(x.dtype)


def rms_norm(x):
    xf = x.astype(jnp.float32)
    return (xf * lax.rsqrt(jnp.mean(jnp.square(xf), axis=-1, keepdims=True) + LN_EPS)).astype(x.dtype)


def causal_depthwise_conv(x, w, b):
    s = x.shape[1]
    xp = jnp.pad(x, ((0, 0), (CONV_W - 1, 0), (0, 0)))
    y = b
    for k in range(CONV_W):
        y = y + xp[:, k:k + s] * w[k]
    return y


def mlstm_chunkwise(q, k, v, i_pre, log_f):
    bsz, nh, s, dh = q.shape
    nc = s // MLSTM_CHUNK

    def chunks(a):
        return jnp.moveaxis(a.reshape(bsz, nh, nc, MLSTM_CHUNK, *a.shape[3:]), 2, 0)

    causal = jnp.tril(jnp.ones((MLSTM_CHUNK, MLSTM_CHUNK), dtype=bool))

    def step(carry, xs):
        c_prev, n_prev, m_prev = carry
        qc, kc, vc, ic, lfc = xs
        b = jnp.cumsum(lfc, axis=-1)
        d_log = jnp.where(causal, b[..., :, None] - b[..., None, :] + ic[..., None, :], -jnp.inf)
        inter_log = b + m_prev[..., None]
        m_t = jnp.maximum(jnp.max(d_log, axis=-1), inter_log)
        scores = jnp.einsum('bhtd,bhsd->bhts', qc, kc) * jnp.exp(d_log - m_t[..., None])
        w_inter = jnp.exp(inter_log - m_t)
        num = (jnp.einsum('bhts,bhse->bhte', scores, vc)
               + w_inter[..., None] * jnp.einsum('bhtd,bhde->bhte', qc, c_prev))
        den = jnp.sum(scores, axis=-1) + w_inter * jnp.einsum('bhtd,bhd->bht', qc, n_prev)
        h = num / jnp.maximum(jnp.abs(den), jnp.exp(-m_t))[..., None]
        b_last = b[..., -1]
        g_log = b_last[..., None] - b + ic
        m_new = jnp.maximum(b_last + m_prev, jnp.max(g_log, axis=-1))
        decay = jnp.exp(b_last + m_prev - m_new)
        wk = jnp.exp(g_log - m_new[..., None])[..., None] * kc
        c_new = decay[..., None, None] * c_prev + jnp.einsum('bhsd,bhse->bhde', wk, vc)
        n_new = decay[..., None] * n_prev + jnp.sum(wk, axis=2)
        return (c_new, n_new, m_new), h

    init = (jnp.zeros((bsz, nh, dh, dh), jnp.float32),
            jnp.zeros((bsz, nh, dh), jnp.float32),
            jnp.zeros((bsz, nh), jnp.float32))
    _, hs = lax.scan(step, init, (chunks(q), chunks(k), chunks(v), chunks(i_pre), chunks(log_f)))
    return jnp.moveaxis(hs, 0, 2).reshape(bsz, nh, s, dh)


def linear_recurrence(a, x):
    def combine(left, right):
        a_l, x_l = left
        a_r, x_r = right
        return a_l * a_r, a_r * x_l + x_r
    _, h = lax.associative_scan(combine, (a, x), axis=1)
    return h


def hybrid_mixer(u, w_in, b_in, w_conv_m, b_conv_m, mh_norm_g, w_conv_r, b_conv_r,
                 w_a, b_a, w_x, b_x, lru_lambda, lru_norm_g, w_out):
    bsz, s, _ = u.shape
    f32 = jnp.float32
    proj = jnp.einsum('bsd,dn->bsn', u, w_in) + b_in
    qk, v, o_pre, i_pre, f_pre, x_r, g_r = jnp.split(proj, IN_SPLITS, axis=-1)

    qk = jax.nn.silu(causal_depthwise_conv(qk, w_conv_m, b_conv_m))
    q, k = jnp.split(qk, 2, axis=-1)

    def to_heads(a):
        return a.reshape(bsz, s, NH_MLSTM, DH_MLSTM).transpose(0, 2, 1, 3).astype(f32)

    h_m = mlstm_chunkwise(to_heads(q), to_heads(k) * DH_MLSTM ** -0.5, to_heads(v),
                          i_pre.astype(f32).transpose(0, 2, 1),
                          jax.nn.log_sigmoid(f_pre.astype(f32)).transpose(0, 2, 1))
    h_m = layer_norm(h_m.transpose(0, 2, 1, 3)).reshape(bsz, s, D_MLSTM)
    y_m = (jax.nn.sigmoid(o_pre.astype(f32)) * h_m).astype(u.dtype) * mh_norm_g

    xc = causal_depthwise_conv(x_r, w_conv_r, b_conv_r)
    xb = xc.reshape(bsz, s, NB_LRU, BS_LRU)
    r = jax.nn.sigmoid((jnp.einsum('bsnc,ncd->bsnd', xb, w_a).reshape(bsz, s, D_LRU) + b_a).astype(f32))
    ig = jax.nn.sigmoid((jnp.einsum('bsnc,ncd->bsnd', xb, w_x).reshape(bsz, s, D_LRU) + b_x).astype(f32))
    log_a = -LRU_C * r * jax.nn.softplus(-lru_lambda.astype(f32))
    x_in = jnp.sqrt(-jnp.expm1(2.0 * log_a)) * (ig * xc.astype(f32))
    h_r = linear_recurrence(jnp.exp(log_a), x_in)
    y_r = h_r.astype(u.dtype) * jax.nn.gelu(g_r)
    y_r = rms_norm(y_r.reshape(bsz, s, NB_LRU, BS_LRU)).reshape(bsz, s, D_LRU) * lru_norm_g

    y = jnp.concatenate([y_m, y_r], axis=-1)
    return jnp.einsum('bsm,md->bsd', y, w_out)


def grouped_moe(h, w_router, b_router, w_gate, w_up, w_down):
    t, d = h.shape
    f32 = jnp.float32
    affinity = jax.nn.sigmoid(h.astype(f32) @ w_router.astype(f32))
    sel = (affinity + b_router.astype(f32)).reshape(t, N_GROUPS, EXPERTS_PER_GROUP)
    group_score = jnp.sum(lax.top_k(sel, TOP_K)[0], axis=-1)
    grp = jnp.argmax(group_score, axis=-1).astype(jnp.int32)
    sel_in_grp = jnp.take_along_axis(sel, grp[:, None, None], axis=1)[:, 0]
    _, local = lax.top_k(sel_in_grp, TOP_K)
    experts = grp[:, None] * EXPERTS_PER_GROUP + local.astype(jnp.int32)
    gates = jnp.take_along_axis(affinity, experts, axis=1)
    gates = gates / jnp.sum(gates, axis=-1, keepdims=True)

    n_assign = t * TOP_K
    e_flat = experts.reshape(n_assign)
    tok_flat = jnp.repeat(jnp.arange(t, dtype=jnp.int32), TOP_K)
    order = jnp.argsort(e_flat)
    e_s, tok_s, g_s = e_flat[order], tok_flat[order], gates.reshape(n_assign)[order]
    counts = jnp.zeros((N_EXPERTS,), jnp.int32).at[e_flat].add(1)
    padded = (counts + MOE_BLOCK - 1) // MOE_BLOCK * MOE_BLOCK
    pend = jnp.cumsum(padded)
    pstart = pend - padded
    start = jnp.cumsum(counts) - counts
    dest = pstart[e_s] + jnp.arange(n_assign, dtype=jnp.int32) - start[e_s]
    n_blk = -(-n_assign // MOE_BLOCK) + N_EXPERTS
    rows = n_blk * MOE_BLOCK
    x_buf = jnp.zeros((rows, d), h.dtype).at[dest].set(h[tok_s])
    blk_start = jnp.arange(n_blk, dtype=jnp.int32) * MOE_BLOCK
    blk_e = jnp.minimum(jnp.searchsorted(pend, blk_start, side='right'), N_EXPERTS - 1)

    def expert_block(args):
        xb, e = args
        hid = jax.nn.silu(xb @ w_gate[e]) * (xb @ w_up[e])
        return hid @ w_down[e]

    y_buf = lax.map(expert_block, (x_buf.reshape(n_blk, MOE_BLOCK, d), blk_e))
    y = y_buf.reshape(rows, d)[dest] * g_s[:, None].astype(h.dtype)
    return jax.ops.segment_sum(y, tok_s, num_segments=t)


def setup_inputs(seed: int = 0) -> dict:
    key = jax.random.key(seed)
    ks = jax.random.split(key, 26)
    f32 = jnp.float32

    def nrm(k, shape, scale):
        return jax.random.normal(k, shape, f32) * scale

    x = nrm(ks[0], (BATCH, SEQ, D_MODEL), 1.0)
    c = nrm(ks[1], (BATCH, D_MODEL), 1.0)
    w_ada = nrm(ks[2], (DEPTH, D_MODEL, 6 * D_MODEL), 0.5 * D_MODEL ** -0.5)
    b_ada = nrm(ks[3], (DEPTH, 6 * D_MODEL), 0.02)
    w_in = nrm(ks[4], (DEPTH, D_MODEL, N_IN), D_MODEL ** -0.5)
    f_off = 4 * D_MLSTM + NH_MLSTM
    b_in = nrm(ks[5], (DEPTH, N_IN), 0.02).at[:, f_off:f_off + NH_MLSTM].add(
        jnp.linspace(3.0, 6.0, NH_MLSTM, dtype=f32))
    w_conv_m = nrm(ks[6], (DEPTH, CONV_W, 2 * D_MLSTM), CONV_W ** -0.5)
    b_conv_m = nrm(ks[7], (DEPTH, 2 * D_MLSTM), 0.02)
    mh_norm_g = 1.0 + nrm(ks[8], (DEPTH, D_MLSTM), 0.02)
    w_conv_r = nrm(ks[9], (DEPTH, CONV_W, D_LRU), CONV_W ** -0.5)
    b_conv_r = nrm(ks[10], (DEPTH, D_LRU), 0.02)
    w_a = nrm(ks[11], (DEPTH, NB_LRU, BS_LRU, BS_LRU), BS_LRU ** -0.5)
    b_a = nrm(ks[12], (DEPTH, D_LRU), 0.02)
    w_x = nrm(ks[13], (DEPTH, NB_LRU, BS_LRU, BS_LRU), BS_LRU ** -0.5)
    b_x = nrm(ks[14], (DEPTH, D_LRU), 0.02)
    a0 = jax.random.uniform(ks[15], (DEPTH, D_LRU), f32, 0.9, 0.999)
    lru_lambda = jnp.log(a0) - jnp.log1p(-a0)
    lru_norm_g = 1.0 + nrm(ks[16], (DEPTH, D_LRU), 0.02)
    w_out = nrm(ks[17], (DEPTH, D_MIX, D_MODEL), DEEPNORM_BETA * D_MIX ** -0.5)
    w_router = nrm(ks[18], (D_MODEL, N_EXPERTS), D_MODEL ** -0.5)
    b_router = nrm(ks[19], (N_EXPERTS,), 0.01)
    w_gate = nrm(ks[20], (DEPTH, N_EXPERTS, D_MODEL, D_FF), D_MODEL ** -0.5)
    w_up = nrm(ks[21], (DEPTH, N_EXPERTS, D_MODEL, D_FF), D_MODEL ** -0.5)
    w_down = nrm(ks[22], (DEPTH, N_EXPERTS, D_FF, D_MODEL), DEEPNORM_BETA * D_FF ** -0.5)
    ln_g = 1.0 + nrm(ks[23], (DEPTH, 2, D_MODEL), 0.02)
    ln_b = nrm(ks[24], (DEPTH, 2, D_MODEL), 0.02)
    return {"x": x, "c": c, "w_ada": w_ada, "b_ada": b_ada, "w_in": w_in, "b_in": b_in,
            "w_conv_m": w_conv_m, "b_conv_m": b_conv_m, "mh_norm_g": mh_norm_g,
            "w_conv_r": w_conv_r, "b_conv_r": b_conv_r, "w_a": w_a, "b_a": b_a, "w_x": w_x, "b_x": b_x,
            "lru_lambda": lru_lambda, "lru_norm_g": lru_norm_g, "w_out": w_out,
            "w_router": w_router, "b_router": b_router, "w_gate": w_gate, "w_up": w_up, "w_down": w_down,
            "ln_g": ln_g, "ln_b": ln_b}


def reference(x, c, w_ada, b_ada, w_in, b_in, w_conv_m, b_conv_m, mh_norm_g, w_conv_r, b_conv_r,
              w_a, b_a, w_x, b_x, lru_lambda, lru_norm_g, w_out, w_router, b_router,
              w_gate, w_up, w_down, ln_g, ln_b):
    bsz, s, d = x.shape
    cond = jax.nn.silu(c)
    for l in range(DEPTH):
        ada = cond @ w_ada[l] + b_ada[l]
        sh1, sc1, g1, sh2, sc2, g2 = [a[:, None, :] for a in jnp.split(ada, 6, axis=-1)]
        u = layer_norm(x) * (1.0 + sc1) + sh1
        y = hybrid_mixer(u, w_in[l], b_in[l], w_conv_m[l], b_conv_m[l], mh_norm_g[l],
                         w_conv_r[l], b_conv_r[l], w_a[l], b_a[l], w_x[l], b_x[l],
                         lru_lambda[l], lru_norm_g[l], w_out[l])
        x = layer_norm(DEEPNORM_ALPHA * x + g1 * y) * ln_g[l, 0] + ln_b[l, 0]
        u = layer_norm(x) * (1.0 + sc2) + sh2
        y = grouped_moe(u.reshape(bsz * s, d), w_router, b_router,
                        w_gate[l], w_up[l], w_down[l]).reshape(bsz, s, d)
        x = layer_norm(DEEPNORM_ALPHA * x + g2 * y) * ln_g[l, 1] + ln_b[l, 1]
    return x
```

```python
import numpy as np
import ml_dtypes
from contextlib import ExitStack
import concourse.bass as bass
import concourse.mybir as mybir
from concourse.bass_utils import run_bass_kernel_spmd

F32 = mybir.dt.float32
BF16 = mybir.dt.bfloat16
I32 = mybir.dt.int32
AF = mybir.ActivationFunctionType
ALU = mybir.AluOpType
AX = mybir.AxisListType

NCORES = 8
D = 1024
SEQ = 16384
TPC = SEQ // NCORES
NT = TPC // 128
DEPTH = 2
DH = 128
NE = 32
DFF = 512
CAP = 256
ALPHA = (2 * DEPTH) ** 0.25
EPS = 1e-5
BLK = 512
NBLK = SEQ // BLK


class Buf:
    __slots__ = ("w", "r")

    def __init__(self):
        self.w = None
        self.r = {}


class Sched:
    ND = 8

    def __init__(self, nc, es):
        self.nc = nc
        self.engs = {"pe": nc.tensor, "dve": nc.vector, "act": nc.scalar, "pool": nc.gpsimd, "sp": nc.sync}
        self.semh = {}
        for e in self.engs:
            self.semh[e] = es.enter_context(nc.semaphore(f"s_{e}"))
        self.cnt = {e: 0 for e in self.engs}
        self.seen = {e: {} for e in self.engs}
        self.semh["cc"] = es.enter_context(nc.semaphore("s_cc"))
        self.ccn = 0
        self.dqn = {}
        for q in ("sp", "act", "pool"):
            self.dqn[q] = 0
            for i in range(self.ND):
                self.semh[(q, i)] = es.enter_context(nc.semaphore(f"d_{q}{i}"))

    def _wait(self, e, key, val):
        if self.seen[e].get(key, 0) >= val:
            return
        self.engs[e].wait_ge(self.semh[key], val)
        self.seen[e][key] = val

    def _deps(self, e, reads, writes):
        deps = {}
        for b in reads:
            if b.w is not None:
                k, v = b.w
                if deps.get(k, 0) < v:
                    deps[k] = v
        for b in writes:
            if b.w is not None:
                k, v = b.w
                if deps.get(k, 0) < v:
                    deps[k] = v
            for k, v in b.r.items():
                if deps.get(k, 0) < v:
                    deps[k] = v
        for k, v in deps.items():
            if e == "pe" and k == "pe":
                continue
            self._wait(e, k, v)

    def _commit(self, tok, reads, writes):
        k, v = tok
        for b in reads:
            if b.r.get(k, 0) < v:
                b.r[k] = v
        for b in writes:
            b.w = tok
            b.r = {}

    def op(self, e, fn, reads=(), writes=()):
        self._deps(e, reads, writes)
        inst = fn(self.engs[e])
        self.cnt[e] += 1
        inst.then_inc(self.semh[e], 1)
        self._commit((e, self.cnt[e]), reads, writes)
        return inst

    def dma(self, q, out, in_, reads=(), writes=(), **kw):
        return self.dma_fn(q, lambda e: e.dma_start(out=out, in_=in_, **kw), reads, writes)

    def dma_fn(self, q, fn, reads=(), writes=()):
        n = self.dqn[q]
        i = n % self.ND
        rnd = n // self.ND
        self.dqn[q] = n + 1
        key = (q, i)
        if rnd > 0:
            self._wait(q, key, 16 * rnd)
        self._deps(q, reads, writes)
        inst = fn(self.engs[q])
        inst.then_inc(self.semh[key], 16)
        self._commit((key, 16 * (rnd + 1)), reads, writes)
        return inst

    def cc(self, fn, reads=(), writes=()):
        self._deps("pool", reads, writes)
        inst = fn(self.engs["pool"])
        self.ccn += 1
        inst.then_inc(self.semh["cc"], 1)
        self._commit(("cc", self.ccn), reads, writes)
        return inst

    def finish(self, bufs, e="sp"):
        self._deps(e, bufs, bufs)

    def barrier(self):
        for e in self.engs:
            for f in self.engs:
                if f != e and self.cnt[f] > 0:
                    self._wait(e, f, self.cnt[f])
            if self.ccn > 0:
                self._wait(e, "cc", self.ccn)
            for q in ("sp", "act", "pool"):
                n = self.dqn[q]
                for i in range(self.ND):
                    c = (n - i + self.ND - 1) // self.ND if n > i else 0
                    if c > 0:
                        self._wait(e, (q, i), 16 * c)


class Ctx:
    def __init__(self, nc, es):
        self.nc = nc
        self.es = es
        self.S = Sched(nc, es)
        self._n = 0

    def sb(self, shape, dt, es=None, name=None):
        self._n += 1
        return (es or self.es).enter_context(self.nc.sbuf_tensor(name or f"sb{self._n}", list(shape), dt))

    def ps(self, shape, dt, es=None, name=None):
        self._n += 1
        return (es or self.es).enter_context(self.nc.psum_tensor(name or f"ps{self._n}", list(shape), dt))

    def dram_in(self, name, shape, dt):
        return self.nc.dram_tensor(name, list(shape), dt, kind="ExternalInput").ap()

    def dram_out(self, name, shape, dt):
        return self.nc.dram_tensor(name, list(shape), dt, kind="ExternalOutput").ap()


def host_consts():
    c = {}
    c["ident_bf"] = np.eye(128, dtype=np.float32).astype(ml_dtypes.bfloat16)
    c["ident_f"] = np.eye(128, dtype=np.float32)
    r = np.arange(128)
    c["tri_f"] = (r[:, None] <= r[None, :]).astype(np.float32)
    c["ones_f"] = np.ones((128, 128), np.float32)
    c["ones_bf"] = np.ones((128, 128), np.float32).astype(ml_dtypes.bfloat16)
    c["tris_bf"] = (r[:, None] < r[None, :]).astype(np.float32).astype(ml_dtypes.bfloat16)
    c["eoff"] = (np.arange(NE, dtype=np.float32) * CAP)[None, :]
    return c


def emit_ada(cx, cT_d, w_ada_d, b_ada_d, ada_bc, b_ada_bc):
    S = cx.S
    with ExitStack() as es:
        cT = cx.sb([128, 8], F32, es)
        b_cT = Buf()
        cond = cx.sb([128, 8], F32, es)
        b_cond = Buf()
        condB = cx.sb([128, 8, 128], F32, es)
        b_condB = Buf()
        bias = cx.sb([128, 6 * D], F32, es)
        b_bias = Buf()
        wbuf = [cx.sb([128, 8, 512], F32, es) for _ in range(2)]
        b_w = [Buf(), Buf()]
        pp = [cx.ps([128, 512], F32, es) for _ in range(2)]
        b_pp = [Buf(), Buf()]
        S.dma("sp", cT[:], cT_d, writes=[b_cT])
        S.dma("sp", bias[:], b_ada_d.partition_broadcast(128), writes=[b_bias])
        S.op("act", lambda e: e.activation(out=cond[:], in_=cT[:], func=AF.Silu), [b_cT], [b_cond])
        for k in range(8):
            S.op("dve", lambda e: e.tensor_copy(out=condB[:, k, :], in_=cond[:, k:k + 1].to_broadcast([128, 128])),
                 [b_cond], [b_condB])
        wv = w_ada_d.rearrange("(k p) n -> p k n", p=128)
        for j in range(12):
            w = wbuf[j % 2]
            S.dma("sp" if j % 2 == 0 else "act", w[:], wv[:, :, j * 512:(j + 1) * 512], writes=[b_w[j % 2]])
            p = pp[j % 2]
            for k in range(8):
                S.op("pe", lambda e: e.matmul(p[:], lhsT=condB[:, k, :], rhs=w[:, k, :], start=(k == 0), stop=(k == 7)),
                     [b_condB, b_w[j % 2]], [b_pp[j % 2]])
            S.op("dve", lambda e: e.tensor_tensor(out=ada_bc[:, j * 512:(j + 1) * 512], in0=p[:],
                                                  in1=bias[:, j * 512:(j + 1) * 512], op=ALU.add),
                 [b_pp[j % 2], b_bias], [b_ada_bc])
        S.barrier()


def emit_ln_stats(cx, x_ap, b_x, st, mv, rs, nb, b_st):
    S = cx.S
    S.op("dve", lambda e: e.bn_stats(out=st[:, 0:6], in_=x_ap[:, 0:512]), [b_x], [b_st])
    S.op("dve", lambda e: e.bn_stats(out=st[:, 6:12], in_=x_ap[:, 512:1024]), [b_x], [b_st])
    S.op("dve", lambda e: e.bn_aggr(out=mv[:], in_=st[:]), [b_st], [b_st])
    S.op("act", lambda e: e.activation(out=rs[:], in_=mv[:, 1:2], func=AF.Sqrt, bias=EPS, scale=1.0), [b_st], [b_st])
    S.op("dve", lambda e: e.reciprocal(out=rs[:], in_=rs[:]), [b_st], [b_st])
    S.op("dve", lambda e: e.scalar_tensor_tensor(out=nb[:], in0=mv[:, 0:1], scalar=-1.0, in1=rs[:],
                                                 op0=ALU.mult, op1=ALU.mult), [b_st], [b_st])


class LNScratch:
    def __init__(self, cx, es):
        self.st = cx.sb([128, 12], F32, es)
        self.mv = cx.sb([128, 2], F32, es)
        self.rs = cx.sb([128, 1], F32, es)
        self.nb = cx.sb([128, 1], F32, es)
        self.b = Buf()


def emit_ln_mod(cx, x_ap, b_x, lns, xn, b_xn, A_ap, B_ap, b_ab, out_ap, b_out, tmp, b_tmp):
    S = cx.S
    emit_ln_stats(cx, x_ap, b_x, lns.st, lns.mv, lns.rs, lns.nb, lns.b)
    S.op("act", lambda e: e.activation(out=xn[:], in_=x_ap, func=AF.Identity, bias=lns.nb[:], scale=lns.rs[:]),
         [b_x, lns.b], [b_xn])
    S.op("dve", lambda e: e.tensor_tensor(out=tmp[:], in0=xn[:], in1=A_ap, op=ALU.mult), [b_xn, b_ab], [b_tmp])
    S.op("pool", lambda e: e.tensor_tensor(out=out_ap, in0=tmp[:], in1=B_ap, op=ALU.add), [b_tmp, b_ab], [b_out])


def build_A():
    nc = bass.Bass("TRN2", target_bir_lowering=False)
    with ExitStack() as es:
        cx = Ctx(nc, es)
        S = cx.S
        x_d = cx.dram_in("x", [TPC, D], F32)
        cT_d = cx.dram_in("cT", [128, 8], F32)
        w_ada_d = cx.dram_in("w_ada", [D, 6 * D], F32)
        b_ada_d = cx.dram_in("b_ada", [1, 6 * D], F32)
        ident_d = cx.dram_in("ident_bf", [128, 128], BF16)
        uT_d = cx.dram_out("uT", [D, TPC], BF16)
        ada_d = cx.dram_out("ada", [1, 6 * D], F32)

        ada = cx.sb([128, 6 * D], F32)
        b_ada = Buf()
        emit_ada(cx, cT_d, w_ada_d, b_ada_d, ada, b_ada)
        b_adaout = Buf()
        S.dma("sp", ada_d, ada[0:1, :], reads=[b_ada], writes=[b_adaout])
        S.finish([b_adaout], "sp")
        ident = cx.sb([128, 128], BF16)
        b_id = Buf()
        S.dma("sp", ident[:], ident_d, writes=[b_id])
        S.op("dve", lambda e: e.tensor_scalar_add(out=ada[:, D:2 * D], in0=ada[:, D:2 * D], scalar1=1.0), [b_ada], [b_ada])
        emit_A_body(cx, x_d, None, None, ada, b_ada, ident, b_id, uT_d)
    return nc


def emit_A_body(cx, x_d, xres, b_xres, ada, b_ada, ident, b_id, uT_d, sh_off=0, sc_off=D):
    S = cx.S
    with ExitStack() as es:
        lns = LNScratch(cx, es)
        xin = [cx.sb([128, D], F32, es) for _ in range(2)]
        b_xin = [Buf(), Buf()]
        xn = cx.sb([128, D], F32, es)
        b_xn = Buf()
        tmp = cx.sb([128, D], F32, es)
        b_tmp = Buf()
        ub = [cx.sb([128, D], BF16, es) for _ in range(2)]
        b_ub = [Buf(), Buf()]
        pT = [cx.ps([128, 8, 128], BF16, es) for _ in range(2)]
        b_pT = [Buf(), Buf()]
        uTs = [cx.sb([128, 8, 128], BF16, es) for _ in range(2)]
        b_uTs = [Buf(), Buf()]
        outs = []
        uT_v = uT_d.rearrange("(k p) t -> p k t", p=128)
        for t in range(NT):
            i = t % 2
            if x_d is not None:
                S.dma("sp", xin[i][:], x_d[t * 128:(t + 1) * 128, :], writes=[b_xin[i]])
                x_ap, bx = xin[i][:], b_xin[i]
            else:
                x_ap, bx = xres[:, t, :], b_xres[t]
            emit_ln_mod(cx, x_ap, bx, lns, xn, b_xn, ada[:, sc_off:sc_off + D], ada[:, sh_off:sh_off + D], b_ada,
                        ub[i][:], b_ub[i], tmp, b_tmp)
            for k in range(8):
                S.op("pe", lambda e: e.transpose(pT[i][:, k, :], ub[i][:, k * 128:(k + 1) * 128], ident[:]),
                     [b_ub[i], b_id], [b_pT[i]])
            S.op("act", lambda e: e.activation(out=uTs[i][:], in_=pT[i][:], func=AF.Copy), [b_pT[i]], [b_uTs[i]])
            outs.append(Buf())
            S.dma("sp", uT_v[:, :, t * 128:(t + 1) * 128], uTs[i][:], reads=[b_uTs[i]], writes=[outs[-1]])
        S.finish(outs, "sp")
        S.barrier()


LN_INV_SQRT_DH = float(-0.5 * np.log(DH))
GELU_C = 1.5957691216057308


def build_B():
    nc = bass.Bass("TRN2", target_bir_lowering=False)
    with ExitStack() as es:
        cx = Ctx(nc, es)
        d = {
            "uT": cx.dram_in("uT", [D, SEQ], BF16),
            "wh": cx.dram_in("wh", [D, 770], F32),
            "pp": cx.dram_in("pp", [128, 24], F32),
            "fv": cx.dram_in("fv", [1, 258], F32),
            "wa": cx.dram_in("wa", [128, 128], F32),
            "wx": cx.dram_in("wx", [128, 128], F32),
            "ident_bf": cx.dram_in("ident_bf", [128, 128], BF16),
            "tri_f": cx.dram_in("tri_f", [128, 128], F32),
            "ones_f": cx.dram_in("ones_f", [128, 128], F32),
            "ones_bf": cx.dram_in("ones_bf", [128, 128], BF16),
            "yT": cx.dram_out("yT", [256, SEQ], BF16),
        }
        outs = emit_B(cx, d)
        cx.S.finish(outs, "sp")
    return nc


def emit_B(cx, d, nblk=NBLK):
    S = cx.S
    outs = []
    NCH = BLK // 128
    with ExitStack() as es:
        def sb(shape, dt):
            return cx.sb(shape, dt, es)

        def sbn(shape, dt, n=2):
            return [cx.sb(shape, dt, es) for _ in range(n)], [Buf() for _ in range(n)]

        W = sb([128, 8, 770], BF16); b_W = Buf()
        S.dma("pool", W[:], d["wh"].rearrange("(k p) n -> p k n", p=128), writes=[b_W])
        wa = sb([128, 128], BF16); b_wa = Buf()
        S.dma("pool", wa[:], d["wa"], writes=[b_wa])
        wx = sb([128, 128], BF16); b_wx = Buf()
        S.dma("pool", wx[:], d["wx"], writes=[b_wx])
        pp = sb([128, 24], F32); b_pp = Buf()
        S.dma("sp", pp[:], d["pp"], writes=[b_pp])
        fv = sb([128, 258], F32); b_fv = Buf()
        S.dma("sp", fv[:], d["fv"].partition_broadcast(128), writes=[b_fv])
        ident = sb([128, 128], BF16); b_id = Buf()
        S.dma("sp", ident[:], d["ident_bf"], writes=[b_id])
        tri = sb([128, 128], F32); b_tri = Buf()
        S.dma("sp", tri[:], d["tri_f"], writes=[b_tri])
        ones_f = sb([128, 128], F32); b_of = Buf()
        S.dma("sp", ones_f[:], d["ones_f"], writes=[b_of])
        ones_bf = sb([128, 128], BF16); b_ob = Buf()
        S.dma("sp", ones_bf[:], d["ones_bf"], writes=[b_ob])
        der = sb([128, 4], F32); b_der = Buf()
        S.op("act", lambda e: e.activation(out=der[:, 3:4], in_=pp[:, 21:22], func=AF.Exp, scale=-1.0), [b_pp], [b_der])
        S.op("act", lambda e: e.activation(out=der[:, 3:4], in_=der[:, 3:4], func=AF.Ln, bias=1.0, scale=1.0), [b_der], [b_der])
        S.op("dve", lambda e: e.tensor_scalar_mul(out=der[:, 0:1], in0=der[:, 3:4], scalar1=-8.0), [b_der], [b_der])
        S.op("dve", lambda e: e.tensor_scalar_mul(out=der[:, 1:2], in0=der[:, 3:4], scalar1=-16.0), [b_der], [b_der])
        S.op("dve", lambda e: e.tensor_scalar_mul(out=der[:, 2:3], in0=pp[:, 22:23], scalar1=float(np.sqrt(128.0))),
             [b_pp, b_der], [b_der])

        dg = {}
        b_dg = Buf()
        for nm, wc in (("q", 0), ("k", 4), ("x", 8)):
            dg[nm] = sb([128, 4, 128], BF16)
            for k in range(4):
                S.op("dve", lambda e: e.tensor_scalar_mul(out=dg[nm][:, k, :], in0=ident[:], scalar1=pp[:, wc + k:wc + k + 1]),
                     [b_id, b_pp], [b_dg])
        C32 = sb([128, 129], F32); b_C32 = Buf()
        Cbf = sb([128, 129], BF16); b_Cbf = Buf()
        S.op("dve", lambda e: e.memset(C32[:], 0.0), [], [b_C32])
        S.op("dve", lambda e: e.memset(Cbf[:], 0.0), [], [b_Cbf])
        hz = sb([128, 1], F32); b_hz = Buf()
        S.op("dve", lambda e: e.memset(hz[:], 0.0), [], [b_hz])

        uTb, b_uTb = sbn([128, 8, BLK], BF16)
        pre = {}
        b_pre = {}
        for nm in ("q", "k", "x"):
            pre[nm], b_pre[nm] = sbn([128, BLK + 8], BF16)
            for i in range(2):
                S.op("pool", lambda e: e.memset(pre[nm][i][:, 0:3], 0.0), [], [b_pre[nm][i]])
        gpre, b_gpre = sbn([128, BLK], F32)
        acc = {}
        b_acc = {}
        for nm in ("x",):
            acc[nm], b_acc[nm] = sbn([128, BLK], F32)
        qT, b_qT = sbn([128, BLK], BF16)
        kT, b_kT = sbn([128, BLK], BF16)
        vo, b_vo = sbn([128, NCH, 256], F32)
        iff, b_if = sbn([128, NCH, 2], F32)
        sm, b_sm = sbn([128, 64], F32)
        vaug, b_vaug = sbn([128, NCH, 144], BF16)
        kc, b_kc = sbn([128, NCH, 128], BF16)
        PT, b_PT = sbn([128, NCH, 128], BF16)
        hm, b_hm = sbn([128, NCH, 128], F32)
        hq, b_hq = sbn([128, NCH, 128], F32)
        so, b_so = sbn([128, NCH, 128], F32)
        ym, b_ym = sbn([128, NCH, 128], BF16)
        tC, b_tC = sbn([128, 129], F32)
        ymT, b_ymT = sbn([128, BLK], BF16)
        yrT, b_yrT = sbn([128, BLK], BF16)
        xcb, b_xcb = sbn([128, BLK], BF16)
        names = ["r", "ig", "a", "a2", "xi", "h", "g2", "sg", "gl", "yr", "sd", "yn"]
        L = {}
        bL = {}
        for nm in names:
            L[nm], bL[nm] = sbn([128, BLK], F32)
        ysq, b_ysq = sbn([128, BLK], BF16)

        pfm = [cx.ps([128, 512], F32, es) for _ in range(2)]
        b_pfm = [Buf(), Buf()]
        pvo = [cx.ps([128, 2, 256], F32, es) for _ in range(2)]
        b_pvo = [Buf(), Buf()]
        pTr = cx.ps([128, 512], F32, es); b_pTr = Buf()
        psm = cx.ps([128, 512], F32, es); b_psm = Buf()
        pST = cx.ps([128, NCH, 128], F32, es); b_pST = Buf()
        pnum = cx.ps([128, NCH, 128], F32, es); b_pnum = Buf()
        p_kc = pTr[:, 0:256].bitcast(BF16).rearrange("p (c n) -> p c n", c=NCH)
        p_ym = pTr[:, 256:512].bitcast(BF16).rearrange("p (c n) -> p c n", c=NCH)
        p_cs = psm[:, 0:8]
        p_if = psm[:, 8:16].rearrange("p (c n) -> p c n", c=NCH)
        p_den = psm[:, 16:20]
        p_upd = psm[:, 32:161]
        fmstate = {"n": 0}

        def fm_bank():
            n = fmstate["n"] % 2
            fmstate["n"] += 1
            return pfm[n], b_pfm[n]

        uT_v = d["uT"].rearrange("(k p) t -> p k t", p=128)

        def emit_proj(blk):
            i = blk % 2
            t0 = blk * BLK
            S.dma("sp", uTb[i][:], uT_v[:, :, t0:t0 + BLK], writes=[b_uTb[i]])
            if blk > 0:
                for nm in ("q", "k", "x"):
                    S.op("pool", lambda e: e.tensor_copy(out=pre[nm][i][:, 0:3], in_=pre[nm][1 - i][:, BLK:BLK + 3]),
                         [b_pre[nm][1 - i]], [b_pre[nm][i]])
            for gi, nm in enumerate(("q", "k", "x", "g")):
                p, bp = fm_bank()
                for k in range(8):
                    S.op("pe", lambda e: e.matmul(p[:], lhsT=W[:, k, gi * 128:(gi + 1) * 128], rhs=uTb[i][:, k, :],
                                                  start=(k == 0), stop=(k == 7)), [b_W, b_uTb[i]], [bp])
                if nm == "g":
                    S.op("act", lambda e: e.activation(out=gpre[i][:], in_=p[:], func=AF.Identity,
                                                       bias=pp[:, 18:19], scale=1.0), [b_pp], [bp, b_gpre[i]])
                else:
                    S.op("act", lambda e: e.activation(out=pre[nm][i][:, 3:BLK + 3], in_=p[:], func=AF.Identity,
                                                       bias=pp[:, 15 + gi:16 + gi], scale=1.0), [b_pp], [bp, b_pre[nm][i]])
            for cp in range(2):
                for cc in range(2):
                    c = cp * 2 + cc
                    for k in range(8):
                        S.op("pe", lambda e: e.matmul(pvo[cp][:, cc, :], lhsT=uTb[i][:, k, c * 128:(c + 1) * 128],
                                                      rhs=W[:, k, 512:768], start=(k == 0), stop=(k == 7)),
                             [b_W, b_uTb[i]], [b_pvo[cp]])
                S.op("dve", lambda e: e.tensor_tensor(out=vo[i][:, cp * 2:cp * 2 + 2, :], in0=pvo[cp][:],
                                                      in1=fv[:, 0:256].unsqueeze(1).to_broadcast([128, 2, 256]), op=ALU.add),
                     [b_fv], [b_pvo[cp], b_vo[i]])
            for c in range(NCH):
                for k in range(8):
                    S.op("pe", lambda e: e.matmul(p_if[:, c, :], lhsT=uTb[i][:, k, c * 128:(c + 1) * 128],
                                                  rhs=W[:, k, 768:770], start=(k == 0), stop=(k == 7)),
                         [b_W, b_uTb[i]], [b_psm])
            S.op("dve", lambda e: e.tensor_tensor(out=iff[i][:], in0=p_if, in1=fv[:, 256:258].unsqueeze(1).to_broadcast([128, NCH, 2]),
                                                  op=ALU.add), [b_fv], [b_psm, b_if[i]])
            for nm, bc in (("q", 12), ("k", 13), ("x", 14)):
                p, bp = fm_bank()
                for k in range(4):
                    S.op("pe", lambda e: e.matmul(p[:], lhsT=dg[nm][:, k, :], rhs=pre[nm][i][:, k:k + BLK],
                                                  start=(k == 0), stop=(k == 3)), [b_dg, b_pre[nm][i]], [bp])
                if nm == "q":
                    S.op("act", lambda e: e.activation(out=qT[i][:], in_=p[:], func=AF.Silu, bias=pp[:, bc:bc + 1], scale=1.0),
                         [b_pp], [bp, b_qT[i]])
                elif nm == "k":
                    S.op("act", lambda e: e.activation(out=kT[i][:], in_=p[:], func=AF.Silu, bias=pp[:, bc:bc + 1], scale=1.0),
                         [b_pp], [bp, b_kT[i]])
                else:
                    S.op("act", lambda e: e.activation(out=acc["x"][i][:], in_=p[:], func=AF.Identity, bias=pp[:, bc:bc + 1], scale=1.0),
                         [b_pp], [bp, b_acc["x"][i]])

        def emit_compute(blk):
            i = blk % 2
            t0 = blk * BLK
            s_ = sm[i]; bs = b_sm[i]
            bc4 = lambda ap: ap.unsqueeze(2).to_broadcast([128, NCH, 128])
            S.op("act", lambda e: e.activation(out=s_[:, 0:4], in_=iff[i][:, :, 1], func=AF.Exp, scale=-1.0), [b_if[i]], [bs])
            S.op("act", lambda e: e.activation(out=s_[:, 4:8], in_=s_[:, 0:4], func=AF.Ln, bias=1.0, scale=1.0), [bs], [bs])
            S.op("pe", lambda e: e.matmul(p_cs[:, 0:4], lhsT=tri[:], rhs=s_[:, 4:8], start=True, stop=True), [b_tri, bs], [b_psm])
            S.op("pe", lambda e: e.matmul(p_cs[:, 4:8], lhsT=ones_f[:], rhs=s_[:, 4:8], start=True, stop=True), [b_of, bs], [b_psm])
            S.op("dve", lambda e: e.tensor_tensor(out=s_[:, 8:12], in0=iff[i][:, :, 0], in1=p_cs[:, 0:4], op=ALU.add),
                 [b_if[i]], [b_psm, bs])
            S.op("act", lambda e: e.activation(out=s_[:, 12:16], in_=s_[:, 8:12], func=AF.Exp, bias=LN_INV_SQRT_DH, scale=1.0), [bs], [bs])
            S.op("act", lambda e: e.activation(out=s_[:, 16:24], in_=p_cs, func=AF.Exp, scale=-1.0), [], [b_psm, bs])
            ws = s_[:, 12:16]
            eb = s_[:, 16:20]
            S.op("dve", lambda e: e.tensor_tensor(out=vaug[i][:, :, 0:128], in0=vo[i][:, :, 0:128], in1=bc4(ws), op=ALU.mult),
                 [b_vo[i], bs], [b_vaug[i]])
            S.op("dve", lambda e: e.tensor_copy(out=vaug[i][:, :, 128], in_=ws), [bs], [b_vaug[i]])
            for c in range(NCH):
                S.op("pe", lambda e: e.transpose(p_kc[:, c, :], kT[i][:, c * 128:(c + 1) * 128], ident[:]), [b_kT[i], b_id], [b_pTr])
            S.op("act", lambda e: e.activation(out=kc[i][:], in_=p_kc, func=AF.Copy), [], [b_pTr, b_kc[i]])
            for c in range(NCH):
                cs = slice(c * 128, (c + 1) * 128)
                S.op("pe", lambda e: e.matmul(pST[:, c, :], lhsT=kT[i][:, cs], rhs=qT[i][:, cs], start=True, stop=True),
                     [b_kT[i], b_qT[i]], [b_pST])
            S.op("dve", lambda e: e.tensor_tensor(out=PT[i][:], in0=pST[:], in1=tri[:].unsqueeze(1).to_broadcast([128, NCH, 128]),
                                                  op=ALU.mult), [b_tri], [b_pST, b_PT[i]])
            for c in range(NCH):
                cs = slice(c * 128, (c + 1) * 128)
                S.op("pe", lambda e: e.matmul(pnum[:, c, :], lhsT=PT[i][:, c, :], rhs=vaug[i][:, c, 0:128], start=True, stop=False),
                     [b_PT[i], b_vaug[i]], [b_pnum])
                S.op("pe", lambda e: e.matmul(pnum[:, c, :], lhsT=qT[i][:, cs], rhs=Cbf[:, 0:128], start=False, stop=True),
                     [b_qT[i], b_Cbf], [b_pnum])
                S.op("pe", lambda e: e.matmul(p_den[:, c:c + 1], lhsT=PT[i][:, c, :], rhs=vaug[i][:, c, 128:129], start=True, stop=False),
                     [b_PT[i], b_vaug[i]], [b_psm])
                S.op("pe", lambda e: e.matmul(p_den[:, c:c + 1], lhsT=qT[i][:, cs], rhs=Cbf[:, 128:129], start=False, stop=True),
                     [b_qT[i], b_Cbf], [b_psm])
                S.op("pe", lambda e: e.matmul(p_upd, lhsT=kc[i][:, c, :], rhs=vaug[i][:, c, 0:129], start=True, stop=True),
                     [b_kc[i], b_vaug[i]], [b_psm])
                j = c % 2
                S.op("dve", lambda e: e.tensor_tensor(out=tC[j][:], in0=p_upd, in1=C32[:], op=ALU.add), [b_C32], [b_psm, b_tC[j]])
                S.op("dve", lambda e: e.tensor_tensor(out=C32[:], in0=tC[j][:], in1=s_[:, 20 + c:21 + c].to_broadcast([128, 129]), op=ALU.mult),
                     [b_tC[j], bs], [b_C32])
                S.op("act", lambda e: e.activation(out=Cbf[:], in_=tC[j][:], func=AF.Copy, scale=s_[:, 20 + c:21 + c]),
                     [b_tC[j], bs], [b_Cbf])
            S.op("dve", lambda e: e.tensor_tensor(out=s_[:, 24:28], in0=p_den, in1=eb, op=ALU.mult), [], [b_psm, bs])
            S.op("act", lambda e: e.activation(out=s_[:, 24:28], in_=s_[:, 24:28], func=AF.Abs), [bs], [bs])
            S.op("dve", lambda e: e.tensor_scalar_max(out=s_[:, 24:28], in0=s_[:, 24:28], scalar1=1.0), [bs], [bs])
            S.op("dve", lambda e: e.reciprocal(out=s_[:, 28:32], in_=s_[:, 24:28]), [bs], [bs])
            S.op("dve", lambda e: e.tensor_tensor(out=s_[:, 28:32], in0=s_[:, 28:32], in1=eb, op=ALU.mult), [bs], [bs])
            S.op("dve", lambda e: e.tensor_tensor(out=hm[i][:], in0=pnum[:], in1=bc4(s_[:, 28:32]), op=ALU.mult), [bs], [b_pnum, b_hm[i]])
            S.op("dve", lambda e: e.tensor_reduce(out=s_[:, 32:36], in_=hm[i][:], axis=AX.X, op=ALU.add), [b_hm[i]], [bs])
            S.op("act", lambda e: e.activation(out=hq[i][:], in_=hm[i][:], func=AF.Square), [b_hm[i]], [b_hq[i]])
            S.op("dve", lambda e: e.tensor_reduce(out=s_[:, 36:40], in_=hq[i][:], axis=AX.X, op=ALU.add), [b_hq[i]], [bs])
            S.op("dve", lambda e: e.tensor_scalar_mul(out=s_[:, 32:36], in0=s_[:, 32:36], scalar1=1.0 / 128.0), [bs], [bs])
            S.op("dve", lambda e: e.tensor_tensor(out=s_[:, 40:44], in0=s_[:, 32:36], in1=s_[:, 32:36], op=ALU.mult), [bs], [bs])
            S.op("dve", lambda e: e.scalar_tensor_tensor(out=s_[:, 36:40], in0=s_[:, 36:40], scalar=1.0 / 128.0, in1=s_[:, 40:44],
                                                         op0=ALU.mult, op1=ALU.subtract), [bs], [bs])
            S.op("act", lambda e: e.activation(out=s_[:, 36:40], in_=s_[:, 36:40], func=AF.Ln, bias=EPS, scale=1.0), [bs], [bs])
            S.op("act", lambda e: e.activation(out=s_[:, 36:40], in_=s_[:, 36:40], func=AF.Exp, scale=-0.5), [bs], [bs])
            S.op("dve", lambda e: e.tensor_tensor(out=hm[i][:], in0=hm[i][:], in1=bc4(s_[:, 32:36]), op=ALU.subtract), [bs], [b_hm[i]])
            S.op("dve", lambda e: e.tensor_tensor(out=hm[i][:], in0=hm[i][:], in1=bc4(s_[:, 36:40]), op=ALU.mult), [bs], [b_hm[i]])
            S.op("act", lambda e: e.activation(out=so[i][:], in_=vo[i][:, :, 128:256], func=AF.Sigmoid), [b_vo[i]], [b_so[i]])
            S.op("dve", lambda e: e.tensor_tensor(out=ym[i][:], in0=hm[i][:], in1=so[i][:], op=ALU.mult), [b_hm[i], b_so[i]], [b_ym[i]])
            for c in range(NCH):
                S.op("pe", lambda e: e.transpose(p_ym[:, c, :], ym[i][:, c, :], ident[:]), [b_ym[i], b_id], [b_pTr])
            S.op("act", lambda e: e.activation(out=ymT[i][:].rearrange("p (c n) -> p c n", c=NCH), in_=p_ym, func=AF.Copy,
                                               scale=pp[:, 23:24]), [b_pp], [b_pTr, b_ymT[i]])
            outs.append(Buf())
            S.dma("sp", d["yT"][0:128, t0:t0 + BLK], ymT[i][:], reads=[b_ymT[i]], writes=[outs[-1]])

            xc = acc["x"][i]; b_xc = b_acc["x"][i]
            S.op("act", lambda e: e.activation(out=xcb[i][:], in_=xc[:], func=AF.Copy), [b_xc], [b_xcb[i]])
            p, bp = fm_bank()
            S.op("pe", lambda e: e.matmul(p[:], lhsT=wa[:], rhs=xcb[i][:], start=True, stop=True), [b_wa, b_xcb[i]], [bp])
            S.op("act", lambda e: e.activation(out=L["r"][i][:], in_=p[:], func=AF.Sigmoid, bias=pp[:, 19:20], scale=1.0),
                 [b_pp], [bp, bL["r"][i]])
            p, bp = fm_bank()
            S.op("pe", lambda e: e.matmul(p[:], lhsT=wx[:], rhs=xcb[i][:], start=True, stop=True), [b_wx, b_xcb[i]], [bp])
            S.op("act", lambda e: e.activation(out=L["ig"][i][:], in_=p[:], func=AF.Sigmoid, bias=pp[:, 20:21], scale=1.0),
                 [b_pp], [bp, bL["ig"][i]])
            g = gpre[i]; bg = b_gpre[i]
            S.op("pool", lambda e: e.tensor_tensor(out=L["g2"][i][:], in0=g[:], in1=g[:], op=ALU.mult), [bg], [bL["g2"][i]])
            S.op("pool", lambda e: e.tensor_scalar(out=L["g2"][i][:], in0=L["g2"][i][:], scalar1=0.044715, scalar2=1.0,
                                                   op0=ALU.mult, op1=ALU.add), [], [bL["g2"][i]])
            S.op("pool", lambda e: e.tensor_tensor(out=L["g2"][i][:], in0=L["g2"][i][:], in1=g[:], op=ALU.mult), [bg], [bL["g2"][i]])
            S.op("act", lambda e: e.activation(out=L["sg"][i][:], in_=L["g2"][i][:], func=AF.Sigmoid, scale=GELU_C),
                 [bL["g2"][i]], [bL["sg"][i]])
            S.op("act", lambda e: e.activation(out=L["a"][i][:], in_=L["r"][i][:], func=AF.Exp, scale=der[:, 0:1]),
                 [bL["r"][i], b_der], [bL["a"][i]])
            S.op("act", lambda e: e.activation(out=L["a2"][i][:], in_=L["r"][i][:], func=AF.Exp, scale=der[:, 1:2]),
                 [bL["r"][i], b_der], [bL["a2"][i]])
            S.op("act", lambda e: e.activation(out=L["a2"][i][:], in_=L["a2"][i][:], func=AF.Ln, bias=1.0, scale=-1.0),
                 [bL["a2"][i]], [bL["a2"][i]])
            S.op("act", lambda e: e.activation(out=L["a2"][i][:], in_=L["a2"][i][:], func=AF.Exp, scale=0.5),
                 [bL["a2"][i]], [bL["a2"][i]])
            S.op("pool", lambda e: e.tensor_tensor(out=L["xi"][i][:], in0=L["ig"][i][:], in1=xc[:], op=ALU.mult),
                 [bL["ig"][i], b_xc], [bL["xi"][i]])
            S.op("dve", lambda e: e.tensor_tensor(out=L["xi"][i][:], in0=L["xi"][i][:], in1=L["a2"][i][:], op=ALU.mult),
                 [bL["a2"][i]], [bL["xi"][i]])
            init = hz[:, 0:1] if blk == 0 else L["h"][1 - i][:, BLK - 1:BLK]
            b_init = b_hz if blk == 0 else bL["h"][1 - i]
            S.op("dve", lambda e: e.tensor_tensor_scan(out=L["h"][i][:], data0=L["a"][i][:], data1=L["xi"][i][:], initial=init,
                                                       op0=ALU.mult, op1=ALU.add),
                 [bL["a"][i], bL["xi"][i], b_init], [bL["h"][i]])
            g = gpre[i]; bg = b_gpre[i]
            S.op("pool", lambda e: e.tensor_tensor(out=L["gl"][i][:], in0=L["sg"][i][:], in1=g[:], op=ALU.mult),
                 [bL["sg"][i], bg], [bL["gl"][i]])
            S.op("dve", lambda e: e.tensor_tensor(out=L["yr"][i][:], in0=L["h"][i][:], in1=L["gl"][i][:], op=ALU.mult),
                 [bL["h"][i], bL["gl"][i]], [bL["yr"][i]])
            S.op("act", lambda e: e.activation(out=ysq[i][:], in_=L["yr"][i][:], func=AF.Square), [bL["yr"][i]], [b_ysq[i]])
            p, bp = fm_bank()
            S.op("pe", lambda e: e.matmul(p[:], lhsT=ones_bf[:], rhs=ysq[i][:], start=True, stop=True), [b_ob, b_ysq[i]], [bp])
            S.op("act", lambda e: e.activation(out=L["sd"][i][:], in_=p[:], func=AF.Ln, bias=128.0 * EPS, scale=1.0),
                 [], [bp, bL["sd"][i]])
            S.op("act", lambda e: e.activation(out=L["sd"][i][:], in_=L["sd"][i][:], func=AF.Exp, scale=-0.5), [], [bL["sd"][i]])
            S.op("pool", lambda e: e.tensor_tensor(out=L["yn"][i][:], in0=L["yr"][i][:], in1=L["sd"][i][:], op=ALU.mult),
                 [bL["yr"][i], bL["sd"][i]], [bL["yn"][i]])
            S.op("act", lambda e: e.activation(out=yrT[i][:], in_=L["yn"][i][:], func=AF.Copy, scale=der[:, 2:3]),
                 [bL["yn"][i], b_der], [b_yrT[i]])
            outs.append(Buf())
            S.dma("sp", d["yT"][128:256, t0:t0 + BLK], yrT[i][:], reads=[b_yrT[i]], writes=[outs[-1]])

        emit_proj(0)
        for blk in range(nblk):
            if blk + 1 < nblk:
                emit_proj(blk + 1)
            emit_compute(blk)
        S.barrier()
    return outs


def pack_B_inputs(inp, l, h, uT_full, hc):
    w_in = inp["w_in"][l]
    b_in = inp["b_in"][l]
    hs = slice(h * 128, (h + 1) * 128)
    o_q, o_k, o_v, o_o, o_i, o_f, o_x, o_g = 0, 1024, 2048, 3072, 4096, 4104, 4112, 5136
    cols = np.concatenate([np.arange(o_q + h * 128, o_q + (h + 1) * 128), np.arange(o_k + h * 128, o_k + (h + 1) * 128),
                           np.arange(o_x + h * 128, o_x + (h + 1) * 128), np.arange(o_g + h * 128, o_g + (h + 1) * 128),
                           np.arange(o_v + h * 128, o_v + (h + 1) * 128), np.arange(o_o + h * 128, o_o + (h + 1) * 128),
                           np.array([o_i + h, o_f + h])])
    wh = np.ascontiguousarray(w_in[:, cols])
    bh = b_in[cols]
    pp = np.zeros((128, 24), np.float32)
    pp[:, 0:4] = inp["w_conv_m"][l][:, hs].T
    pp[:, 4:8] = inp["w_conv_m"][l][:, 1024 + h * 128:1024 + (h + 1) * 128].T
    pp[:, 8:12] = inp["w_conv_r"][l][:, hs].T
    pp[:, 12] = inp["b_conv_m"][l][hs]
    pp[:, 13] = inp["b_conv_m"][l][1024 + h * 128:1024 + (h + 1) * 128]
    pp[:, 14] = inp["b_conv_r"][l][hs]
    pp[:, 15] = bh[0:128]
    pp[:, 16] = bh[128:256]
    pp[:, 17] = bh[256:384]
    pp[:, 18] = bh[384:512]
    pp[:, 19] = inp["b_a"][l][hs]
    pp[:, 20] = inp["b_x"][l][hs]
    pp[:, 21] = inp["lru_lambda"][l][hs]
    pp[:, 22] = inp["lru_norm_g"][l][hs]
    pp[:, 23] = inp["mh_norm_g"][l][hs]
    fv = np.ascontiguousarray(bh[512:770][None, :])
    return {"uT": uT_full, "wh": wh, "pp": pp, "fv": fv,
            "wa": np.ascontiguousarray(inp["w_a"][l][h]), "wx": np.ascontiguousarray(inp["w_x"][l][h]),
            "ident_bf": hc["ident_bf"], "tri_f": hc["tri_f"], "ones_f": hc["ones_f"], "ones_bf": hc["ones_bf"]}


NSLOT = NE * CAP
BIGROW = float(NSLOT + 64)


def build_C(debug=False):
    nc = bass.Bass("TRN2", target_bir_lowering=False)
    with ExitStack() as es:
        cx = Ctx(nc, es)
        S = cx.S
        d = {
            "x": cx.dram_in("x", [TPC, D], F32),
            "yT": cx.dram_in("yT", [2 * D, TPC], BF16),
            "ada": cx.dram_in("ada", [1, 6 * D], F32),
            "w_out": cx.dram_in("w_out", [2 * D, D], F32),
            "lnp": cx.dram_in("lnp", [1, 4 * D], F32),
            "w_router": cx.dram_in("w_router", [D, NE], F32),
            "b_router": cx.dram_in("b_router", [1, NE], F32),
            "eoff": cx.dram_in("eoff", [1, NE], F32),
            "w_gate": cx.dram_in("w_gate", [NE, D, DFF], F32),
            "w_up": cx.dram_in("w_up", [NE, D, DFF], F32),
            "w_down": cx.dram_in("w_down", [NE, DFF, D], F32),
            "ident_bf": cx.dram_in("ident_bf", [128, 128], BF16),
            "ident_f": cx.dram_in("ident_f", [128, 128], F32),
            "tris_bf": cx.dram_in("tris_bf", [128, 128], BF16),
            "ones_bf": cx.dram_in("ones_bf", [128, 128], BF16),
            "xout": cx.dram_out("xout", [TPC, D], F32),
        }
        if debug:
            d["xbuf"] = nc.dram_tensor("xbuf", [NSLOT, D], BF16, kind="ExternalOutput")
            d["ybuf"] = nc.dram_tensor("ybuf", [NSLOT, D], F32, kind="ExternalOutput")
            d["dbg_dest"] = cx.dram_out("dbg_dest", [128, NT * 2], I32)
            d["dbg_gate"] = cx.dram_out("dbg_gate", [128, NT, 2], F32)
        else:
            d["xbuf"] = nc.dram_tensor("xbuf", [NSLOT, D], BF16)
            d["ybuf"] = nc.dram_tensor("ybuf", [NSLOT, D], F32)
        adac = cx.sb([128, 4, D], F32)
        b_adac = Buf()
        S.dma("sp", adac[:], d["ada"][:, 2 * D:6 * D].partition_broadcast(128).rearrange("p o (a n) -> p (o a) n", a=4),
              writes=[b_adac])
        S.op("dve", lambda e: e.tensor_scalar_add(out=adac[:, 2, :], in0=adac[:, 2, :], scalar1=1.0), [b_adac], [b_adac])
        d["x1s"] = nc.dram_tensor("x1s", [TPC, D], F32)
        outs = emit_C(cx, d, d["x"], None, None, adac[:, 0, :], adac[:, 1, :], adac[:, 2, :], adac[:, 3, :], b_adac,
                      d["xout"])
        S.finish(outs, "sp")
    return nc


def emit_C(cx, d, x_d, xres, b_xres, g1_ap, sh2_ap, sc2_ap, g2_ap, b_ada, xout_d):
    S = cx.S
    nc = cx.nc
    outs = []
    with ExitStack() as es0:
        ident = cx.sb([128, 128], BF16, es0); b_id = Buf()
        S.dma("sp", ident[:], d["ident_bf"], writes=[b_id])
        scat = []
        bc_reg = nc.gpsimd.alloc_register(f"bc{cx._n}")
        nc.gpsimd.reg_mov(bc_reg, NSLOT - 1)
        zt = cx.sb([128, D], BF16, es0); b_zt = Buf()
        S.op("pool", lambda e: e.memset(zt[:], 0.0), [], [b_zt])
        zfill = []
        for r0 in range(0, NSLOT, 1024):
            zfill.append(Buf())
            S.dma("sp" if (r0 // 1024) % 2 == 0 else "act",
                  d["xbuf"][r0:r0 + 1024, :].rearrange("(p s) n -> p s n", p=128),
                  zt[:].unsqueeze(1).to_broadcast([128, 8, D]), reads=[b_zt], writes=[zfill[-1]])
        NG = NT * NE
        destS = cx.sb([128, 2 * NT], I32, es0)
        destG = cx.sb([128, 2 * NT], I32, es0)
        gate2 = cx.sb([128, NT, 2], F32, es0)
        b_rt = Buf()
        u2b_all = cx.sb([128, NT, D], BF16, es0)
        b_u2b = [Buf() for _ in range(NT)]
        x1w = []
        with ExitStack() as es:
            def sb(shape, dt):
                return cx.sb(shape, dt, es)

            def sbn(shape, dt, n=2):
                return [cx.sb(shape, dt, es) for _ in range(n)], [Buf() for _ in range(n)]

            wout = sb([128, 16, D], BF16); b_wout = Buf()
            wo_v = d["w_out"].rearrange("(m p) n -> p m n", p=128)
            for q4 in range(4):
                S.dma("pool", wout[:, q4 * 4:(q4 + 1) * 4, :], wo_v[:, q4 * 4:(q4 + 1) * 4, :], writes=[b_wout])
            lnp = sb([128, 2, D], F32); b_lnp = Buf()
            S.dma("sp", lnp[:], d["lnp"][:, 0:2 * D].partition_broadcast(128).rearrange("p o (a n) -> p (o a) n", a=2), writes=[b_lnp])
            identf = sb([128, 128], F32); b_idf = Buf()
            S.dma("sp", identf[:], d["ident_f"], writes=[b_idf])
            tris = sb([128, 128], BF16); b_tris = Buf()
            S.dma("sp", tris[:], d["tris_bf"], writes=[b_tris])
            ones = sb([128, 128], BF16); b_ones = Buf()
            S.dma("sp", ones[:], d["ones_bf"], writes=[b_ones])
            wr = sb([128, 8, NE], F32); b_wr = Buf()
            S.dma("sp", wr[:], d["w_router"].rearrange("(k p) n -> p k n", p=128), writes=[b_wr])
            br = sb([128, NE], F32); b_br = Buf()
            S.dma("sp", br[:], d["b_router"].partition_broadcast(128), writes=[b_br])
            eoff = sb([128, NE], F32); b_eoff = Buf()
            S.dma("sp", eoff[:], d["eoff"].partition_broadcast(128), writes=[b_eoff])

            ytb, b_ytb = sbn([128, 16, 256], BF16)
            xin, b_xin = sbn([128, D], F32)
            t1_, b_t1_ = sbn([128, D], F32)
            z_, b_z_ = sbn([128, D], F32)
            x1t_, b_x1t_ = sbn([128, D], F32)
            u2f_, b_u2f_ = sbn([128, D], F32)
            u2T_, b_u2T_ = sbn([128, 8, 128], F32)
            xnA = sb([128, D], F32); b_xnA = Buf()
            tmpA = sb([128, D], F32); b_tmpA = Buf()
            xnB = sb([128, D], F32); b_xnB = Buf()
            tmpB = sb([128, D], F32); b_tmpB = Buf()
            lnsA = LNScratch(cx, es)
            lnsB = LNScratch(cx, es)
            aff_all = sb([128, NT, NE], F32)
            b_aff = [Buf() for _ in range(NT)]

            po = [cx.ps([128, 512], F32, es) for _ in range(2)]
            b_po = [Buf(), Buf()]
            pT = [cx.ps([128, 4, 128], F32, es) for _ in range(2)]
            b_pT = [Buf(), Buf()]
            plog = cx.ps([128, 512], F32, es); b_plog = Buf()
            ppre = cx.ps([128, 512], F32, es); b_ppre = Buf()
            ptot = cx.ps([128, 512], F32, es); b_ptot = Buf()

            yT_v = d["yT"].rearrange("(m p) t -> p m t", p=128)

            def S1(t):
                i = t % 2
                if t % 2 == 0:
                    yi = (t // 2) % 2
                    S.dma("sp", ytb[yi][:], yT_v[:, :, t * 128:t * 128 + 256], writes=[b_ytb[yi]])
                yi = (t // 2) % 2
                tsl = slice((t % 2) * 128, (t % 2) * 128 + 128)
                S.dma("act", xin[i][:], x_d[t * 128:(t + 1) * 128, :], writes=[b_xin[i]])
                for half in range(2):
                    hs = slice(half * 512, (half + 1) * 512)
                    for m in range(16):
                        S.op("pe", lambda e: e.matmul(po[half][:], lhsT=ytb[yi][:, m, tsl], rhs=wout[:, m, hs],
                                                      start=(m == 0), stop=(m == 15)), [b_ytb[yi], b_wout], [b_po[half]])
                    S.op("dve", lambda e: e.tensor_tensor(out=t1_[i][:, hs], in0=po[half][:], in1=g1_ap[:, hs], op=ALU.mult),
                         [b_ada], [b_po[half], b_t1_[i]])
                S.op("dve", lambda e: e.scalar_tensor_tensor(out=z_[i][:], in0=xin[i][:], scalar=ALPHA, in1=t1_[i][:],
                                                             op0=ALU.mult, op1=ALU.add), [b_xin[i], b_t1_[i]], [b_z_[i]])

            def S2(t):
                i = t % 2
                emit_ln_mod(cx, z_[i][:], b_z_[i], lnsA, xnA, b_xnA, lnp[:, 0, :], lnp[:, 1, :], b_lnp, x1t_[i][:], b_x1t_[i], tmpA, b_tmpA)
                x1w.append(Buf())
                S.dma("act", d["x1s"][t * 128:(t + 1) * 128, :], x1t_[i][:], reads=[b_x1t_[i]], writes=[x1w[-1]])

            def S3(t):
                i = t % 2
                emit_ln_mod(cx, x1t_[i][:], b_x1t_[i], lnsB, xnB, b_xnB, sc2_ap, sh2_ap, b_ada, u2f_[i][:], b_u2f_[i], tmpB, b_tmpB)
                S.op("act", lambda e: e.activation(out=u2b_all[:, t, :], in_=u2f_[i][:], func=AF.Copy), [b_u2f_[i]], [b_u2b[t]])

            def S4(t):
                i = t % 2
                u2f, b_u2f, u2T, b_u2T = u2f_[i], b_u2f_[i], u2T_[i], b_u2T_[i]
                for k in range(8):
                    S.op("pe", lambda e: e.transpose(pT[k // 4][:, k % 4, :], u2f[:, k * 128:(k + 1) * 128], identf[:]),
                         [b_u2f, b_idf], [b_pT[k // 4]])
                S.op("act", lambda e: e.activation(out=u2T[:, 0:4, :], in_=pT[0][:], func=AF.Copy), [], [b_pT[0], b_u2T])
                S.op("dve", lambda e: e.tensor_copy(out=u2T[:, 4:8, :], in_=pT[1][:]), [], [b_pT[1], b_u2T])
                for k in range(8):
                    S.op("pe", lambda e: e.matmul(plog[:, 0:NE], lhsT=u2T[:, k, :], rhs=wr[:, k, :], start=(k == 0), stop=(k == 7)),
                         [b_u2T, b_wr], [b_plog])
                S.op("act", lambda e: e.activation(out=aff_all[:, t, :], in_=plog[:, 0:NE], func=AF.Sigmoid), [], [b_plog, b_aff[t]])

            for kk in range(NT + 3):
                if kk < NT:
                    S1(kk)
                if 0 <= kk - 1 < NT:
                    S2(kk - 1)
                if 0 <= kk - 2 < NT:
                    S3(kk - 2)
                if 0 <= kk - 3 < NT:
                    S4(kk - 3)

            RR = sb([128, 12, NG], F32); b_R = Buf()
            aff = aff_all[:].rearrange("p t e -> p (t e)")
            sel, eq, selm, ge, msk, gv, pos, val, vld, dd, cntf = (RR[:, n, :] for n in range(11))
            q3 = lambda ap: ap.rearrange("p (a j) -> p a j", j=4)
            t3 = lambda ap: ap.rearrange("p (t e) -> p t e", e=NE)
            r128 = sb([128, 4, NT * 8], F32)
            m1, m2, gsc, gone = (r128[:, n, :] for n in range(4))
            r16 = sb([128, 8, NT], F32)
            gmax, gsum, rgs, first, second, g1s, gts = (r16[:, n, :] for n in range(7))
            mk = sb([128, NG], BF16); b_mk = Buf()
            S.op("dve", lambda e: e.tensor_tensor(out=t3(sel), in0=aff_all[:], in1=br[:].unsqueeze(1).to_broadcast([128, NT, NE]), op=ALU.add),
                 b_aff + [b_br], [b_R])
            S.op("dve", lambda e: e.tensor_reduce(out=m1, in_=q3(sel), axis=AX.X, op=ALU.max), [], [b_R])
            S.op("dve", lambda e: e.tensor_tensor(out=q3(eq), in0=q3(sel), in1=m1.unsqueeze(2).to_broadcast([128, NT * 8, 4]), op=ALU.is_equal), [], [b_R])
            S.op("dve", lambda e: e.scalar_tensor_tensor(out=selm, in0=eq, scalar=-1e9, in1=sel, op0=ALU.mult, op1=ALU.add), [], [b_R])
            S.op("dve", lambda e: e.tensor_reduce(out=m2, in_=q3(selm), axis=AX.X, op=ALU.max), [], [b_R])
            S.op("dve", lambda e: e.tensor_tensor(out=gsc, in0=m1, in1=m2, op=ALU.add), [], [b_R])
            g8 = lambda ap: ap.rearrange("p (t g) -> p t g", g=8)
            S.op("dve", lambda e: e.tensor_reduce(out=gmax, in_=g8(gsc), axis=AX.X, op=ALU.max), [], [b_R])
            S.op("dve", lambda e: e.tensor_tensor(out=g8(gone), in0=g8(gsc), in1=gmax.unsqueeze(2).to_broadcast([128, NT, 8]), op=ALU.is_equal), [], [b_R])
            S.op("dve", lambda e: e.tensor_tensor(out=q3(ge), in0=q3(sel), in1=m2.unsqueeze(2).to_broadcast([128, NT * 8, 4]), op=ALU.is_ge), [], [b_R])
            S.op("dve", lambda e: e.tensor_tensor(out=q3(msk), in0=q3(ge), in1=gone.unsqueeze(2).to_broadcast([128, NT * 8, 4]), op=ALU.mult), [], [b_R])
            S.op("dve", lambda e: e.tensor_tensor(out=gv, in0=aff, in1=msk, op=ALU.mult), [], [b_R])
            S.op("dve", lambda e: e.tensor_reduce(out=gsum, in_=t3(gv), axis=AX.X, op=ALU.add), [], [b_R])
            S.op("dve", lambda e: e.reciprocal(out=rgs, in_=gsum), [], [b_R])
            S.op("dve", lambda e: e.tensor_tensor(out=t3(gv), in0=t3(gv), in1=rgs.unsqueeze(2).to_broadcast([128, NT, NE]), op=ALU.mult), [], [b_R])
            S.op("dve", lambda e: e.tensor_copy(out=mk[:], in_=msk), [b_R], [b_mk])
            S.op("pe", lambda e: e.matmul(ppre[:], lhsT=tris[:], rhs=mk[:], start=True, stop=True), [b_tris, b_mk], [b_ppre])
            S.op("pe", lambda e: e.matmul(ptot[:], lhsT=ones[:], rhs=mk[:], start=True, stop=True), [b_ones, b_mk], [b_ptot])
            S.op("dve", lambda e: e.tensor_copy(out=dd, in_=ptot[:]), [], [b_ptot, b_R])
            S.op("dve", lambda e: e.memset(cntf[:, 0:NE], 0.0), [], [b_R])
            for t in range(1, NT):
                S.op("dve", lambda e: e.tensor_tensor(out=cntf[:, t * NE:(t + 1) * NE], in0=cntf[:, (t - 1) * NE:t * NE],
                                                      in1=dd[:, (t - 1) * NE:t * NE], op=ALU.add), [], [b_R])
            S.op("dve", lambda e: e.tensor_tensor(out=pos, in0=ppre[:], in1=cntf, op=ALU.add), [], [b_ppre, b_R])
            S.op("dve", lambda e: e.tensor_scalar(out=vld, in0=pos, scalar1=float(CAP) - 0.5, scalar2=None, op0=ALU.is_lt), [], [b_R])
            S.op("dve", lambda e: e.tensor_tensor(out=vld, in0=vld, in1=msk, op=ALU.mult), [], [b_R])
            S.op("dve", lambda e: e.tensor_tensor(out=t3(val), in0=t3(pos), in1=eoff[:].unsqueeze(1).to_broadcast([128, NT, NE]), op=ALU.add),
                 [b_eoff], [b_R])
            S.op("dve", lambda e: e.scalar_tensor_tensor(out=dd, in0=val, scalar=1.0, in1=vld, op0=ALU.add, op1=ALU.mult), [], [b_R])
            S.op("dve", lambda e: e.tensor_scalar_add(out=dd, in0=dd, scalar1=-1.0), [], [b_R])
            S.op("dve", lambda e: e.tensor_reduce(out=first, in_=t3(dd), axis=AX.X, op=ALU.max), [], [b_R])
            S.op("dve", lambda e: e.tensor_tensor(out=t3(eq), in0=t3(dd), in1=first.unsqueeze(2).to_broadcast([128, NT, NE]), op=ALU.is_equal), [], [b_R])
            S.op("dve", lambda e: e.scalar_tensor_tensor(out=selm, in0=eq, scalar=-1e9, in1=dd, op0=ALU.mult, op1=ALU.add), [], [b_R])
            S.op("dve", lambda e: e.tensor_reduce(out=second, in_=t3(selm), axis=AX.X, op=ALU.max), [], [b_R])
            S.op("dve", lambda e: e.tensor_tensor(out=gv, in0=gv, in1=vld, op=ALU.mult), [], [b_R])
            S.op("dve", lambda e: e.tensor_reduce(out=gts, in_=t3(gv), axis=AX.X, op=ALU.add), [], [b_R])
            S.op("dve", lambda e: e.tensor_tensor(out=ge, in0=gv, in1=eq, op=ALU.mult), [], [b_R])
            S.op("dve", lambda e: e.tensor_reduce(out=g1s, in_=t3(ge), axis=AX.X, op=ALU.add), [], [b_R])
            S.op("dve", lambda e: e.tensor_copy(out=gate2[:, :, 0], in_=g1s), [b_R], [b_rt])
            S.op("dve", lambda e: e.tensor_tensor(out=gate2[:, :, 1], in0=gts, in1=g1s, op=ALU.subtract), [b_R], [b_rt])
            fs = r16[:, 3:5, :]
            neg = r16[:, 5:7, :]
            dS = destS[:].rearrange("p (t s) -> p s t", s=2)
            dG = destG[:].rearrange("p (t s) -> p s t", s=2)
            S.op("dve", lambda e: e.tensor_scalar(out=neg, in0=fs, scalar1=0.0, scalar2=BIGROW + 1.0, op0=ALU.is_lt, op1=ALU.mult), [b_rt], [b_R])
            S.op("dve", lambda e: e.tensor_tensor(out=neg, in0=neg, in1=fs, op=ALU.add), [], [b_R])
            S.op("dve", lambda e: e.tensor_copy(out=dS, in_=neg), [b_R], [b_rt])
            S.op("dve", lambda e: e.tensor_scalar_max(out=neg, in0=fs, scalar1=0.0), [b_rt], [b_R])
            S.op("dve", lambda e: e.tensor_copy(out=dG, in_=neg), [b_R], [b_rt])
            for t in range(NT):
                for sidx in range(2):
                    scat.append(Buf())
                    S.dma_fn("pool", lambda e: e.indirect_dma_start(
                        out=d["xbuf"][:, :], out_offset=bass.IndirectOffsetOnAxis(ap=destS[:, 2 * t + sidx:2 * t + sidx + 1], axis=0),
                        in_=u2b_all[:, t, :], in_offset=None, bounds_check=bc_reg, oob_is_err=False),
                        [b_u2b[t], b_rt] + zfill, [scat[-1]])
        S.barrier()
        if "dbg_dest" in d:
            for nm, src in (("dbg_dest", destS), ("dbg_gate", gate2)):
                outs.append(Buf())
                S.dma("sp", d[nm], src[:], reads=[b_rt], writes=[outs[-1]])
        ysc = []
        with ExitStack() as es:
            def sbn(shape, dt, n=2):
                return [cx.sb(shape, dt, es) for _ in range(n)], [Buf() for _ in range(n)]

            wg, b_wg = sbn([128, 8, DFF], BF16)
            wu, b_wu = sbn([128, 8, DFF], BF16)
            wd, b_wd = sbn([128, 4, D], BF16)
            Xe, b_Xe = sbn([128, CAP // 128, D], BF16)
            XT, b_XT = sbn([128, 8, CAP], BF16)
            hT, b_hT = sbn([128, 4, CAP], BF16)
            sg, b_sg = sbn([128, CAP], F32)
            Ye, b_Ye = sbn([128, D], F32)
            pX = cx.ps([128, 8, 128], BF16, es); b_pX = Buf()
            pg = [cx.ps([128, 512], F32, es) for _ in range(2)]
            b_pg = [Buf(), Buf()]
            pu = [cx.ps([128, 512], F32, es) for _ in range(2)]
            b_pu = [Buf(), Buf()]
            pd = [cx.ps([128, 512], F32, es) for _ in range(2)]
            b_pd = [Buf(), Buf()]
            wg_v = d["w_gate"].rearrange("e (k p) n -> e p k n", p=128)
            wu_v = d["w_up"].rearrange("e (k p) n -> e p k n", p=128)
            wd_v = d["w_down"].rearrange("e (k p) n -> e p k n", p=128)
            nst = CAP // 128
            pcnt = 0
            dcnt = 0
            for ex in range(NE):
                i = ex % 2
                S.dma("pool", wg[i][:], wg_v[ex], writes=[b_wg[i]])
                S.dma("pool", wu[i][:], wu_v[ex], writes=[b_wu[i]])
                S.dma("pool", wd[i][:], wd_v[ex], writes=[b_wd[i]])
                S.dma("sp", Xe[i][:], d["xbuf"][ex * CAP:(ex + 1) * CAP, :].rearrange("(s p) n -> p s n", p=128),
                      reads=scat, writes=[b_Xe[i]])
                for st in range(nst):
                    for k in range(8):
                        S.op("pe", lambda e: e.transpose(pX[:, k, :], Xe[i][:, st, k * 128:(k + 1) * 128], ident[:]),
                             [b_Xe[i], b_id], [b_pX])
                    if st % 2 == 0:
                        S.op("act", lambda e: e.activation(out=XT[i][:, :, st * 128:(st + 1) * 128], in_=pX[:], func=AF.Copy),
                             [], [b_pX, b_XT[i]])
                    else:
                        S.op("dve", lambda e: e.tensor_copy(out=XT[i][:, :, st * 128:(st + 1) * 128], in_=pX[:]),
                             [], [b_pX, b_XT[i]])
                for f in range(4):
                    pi = pcnt % 2
                    pcnt += 1
                    for k in range(8):
                        S.op("pe", lambda e: e.matmul(pg[pi][:, 0:CAP], lhsT=wg[i][:, k, f * 128:(f + 1) * 128], rhs=XT[i][:, k, :],
                                                      start=(k == 0), stop=(k == 7)), [b_wg[i], b_XT[i]], [b_pg[pi]])
                    for k in range(8):
                        S.op("pe", lambda e: e.matmul(pu[pi][:, 0:CAP], lhsT=wu[i][:, k, f * 128:(f + 1) * 128], rhs=XT[i][:, k, :],
                                                      start=(k == 0), stop=(k == 7)), [b_wu[i], b_XT[i]], [b_pu[pi]])
                    S.op("act", lambda e: e.activation(out=sg[pi][:], in_=pg[pi][:, 0:CAP], func=AF.Silu), [], [b_pg[pi], b_sg[pi]])
                    S.op("dve", lambda e: e.tensor_tensor(out=hT[i][:, f, :], in0=pu[pi][:, 0:CAP], in1=sg[pi][:], op=ALU.mult),
                         [b_sg[pi]], [b_pu[pi], b_hT[i]])
                for st in range(nst):
                    yi = dcnt % 2
                    dcnt += 1
                    for half in range(2):
                        hs = slice(half * 512, (half + 1) * 512)
                        for f in range(4):
                            S.op("pe", lambda e: e.matmul(pd[half][:], lhsT=hT[i][:, f, st * 128:(st + 1) * 128], rhs=wd[i][:, f, hs],
                                                          start=(f == 0), stop=(f == 3)), [b_hT[i], b_wd[i]], [b_pd[half]])
                        if half == 0:
                            S.op("act", lambda e: e.activation(out=Ye[yi][:, hs], in_=pd[half][:], func=AF.Copy),
                                 [], [b_pd[half], b_Ye[yi]])
                        else:
                            S.op("dve", lambda e: e.tensor_copy(out=Ye[yi][:, hs], in_=pd[half][:]), [], [b_pd[half], b_Ye[yi]])
                    ysc.append(Buf())
                    S.dma("act", d["ybuf"][ex * CAP + st * 128:ex * CAP + (st + 1) * 128, :], Ye[yi][:],
                          reads=[b_Ye[yi]], writes=[ysc[-1]])
            S.barrier()

        with ExitStack() as es:
            def sbn(shape, dt, n=2):
                return [cx.sb(shape, dt, es) for _ in range(n)], [Buf() for _ in range(n)]

            lnp = cx.sb([128, 2, D], F32, es); b_lnp = Buf()
            S.dma("sp", lnp[:], d["lnp"][:, 2 * D:4 * D].partition_broadcast(128).rearrange("p o (a n) -> p (o a) n", a=2), writes=[b_lnp])
            Yg, b_Yg = sbn([128, 2 * D], F32, 3)
            x1r, b_x1r = sbn([128, D], F32, 3)
            acc, b_acc = sbn([128, D], F32)
            z_, b_z_ = sbn([128, D], F32)
            xn = cx.sb([128, D], F32, es); b_xn = Buf()
            tmp = cx.sb([128, D], F32, es); b_tmp = Buf()
            xo, b_xo = sbn([128, D], F32)
            lns = LNScratch(cx, es)

            def G(t):
                i = t % 3
                S.dma("sp", x1r[i][:], d["x1s"][t * 128:(t + 1) * 128, :], reads=x1w, writes=[b_x1r[i]])
                for sidx in range(2):
                    S.dma_fn("pool", lambda e: e.indirect_dma_start(
                        out=Yg[i][:, sidx * D:(sidx + 1) * D], out_offset=None, in_=d["ybuf"][:, :],
                        in_offset=bass.IndirectOffsetOnAxis(ap=destG[:, 2 * t + sidx:2 * t + sidx + 1], axis=0),
                        bounds_check=bc_reg, oob_is_err=False), [b_rt] + ysc, [b_Yg[i]])

            def Cmb(t):
                i = t % 3
                j = t % 2
                S.op("dve", lambda e: e.tensor_scalar_mul(out=acc[j][:], in0=Yg[i][:, 0:D], scalar1=gate2[:, t, 0:1]),
                     [b_Yg[i], b_rt], [b_acc[j]])
                S.op("dve", lambda e: e.scalar_tensor_tensor(out=acc[j][:], in0=Yg[i][:, D:2 * D], scalar=gate2[:, t, 1:2],
                                                             in1=acc[j][:], op0=ALU.mult, op1=ALU.add),
                     [b_Yg[i], b_rt], [b_acc[j]])
                S.op("pool", lambda e: e.tensor_tensor(out=acc[j][:], in0=acc[j][:], in1=g2_ap, op=ALU.mult), [b_ada], [b_acc[j]])
                S.op("dve", lambda e: e.scalar_tensor_tensor(out=z_[j][:], in0=x1r[i][:], scalar=ALPHA, in1=acc[j][:],
                                                             op0=ALU.mult, op1=ALU.add), [b_x1r[i], b_acc[j]], [b_z_[j]])
                emit_ln_mod(cx, z_[j][:], b_z_[j], lns, xn, b_xn, lnp[:, 0, :], lnp[:, 1, :], b_lnp, xo[j][:], b_xo[j], tmp, b_tmp)
                outs.append(Buf())
                S.dma("sp", xout_d[t * 128:(t + 1) * 128, :], xo[j][:], reads=[b_xo[j]], writes=[outs[-1]])

            G(0)
            G(1)
            for t in range(NT):
                if t + 2 < NT:
                    G(t + 2)
                Cmb(t)
            S.barrier()
        S.barrier()
    return outs


def pack_C_inputs(inp, l, j, x_shard, yT_shard, ada_row, hc):
    lnp = np.concatenate([inp["ln_g"][l, 0], inp["ln_b"][l, 0], inp["ln_g"][l, 1], inp["ln_b"][l, 1]])[None, :]
    return {"x": x_shard, "yT": yT_shard, "ada": ada_row, "w_out": np.ascontiguousarray(inp["w_out"][l]),
            "lnp": np.ascontiguousarray(lnp), "w_router": inp["w_router"], "b_router": inp["b_router"][None, :],
            "eoff": hc["eoff"], "w_gate": np.ascontiguousarray(inp["w_gate"][l]), "w_up": np.ascontiguousarray(inp["w_up"][l]),
            "w_down": np.ascontiguousarray(inp["w_down"][l]), "ident_bf": hc["ident_bf"], "ident_f": hc["ident_f"],
            "tris_bf": hc["tris_bf"], "ones_bf": hc["ones_bf"]}


def _run(nc, in_maps):
    res = run_bass_kernel_spmd(nc, in_maps, core_ids=list(range(NCORES)))
    return res.results


def kernel_unfused(**inputs):
    inp = {k: np.asarray(v) for k, v in inputs.items()}
    hc = host_consts()
    x = np.ascontiguousarray(inp["x"][0], dtype=np.float32)
    cT = np.ascontiguousarray(inp["c"][0].reshape(8, 128).T)
    for l in range(DEPTH):
        ra = _run(build_A(), [{"x": np.ascontiguousarray(x[j * TPC:(j + 1) * TPC]), "cT": cT,
                               "w_ada": np.ascontiguousarray(inp["w_ada"][l]),
                               "b_ada": np.ascontiguousarray(inp["b_ada"][l][None, :]),
                               "ident_bf": hc["ident_bf"]} for j in range(NCORES)])
        uT = np.ascontiguousarray(np.concatenate([r["uT"] for r in ra], axis=1))
        ada = ra[0]["ada"]
        rb = _run(build_B(), [pack_B_inputs(inp, l, h, uT, hc) for h in range(NCORES)])
        yT = np.concatenate([r["yT"][0:128] for r in rb] + [r["yT"][128:256] for r in rb], axis=0)
        rc = _run(build_C(), [pack_C_inputs(inp, l, j, np.ascontiguousarray(x[j * TPC:(j + 1) * TPC]),
                                            np.ascontiguousarray(yT[:, j * TPC:(j + 1) * TPC]), ada, hc)
                              for j in range(NCORES)])
        x = np.concatenate([r["xout"] for r in rc], axis=0)
    return x[None].astype(np.float32)


def kernel(**inputs):
    return kernel_unfused(**inputs)
```

```python
import numpy as np
import ml_dtypes
from contextlib import ExitStack
import concourse.bass as bass
import concourse.mybir as mybir
from concourse.bass_utils import run_bass_kernel_spmd

F32 = mybir.dt.float32
BF16 = mybir.dt.bfloat16
I32 = mybir.dt.int32
AF = mybir.ActivationFunctionType
ALU = mybir.AluOpType
AX = mybir.AxisListType

NCORES = 8
D = 1024
SEQ = 16384
TPC = SEQ // NCORES
NT = TPC // 128
DEPTH = 2
DH = 128
NE = 32
DFF = 512
CAP = 256
ALPHA = (2 * DEPTH) ** 0.25
EPS = 1e-5
BLK = 512
NBLK = SEQ // BLK


class Buf:
    __slots__ = ("w", "r")

    def __init__(self):
        self.w = None
        self.r = {}


class Sched:
    ND = 8

    def __init__(self, nc, es):
        self.nc = nc
        self.engs = {"pe": nc.tensor, "dve": nc.vector, "act": nc.scalar, "pool": nc.gpsimd, "sp": nc.sync}
        self.semh = {}
        for e in self.engs:
            self.semh[e] = es.enter_context(nc.semaphore(f"s_{e}"))
        self.cnt = {e: 0 for e in self.engs}
        self.seen = {e: {} for e in self.engs}
        self.semh["cc"] = es.enter_context(nc.semaphore("s_cc"))
        self.ccn = 0
        self.dqn = {}
        for q in ("sp", "act", "pool"):
            self.dqn[q] = 0
            for i in range(self.ND):
                self.semh[(q, i)] = es.enter_context(nc.semaphore(f"d_{q}{i}"))

    def _wait(self, e, key, val):
        if self.seen[e].get(key, 0) >= val:
            return
        self.engs[e].wait_ge(self.semh[key], val)
        self.seen[e][key] = val

    def _deps(self, e, reads, writes):
        deps = {}
        for b in reads:
            if b.w is not None:
                k, v = b.w
                if deps.get(k, 0) < v:
                    deps[k] = v
        for b in writes:
            if b.w is not None:
                k, v = b.w
                if deps.get(k, 0) < v:
                    deps[k] = v
            for k, v in b.r.items():
                if deps.get(k, 0) < v:
                    deps[k] = v
        for k, v in deps.items():
            if e == "pe" and k == "pe":
                continue
            self._wait(e, k, v)

    def _commit(self, tok, reads, writes):
        k, v = tok
        for b in reads:
            if b.r.get(k, 0) < v:
                b.r[k] = v
        for b in writes:
            b.w = tok
            b.r = {}

    def op(self, e, fn, reads=(), writes=()):
        self._deps(e, reads, writes)
        inst = fn(self.engs[e])
        self.cnt[e] += 1
        inst.then_inc(self.semh[e], 1)
        self._commit((e, self.cnt[e]), reads, writes)
        return inst

    def dma(self, q, out, in_, reads=(), writes=(), **kw):
        return self.dma_fn(q, lambda e: e.dma_start(out=out, in_=in_, **kw), reads, writes)

    def dma_fn(self, q, fn, reads=(), writes=()):
        n = self.dqn[q]
        i = n % self.ND
        rnd = n // self.ND
        self.dqn[q] = n + 1
        key = (q, i)
        if rnd > 0:
            self._wait(q, key, 16 * rnd)
        self._deps(q, reads, writes)
        inst = fn(self.engs[q])
        inst.then_inc(self.semh[key], 16)
        self._commit((key, 16 * (rnd + 1)), reads, writes)
        return inst

    def cc(self, fn, reads=(), writes=()):
        self._deps("pool", reads, writes)
        inst = fn(self.engs["pool"])
        self.ccn += 1
        inst.then_inc(self.semh["cc"], 1)
        self._commit(("cc", self.ccn), reads, writes)
        return inst

    def finish(self, bufs, e="sp"):
        self._deps(e, bufs, bufs)

    def barrier(self):
        for e in self.engs:
            for f in self.engs:
                if f != e and self.cnt[f] > 0:
                    self._wait(e, f, self.cnt[f])
            if self.ccn > 0:
                self._wait(e, "cc", self.ccn)
            for q in ("sp", "act", "pool"):
                n = self.dqn[q]
                for i in range(self.ND):
                    c = (n - i + self.ND - 1) // self.ND if n > i else 0
                    if c > 0:
                        self._wait(e, (q, i), 16 * c)


class Ctx:
    def __init__(self, nc, es):
        self.nc = nc
        self.es = es
        self.S = Sched(nc, es)
        self._n = 0

    def sb(self, shape, dt, es=None, name=None):
        self._n += 1
        return (es or self.es).enter_context(self.nc.sbuf_tensor(name or f"sb{self._n}", list(shape), dt))

    def ps(self, shape, dt, es=None, name=None):
        self._n += 1
        return (es or self.es).enter_context(self.nc.psum_tensor(name or f"ps{self._n}", list(shape), dt))

    def dram_in(self, name, shape, dt):
        return self.nc.dram_tensor(name, list(shape), dt, kind="ExternalInput").ap()

    def dram_out(self, name, shape, dt):
        return self.nc.dram_tensor(name, list(shape), dt, kind="ExternalOutput").ap()


def host_consts():
    c = {}
    c["ident_bf"] = np.eye(128, dtype=np.float32).astype(ml_dtypes.bfloat16)
    c["ident_f"] = np.eye(128, dtype=np.float32)
    r = np.arange(128)
    c["tri_f"] = (r[:, None] <= r[None, :]).astype(np.float32)
    c["ones_f"] = np.ones((128, 128), np.float32)
    c["ones_bf"] = np.ones((128, 128), np.float32).astype(ml_dtypes.bfloat16)
    c["tris_bf"] = (r[:, None] < r[None, :]).astype(np.float32).astype(ml_dtypes.bfloat16)
    c["eoff"] = (np.arange(NE, dtype=np.float32) * CAP)[None, :]
    return c


def emit_ada(cx, cT_d, w_ada_d, b_ada_d, ada_bc, b_ada_bc):
    S = cx.S
    with ExitStack() as es:
        cT = cx.sb([128, 8], F32, es)
        b_cT = Buf()
        cond = cx.sb([128, 8], F32, es)
        b_cond = Buf()
        condB = cx.sb([128, 8, 128], F32, es)
        b_condB = Buf()
        bias = cx.sb([128, 6 * D], F32, es)
        b_bias = Buf()
        wbuf = [cx.sb([128, 8, 512], F32, es) for _ in range(2)]
        b_w = [Buf(), Buf()]
        pp = [cx.ps([128, 512], F32, es) for _ in range(2)]
        b_pp = [Buf(), Buf()]
        S.dma("sp", cT[:], cT_d, writes=[b_cT])
        S.dma("sp", bias[:], b_ada_d.partition_broadcast(128), writes=[b_bias])
        S.op("act", lambda e: e.activation(out=cond[:], in_=cT[:], func=AF.Silu), [b_cT], [b_cond])
        for k in range(8):
            S.op("dve", lambda e: e.tensor_copy(out=condB[:, k, :], in_=cond[:, k:k + 1].to_broadcast([128, 128])),
                 [b_cond], [b_condB])
        wv = w_ada_d.rearrange("(k p) n -> p k n", p=128)
        for j in range(12):
            w = wbuf[j % 2]
            S.dma("sp" if j % 2 == 0 else "act", w[:], wv[:, :, j * 512:(j + 1) * 512], writes=[b_w[j % 2]])
            p = pp[j % 2]
            for k in range(8):
                S.op("pe", lambda e: e.matmul(p[:], lhsT=condB[:, k, :], rhs=w[:, k, :], start=(k == 0), stop=(k == 7)),
                     [b_condB, b_w[j % 2]], [b_pp[j % 2]])
            S.op("dve", lambda e: e.tensor_tensor(out=ada_bc[:, j * 512:(j + 1) * 512], in0=p[:],
                                                  in1=bias[:, j * 512:(j + 1) * 512], op=ALU.add),
                 [b_pp[j % 2], b_bias], [b_ada_bc])
        S.barrier()


def emit_ln_stats(cx, x_ap, b_x, st, mv, rs, nb, b_st):
    S = cx.S
    S.op("dve", lambda e: e.bn_stats(out=st[:, 0:6], in_=x_ap[:, 0:512]), [b_x], [b_st])
    S.op("dve", lambda e: e.bn_stats(out=st[:, 6:12], in_=x_ap[:, 512:1024]), [b_x], [b_st])
    S.op("dve", lambda e: e.bn_aggr(out=mv[:], in_=st[:]), [b_st], [b_st])
    S.op("act", lambda e: e.activation(out=rs[:], in_=mv[:, 1:2], func=AF.Sqrt, bias=EPS, scale=1.0), [b_st], [b_st])
    S.op("dve", lambda e: e.reciprocal(out=rs[:], in_=rs[:]), [b_st], [b_st])
    S.op("dve", lambda e: e.scalar_tensor_tensor(out=nb[:], in0=mv[:, 0:1], scalar=-1.0, in1=rs[:],
                                                 op0=ALU.mult, op1=ALU.mult), [b_st], [b_st])


class LNScratch:
    def __init__(self, cx, es):
        self.st = cx.sb([128, 12], F32, es)
        self.mv = cx.sb([128, 2], F32, es)
        self.rs = cx.sb([128, 1], F32, es)
        self.nb = cx.sb([128, 1], F32, es)
        self.b = Buf()


def emit_ln_mod(cx, x_ap, b_x, lns, xn, b_xn, A_ap, B_ap, b_ab, out_ap, b_out, tmp, b_tmp):
    S = cx.S
    emit_ln_stats(cx, x_ap, b_x, lns.st, lns.mv, lns.rs, lns.nb, lns.b)
    S.op("act", lambda e: e.activation(out=xn[:], in_=x_ap, func=AF.Identity, bias=lns.nb[:], scale=lns.rs[:]),
         [b_x, lns.b], [b_xn])
    S.op("dve", lambda e: e.tensor_tensor(out=tmp[:], in0=xn[:], in1=A_ap, op=ALU.mult), [b_xn, b_ab], [b_tmp])
    S.op("pool", lambda e: e.tensor_tensor(out=out_ap, in0=tmp[:], in1=B_ap, op=ALU.add), [b_tmp, b_ab], [b_out])


def build_A():
    nc = bass.Bass("TRN2", target_bir_lowering=False)
    with ExitStack() as es:
        cx = Ctx(nc, es)
        S = cx.S
        x_d = cx.dram_in("x", [TPC, D], F32)
        cT_d = cx.dram_in("cT", [128, 8], F32)
        w_ada_d = cx.dram_in("w_ada", [D, 6 * D], F32)
        b_ada_d = cx.dram_in("b_ada", [1, 6 * D], F32)
        ident_d = cx.dram_in("ident_bf", [128, 128], BF16)
        uT_d = cx.dram_out("uT", [D, TPC], BF16)
        ada_d = cx.dram_out("ada", [1, 6 * D], F32)

        ada = cx.sb([128, 6 * D], F32)
        b_ada = Buf()
        emit_ada(cx, cT_d, w_ada_d, b_ada_d, ada, b_ada)
        b_adaout = Buf()
        S.dma("sp", ada_d, ada[0:1, :], reads=[b_ada], writes=[b_adaout])
        S.finish([b_adaout], "sp")
        ident = cx.sb([128, 128], BF16)
        b_id = Buf()
        S.dma("sp", ident[:], ident_d, writes=[b_id])
        S.op("dve", lambda e: e.tensor_scalar_add(out=ada[:, D:2 * D], in0=ada[:, D:2 * D], scalar1=1.0), [b_ada], [b_ada])
        emit_A_body(cx, x_d, None, None, ada, b_ada, ident, b_id, uT_d)
    return nc


def emit_A_body(cx, x_d, xres, b_xres, ada, b_ada, ident, b_id, uT_d, sh_off=0, sc_off=D):
    S = cx.S
    with ExitStack() as es:
        lns = LNScratch(cx, es)
        xin = [cx.sb([128, D], F32, es) for _ in range(2)]
        b_xin = [Buf(), Buf()]
        xn = cx.sb([128, D], F32, es)
        b_xn = Buf()
        tmp = cx.sb([128, D], F32, es)
        b_tmp = Buf()
        ub = [cx.sb([128, D], BF16, es) for _ in range(2)]
        b_ub = [Buf(), Buf()]
        pT = [cx.ps([128, 8, 128], BF16, es) for _ in range(2)]
        b_pT = [Buf(), Buf()]
        uTs = [cx.sb([128, 8, 128], BF16, es) for _ in range(2)]
        b_uTs = [Buf(), Buf()]
        outs = []
        uT_v = uT_d.rearrange("(k p) t -> p k t", p=128)
        for t in range(NT):
            i = t % 2
            if x_d is not None:
                S.dma("sp", xin[i][:], x_d[t * 128:(t + 1) * 128, :], writes=[b_xin[i]])
                x_ap, bx = xin[i][:], b_xin[i]
            else:
                x_ap, bx = xres[:, t, :], b_xres[t]
            emit_ln_mod(cx, x_ap, bx, lns, xn, b_xn, ada[:, sc_off:sc_off + D], ada[:, sh_off:sh_off + D], b_ada,
                        ub[i][:], b_ub[i], tmp, b_tmp)
            for k in range(8):
                S.op("pe", lambda e: e.transpose(pT[i][:, k, :], ub[i][:, k * 128:(k + 1) * 128], ident[:]),
                     [b_ub[i], b_id], [b_pT[i]])
            S.op("act", lambda e: e.activation(out=uTs[i][:], in_=pT[i][:], func=AF.Copy), [b_pT[i]], [b_uTs[i]])
            outs.append(Buf())
            S.dma("sp", uT_v[:, :, t * 128:(t + 1) * 128], uTs[i][:], reads=[b_uTs[i]], writes=[outs[-1]])
        S.finish(outs, "sp")
        S.barrier()


LN_INV_SQRT_DH = float(-0.5 * np.log(DH))
GELU_C = 1.5957691216057308


def build_B():
    nc = bass.Bass("TRN2", target_bir_lowering=False)
    with ExitStack() as es:
        cx = Ctx(nc, es)
        d = {
            "uT": cx.dram_in("uT", [D, SEQ], BF16),
            "wh": cx.dram_in("wh", [D, 770], F32),
            "pp": cx.dram_in("pp", [128, 24], F32),
            "fv": cx.dram_in("fv", [1, 258], F32),
            "wa": cx.dram_in("wa", [128, 128], F32),
            "wx": cx.dram_in("wx", [128, 128], F32),
            "ident_bf": cx.dram_in("ident_bf", [128, 128], BF16),
            "tri_f": cx.dram_in("tri_f", [128, 128], F32),
            "ones_f": cx.dram_in("ones_f", [128, 128], F32),
            "ones_bf": cx.dram_in("ones_bf", [128, 128], BF16),
            "yT": cx.dram_out("yT", [256, SEQ], BF16),
        }
        outs = emit_B(cx, d)
        cx.S.finish(outs, "sp")
    return nc


def emit_B(cx, d, nblk=NBLK):
    S = cx.S
    outs = []
    NCH = BLK // 128
    with ExitStack() as es:
        def sb(shape, dt):
            return cx.sb(shape, dt, es)

        def sbn(shape, dt, n=2):
            return [cx.sb(shape, dt, es) for _ in range(n)], [Buf() for _ in range(n)]

        W = sb([128, 8, 770], BF16); b_W = Buf()
        S.dma("pool", W[:], d["wh"].rearrange("(k p) n -> p k n", p=128), writes=[b_W])
        wa = sb([128, 128], BF16); b_wa = Buf()
        S.dma("pool", wa[:], d["wa"], writes=[b_wa])
        wx = sb([128, 128], BF16); b_wx = Buf()
        S.dma("pool", wx[:], d["wx"], writes=[b_wx])
        pp = sb([128, 24], F32); b_pp = Buf()
        S.dma("sp", pp[:], d["pp"], writes=[b_pp])
        fv = sb([128, 258], F32); b_fv = Buf()
        S.dma("sp", fv[:], d["fv"].partition_broadcast(128), writes=[b_fv])
        ident = sb([128, 128], BF16); b_id = Buf()
        S.dma("sp", ident[:], d["ident_bf"], writes=[b_id])
        tri = sb([128, 128], F32); b_tri = Buf()
        S.dma("sp", tri[:], d["tri_f"], writes=[b_tri])
        ones_f = sb([128, 128], F32); b_of = Buf()
        S.dma("sp", ones_f[:], d["ones_f"], writes=[b_of])
        ones_bf = sb([128, 128], BF16); b_ob = Buf()
        S.dma("sp", ones_bf[:], d["ones_bf"], writes=[b_ob])
        der = sb([128, 4], F32); b_der = Buf()
        S.op("act", lambda e: e.activation(out=der[:, 3:4], in_=pp[:, 21:22], func=AF.Exp, scale=-1.0), [b_pp], [b_der])
        S.op("act", lambda e: e.activation(out=der[:, 3:4], in_=der[:, 3:4], func=AF.Ln, bias=1.0, scale=1.0), [b_der], [b_der])
        S.op("dve", lambda e: e.tensor_scalar_mul(out=der[:, 0:1], in0=der[:, 3:4], scalar1=-8.0), [b_der], [b_der])
        S.op("dve", lambda e: e.tensor_scalar_mul(out=der[:, 1:2], in0=der[:, 3:4], scalar1=-16.0), [b_der], [b_der])
        S.op("dve", lambda e: e.tensor_scalar_mul(out=der[:, 2:3], in0=pp[:, 22:23], scalar1=float(np.sqrt(128.0))),
             [b_pp, b_der], [b_der])

        dg = {}
        b_dg = Buf()
        for nm, wc in (("q", 0), ("k", 4), ("x", 8)):
            dg[nm] = sb([128, 4, 128], BF16)
            for k in range(4):
                S.op("dve", lambda e: e.tensor_scalar_mul(out=dg[nm][:, k, :], in0=ident[:], scalar1=pp[:, wc + k:wc + k + 1]),
                     [b_id, b_pp], [b_dg])
        C32 = sb([128, 129], F32); b_C32 = Buf()
        Cbf = sb([128, 129], BF16); b_Cbf = Buf()
        S.op("dve", lambda e: e.memset(C32[:], 0.0), [], [b_C32])
        S.op("dve", lambda e: e.memset(Cbf[:], 0.0), [], [b_Cbf])
        hz = sb([128, 1], F32); b_hz = Buf()
        S.op("dve", lambda e: e.memset(hz[:], 0.0), [], [b_hz])

        uTb, b_uTb = sbn([128, 8, BLK], BF16)
        pre = {}
        b_pre = {}
        for nm in ("q", "k", "x"):
            pre[nm], b_pre[nm] = sbn([128, BLK + 8], BF16)
            for i in range(2):
                S.op("pool", lambda e: e.memset(pre[nm][i][:, 0:3], 0.0), [], [b_pre[nm][i]])
        gpre, b_gpre = sbn([128, BLK], F32)
        acc = {}
        b_acc = {}
        for nm in ("x",):
            acc[nm], b_acc[nm] = sbn([128, BLK], F32)
        qT, b_qT = sbn([128, BLK], BF16)
        kT, b_kT = sbn([128, BLK], BF16)
        vo, b_vo = sbn([128, NCH, 256], F32)
        iff, b_if = sbn([128, NCH, 2], F32)
        sm, b_sm = sbn([128, 64], F32)
        vaug, b_vaug = sbn([128, NCH, 144], BF16)
        kc, b_kc = sbn([128, NCH, 128], BF16)
        PT, b_PT = sbn([128, NCH, 128], BF16)
        hm, b_hm = sbn([128, NCH, 128], F32)
        hq, b_hq = sbn([128, NCH, 128], F32)
        so, b_so = sbn([128, NCH, 128], F32)
        ym, b_ym = sbn([128, NCH, 128], BF16)
        tC, b_tC = sbn([128, 129], F32)
        ymT, b_ymT = sbn([128, BLK], BF16)
        yrT, b_yrT = sbn([128, BLK], BF16)
        xcb, b_xcb = sbn([128, BLK], BF16)
        names = ["r", "ig", "a", "a2", "xi", "h", "g2", "sg", "gl", "yr", "sd", "yn"]
        L = {}
        bL = {}
        for nm in names:
            L[nm], bL[nm] = sbn([128, BLK], F32)
        ysq, b_ysq = sbn([128, BLK], BF16)

        pfm = [cx.ps([128, 512], F32, es) for _ in range(2)]
        b_pfm = [Buf(), Buf()]
        pvo = [cx.ps([128, 2, 256], F32, es) for _ in range(2)]
        b_pvo = [Buf(), Buf()]
        pTr = cx.ps([128, 512], F32, es); b_pTr = Buf()
        psm = cx.ps([128, 512], F32, es); b_psm = Buf()
        pST = cx.ps([128, NCH, 128], F32, es); b_pST = Buf()
        pnum = cx.ps([128, NCH, 128], F32, es); b_pnum = Buf()
        p_kc = pTr[:, 0:256].bitcast(BF16).rearrange("p (c n) -> p c n", c=NCH)
        p_ym = pTr[:, 256:512].bitcast(BF16).rearrange("p (c n) -> p c n", c=NCH)
        p_cs = psm[:, 0:8]
        p_if = psm[:, 8:16].rearrange("p (c n) -> p c n", c=NCH)
        p_den = psm[:, 16:20]
        p_upd = psm[:, 32:161]
        fmstate = {"n": 0}

        def fm_bank():
            n = fmstate["n"] % 2
            fmstate["n"] += 1
            return pfm[n], b_pfm[n]

        uT_v = d["uT"].rearrange("(k p) t -> p k t", p=128)

        def proj_pieces(blk):
            i = blk % 2
            t0 = blk * BLK
            pieces = []

            def p_load():
                S.dma("sp", uTb[i][:], uT_v[:, :, t0:t0 + BLK], writes=[b_uTb[i]])
                if blk > 0:
                    for nm in ("q", "k", "x"):
                        S.op("pool", lambda e: e.tensor_copy(out=pre[nm][i][:, 0:3], in_=pre[nm][1 - i][:, BLK:BLK + 3]),
                             [b_pre[nm][1 - i]], [b_pre[nm][i]])

            def p_fm(gi, nm):
                def f():
                    p, bp = fm_bank()
                    for k in range(8):
                        S.op("pe", lambda e: e.matmul(p[:], lhsT=W[:, k, gi * 128:(gi + 1) * 128], rhs=uTb[i][:, k, :],
                                                      start=(k == 0), stop=(k == 7)), [b_W, b_uTb[i]], [bp])
                    if nm == "g":
                        S.op("act", lambda e: e.activation(out=gpre[i][:], in_=p[:], func=AF.Identity,
                                                           bias=pp[:, 18:19], scale=1.0), [b_pp], [bp, b_gpre[i]])
                    else:
                        S.op("act", lambda e: e.activation(out=pre[nm][i][:, 3:BLK + 3], in_=p[:], func=AF.Identity,
                                                           bias=pp[:, 15 + gi:16 + gi], scale=1.0), [b_pp], [bp, b_pre[nm][i]])
                return f

            def p_vo(cp):
                def f():
                    for cc in range(2):
                        c = cp * 2 + cc
                        for k in range(8):
                            S.op("pe", lambda e: e.matmul(pvo[cp][:, cc, :], lhsT=uTb[i][:, k, c * 128:(c + 1) * 128],
                                                          rhs=W[:, k, 512:768], start=(k == 0), stop=(k == 7)),
                                 [b_W, b_uTb[i]], [b_pvo[cp]])
                    S.op("dve", lambda e: e.tensor_tensor(out=vo[i][:, cp * 2:cp * 2 + 2, :], in0=pvo[cp][:],
                                                          in1=fv[:, 0:256].unsqueeze(1).to_broadcast([128, 2, 256]), op=ALU.add),
                         [b_fv], [b_pvo[cp], b_vo[i]])
                return f

            def p_ifproj():
                for c in range(NCH):
                    for k in range(8):
                        S.op("pe", lambda e: e.matmul(p_if[:, c, :], lhsT=uTb[i][:, k, c * 128:(c + 1) * 128],
                                                      rhs=W[:, k, 768:770], start=(k == 0), stop=(k == 7)),
                             [b_W, b_uTb[i]], [b_psm])
                S.op("dve", lambda e: e.tensor_tensor(out=iff[i][:], in0=p_if, in1=fv[:, 256:258].unsqueeze(1).to_broadcast([128, NCH, 2]),
                                                      op=ALU.add), [b_fv], [b_psm, b_if[i]])

            def p_conv(nm, bc):
                def f():
                    p, bp = fm_bank()
                    for k in range(4):
                        S.op("pe", lambda e: e.matmul(p[:], lhsT=dg[nm][:, k, :], rhs=pre[nm][i][:, k:k + BLK],
                                                      start=(k == 0), stop=(k == 3)), [b_dg, b_pre[nm][i]], [bp])
                    if nm == "q":
                        S.op("act", lambda e: e.activation(out=qT[i][:], in_=p[:], func=AF.Silu, bias=pp[:, bc:bc + 1], scale=1.0),
                             [b_pp], [bp, b_qT[i]])
                    elif nm == "k":
                        S.op("act", lambda e: e.activation(out=kT[i][:], in_=p[:], func=AF.Silu, bias=pp[:, bc:bc + 1], scale=1.0),
                             [b_pp], [bp, b_kT[i]])
                    else:
                        S.op("act", lambda e: e.activation(out=acc["x"][i][:], in_=p[:], func=AF.Identity, bias=pp[:, bc:bc + 1], scale=1.0),
                             [b_pp], [bp, b_acc["x"][i]])
                return f

            pieces = [p_load, p_ifproj, p_fm(0, "q"), p_fm(1, "k"), p_fm(2, "x"), p_fm(3, "g"), p_vo(0), p_vo(1),
                      p_conv("q", 12), p_conv("k", 13), p_conv("x", 14)]
            return pieces

        def emit_compute(blk, pieces):
            def pump(n):
                for _ in range(n):
                    if pieces:
                        pieces.pop(0)()
            i = blk % 2
            t0 = blk * BLK
            s_ = sm[i]; bs = b_sm[i]
            bc4 = lambda ap: ap.unsqueeze(2).to_broadcast([128, NCH, 128])
            S.op("act", lambda e: e.activation(out=s_[:, 0:4], in_=iff[i][:, :, 1], func=AF.Exp, scale=-1.0), [b_if[i]], [bs])
            S.op("act", lambda e: e.activation(out=s_[:, 4:8], in_=s_[:, 0:4], func=AF.Ln, bias=1.0, scale=1.0), [bs], [bs])
            S.op("pe", lambda e: e.matmul(p_cs[:, 0:4], lhsT=tri[:], rhs=s_[:, 4:8], start=True, stop=True), [b_tri, bs], [b_psm])
            S.op("pe", lambda e: e.matmul(p_cs[:, 4:8], lhsT=ones_f[:], rhs=s_[:, 4:8], start=True, stop=True), [b_of, bs], [b_psm])
            S.op("dve", lambda e: e.tensor_tensor(out=s_[:, 8:12], in0=iff[i][:, :, 0], in1=p_cs[:, 0:4], op=ALU.add),
                 [b_if[i]], [b_psm, bs])
            S.op("act", lambda e: e.activation(out=s_[:, 12:16], in_=s_[:, 8:12], func=AF.Exp, bias=LN_INV_SQRT_DH, scale=1.0), [bs], [bs])
            S.op("act", lambda e: e.activation(out=s_[:, 16:24], in_=p_cs, func=AF.Exp, scale=-1.0), [], [b_psm, bs])
            pump(3)
            ws = s_[:, 12:16]
            eb = s_[:, 16:20]
            S.op("dve", lambda e: e.tensor_tensor(out=vaug[i][:, :, 0:128], in0=vo[i][:, :, 0:128], in1=bc4(ws), op=ALU.mult),
                 [b_vo[i], bs], [b_vaug[i]])
            S.op("dve", lambda e: e.tensor_copy(out=vaug[i][:, :, 128], in_=ws), [bs], [b_vaug[i]])
            for c in range(NCH):
                S.op("pe", lambda e: e.transpose(p_kc[:, c, :], kT[i][:, c * 128:(c + 1) * 128], ident[:]), [b_kT[i], b_id], [b_pTr])
            S.op("act", lambda e: e.activation(out=kc[i][:], in_=p_kc, func=AF.Copy), [], [b_pTr, b_kc[i]])
            for c in range(NCH):
                cs = slice(c * 128, (c + 1) * 128)
                S.op("pe", lambda e: e.matmul(pST[:, c, :], lhsT=kT[i][:, cs], rhs=qT[i][:, cs], start=True, stop=True),
                     [b_kT[i], b_qT[i]], [b_pST])
            S.op("dve", lambda e: e.tensor_tensor(out=PT[i][:], in0=pST[:], in1=tri[:].unsqueeze(1).to_broadcast([128, NCH, 128]),
                                                  op=ALU.mult), [b_tri], [b_pST, b_PT[i]])
            for c in range(NCH):
                pump(1)
                cs = slice(c * 128, (c + 1) * 128)
                S.op("pe", lambda e: e.matmul(pnum[:, c, :], lhsT=PT[i][:, c, :], rhs=vaug[i][:, c, 0:128], start=True, stop=False),
                     [b_PT[i], b_vaug[i]], [b_pnum])
                S.op("pe", lambda e: e.matmul(pnum[:, c, :], lhsT=qT[i][:, cs], rhs=Cbf[:, 0:128], start=False, stop=True),
                     [b_qT[i], b_Cbf], [b_pnum])
                S.op("pe", lambda e: e.matmul(p_den[:, c:c + 1], lhsT=PT[i][:, c, :], rhs=vaug[i][:, c, 128:129], start=True, stop=False),
                     [b_PT[i], b_vaug[i]], [b_psm])
                S.op("pe", lambda e: e.matmul(p_den[:, c:c + 1], lhsT=qT[i][:, cs], rhs=Cbf[:, 128:129], start=False, stop=True),
                     [b_qT[i], b_Cbf], [b_psm])
                S.op("pe", lambda e: e.matmul(p_upd, lhsT=kc[i][:, c, :], rhs=vaug[i][:, c, 0:129], start=True, stop=True),
                     [b_kc[i], b_vaug[i]], [b_psm])
                j = c % 2
                S.op("dve", lambda e: e.tensor_tensor(out=tC[j][:], in0=p_upd, in1=C32[:], op=ALU.add), [b_C32], [b_psm, b_tC[j]])
                S.op("dve", lambda e: e.tensor_tensor(out=C32[:], in0=tC[j][:], in1=s_[:, 20 + c:21 + c].to_broadcast([128, 129]), op=ALU.mult),
                     [b_tC[j], bs], [b_C32])
                S.op("act", lambda e: e.activation(out=Cbf[:], in_=tC[j][:], func=AF.Copy, scale=s_[:, 20 + c:21 + c]),
                     [b_tC[j], bs], [b_Cbf])
            pump(1)
            S.op("dve", lambda e: e.tensor_tensor(out=s_[:, 24:28], in0=p_den, in1=eb, op=ALU.mult), [], [b_psm, bs])
            S.op("act", lambda e: e.activation(out=s_[:, 24:28], in_=s_[:, 24:28], func=AF.Abs), [bs], [bs])
            S.op("dve", lambda e: e.tensor_scalar_max(out=s_[:, 24:28], in0=s_[:, 24:28], scalar1=1.0), [bs], [bs])
            S.op("dve", lambda e: e.reciprocal(out=s_[:, 28:32], in_=s_[:, 24:28]), [bs], [bs])
            S.op("dve", lambda e: e.tensor_tensor(out=s_[:, 28:32], in0=s_[:, 28:32], in1=eb, op=ALU.mult), [bs], [bs])
            S.op("dve", lambda e: e.tensor_tensor(out=hm[i][:], in0=pnum[:], in1=bc4(s_[:, 28:32]), op=ALU.mult), [bs], [b_pnum, b_hm[i]])
            S.op("dve", lambda e: e.tensor_reduce(out=s_[:, 32:36], in_=hm[i][:], axis=AX.X, op=ALU.add), [b_hm[i]], [bs])
            S.op("act", lambda e: e.activation(out=hq[i][:], in_=hm[i][:], func=AF.Square), [b_hm[i]], [b_hq[i]])
            S.op("dve", lambda e: e.tensor_reduce(out=s_[:, 36:40], in_=hq[i][:], axis=AX.X, op=ALU.add), [b_hq[i]], [bs])
            S.op("dve", lambda e: e.tensor_scalar_mul(out=s_[:, 32:36], in0=s_[:, 32:36], scalar1=1.0 / 128.0), [bs], [bs])
            S.op("dve", lambda e: e.tensor_tensor(out=s_[:, 40:44], in0=s_[:, 32:36], in1=s_[:, 32:36], op=ALU.mult), [bs], [bs])
            S.op("dve", lambda e: e.scalar_tensor_tensor(out=s_[:, 36:40], in0=s_[:, 36:40], scalar=1.0 / 128.0, in1=s_[:, 40:44],
                                                         op0=ALU.mult, op1=ALU.subtract), [bs], [bs])
            S.op("act", lambda e: e.activation(out=s_[:, 36:40], in_=s_[:, 36:40], func=AF.Ln, bias=EPS, scale=1.0), [bs], [bs])
            S.op("act", lambda e: e.activation(out=s_[:, 36:40], in_=s_[:, 36:40], func=AF.Exp, scale=-0.5), [bs], [bs])
            S.op("dve", lambda e: e.tensor_tensor(out=hm[i][:], in0=hm[i][:], in1=bc4(s_[:, 32:36]), op=ALU.subtract), [bs], [b_hm[i]])
            S.op("dve", lambda e: e.tensor_tensor(out=hm[i][:], in0=hm[i][:], in1=bc4(s_[:, 36:40]), op=ALU.mult), [bs], [b_hm[i]])
            S.op("act", lambda e: e.activation(out=so[i][:], in_=vo[i][:, :, 128:256], func=AF.Sigmoid), [b_vo[i]], [b_so[i]])
            S.op("dve", lambda e: e.tensor_tensor(out=ym[i][:], in0=hm[i][:], in1=so[i][:], op=ALU.mult), [b_hm[i], b_so[i]], [b_ym[i]])
            pump(1)
            for c in range(NCH):
                S.op("pe", lambda e: e.transpose(p_ym[:, c, :], ym[i][:, c, :], ident[:]), [b_ym[i], b_id], [b_pTr])
            S.op("act", lambda e: e.activation(out=ymT[i][:].rearrange("p (c n) -> p c n", c=NCH), in_=p_ym, func=AF.Copy,
                                               scale=pp[:, 23:24]), [b_pp], [b_pTr, b_ymT[i]])
            outs.append(Buf())
            S.dma("sp", d["yT"][0:128, t0:t0 + BLK], ymT[i][:], reads=[b_ymT[i]], writes=[outs[-1]])

            xc = acc["x"][i]; b_xc = b_acc["x"][i]
            S.op("act", lambda e: e.activation(out=xcb[i][:], in_=xc[:], func=AF.Copy), [b_xc], [b_xcb[i]])
            p, bp = fm_bank()
            S.op("pe", lambda e: e.matmul(p[:], lhsT=wa[:], rhs=xcb[i][:], start=True, stop=True), [b_wa, b_xcb[i]], [bp])
            S.op("act", lambda e: e.activation(out=L["r"][i][:], in_=p[:], func=AF.Sigmoid, bias=pp[:, 19:20], scale=1.0),
                 [b_pp], [bp, bL["r"][i]])
            p, bp = fm_bank()
            S.op("pe", lambda e: e.matmul(p[:], lhsT=wx[:], rhs=xcb[i][:], start=True, stop=True), [b_wx, b_xcb[i]], [bp])
            S.op("act", lambda e: e.activation(out=L["ig"][i][:], in_=p[:], func=AF.Sigmoid, bias=pp[:, 20:21], scale=1.0),
                 [b_pp], [bp, bL["ig"][i]])
            g = gpre[i]; bg = b_gpre[i]
            S.op("pool", lambda e: e.tensor_tensor(out=L["g2"][i][:], in0=g[:], in1=g[:], op=ALU.mult), [bg], [bL["g2"][i]])
            S.op("pool", lambda e: e.tensor_scalar(out=L["g2"][i][:], in0=L["g2"][i][:], scalar1=0.044715, scalar2=1.0,
                                                   op0=ALU.mult, op1=ALU.add), [], [bL["g2"][i]])
            S.op("pool", lambda e: e.tensor_tensor(out=L["g2"][i][:], in0=L["g2"][i][:], in1=g[:], op=ALU.mult), [bg], [bL["g2"][i]])
            S.op("act", lambda e: e.activation(out=L["sg"][i][:], in_=L["g2"][i][:], func=AF.Sigmoid, scale=GELU_C),
                 [bL["g2"][i]], [bL["sg"][i]])
            S.op("act", lambda e: e.activation(out=L["a"][i][:], in_=L["r"][i][:], func=AF.Exp, scale=der[:, 0:1]),
                 [bL["r"][i], b_der], [bL["a"][i]])
            S.op("act", lambda e: e.activation(out=L["a2"][i][:], in_=L["r"][i][:], func=AF.Exp, scale=der[:, 1:2]),
                 [bL["r"][i], b_der], [bL["a2"][i]])
            S.op("act", lambda e: e.activation(out=L["a2"][i][:], in_=L["a2"][i][:], func=AF.Ln, bias=1.0, scale=-1.0),
                 [bL["a2"][i]], [bL["a2"][i]])
            S.op("act", lambda e: e.activation(out=L["a2"][i][:], in_=L["a2"][i][:], func=AF.Exp, scale=0.5),
                 [bL["a2"][i]], [bL["a2"][i]])
            S.op("pool", lambda e: e.tensor_tensor(out=L["xi"][i][:], in0=L["ig"][i][:], in1=xc[:], op=ALU.mult),
                 [bL["ig"][i], b_xc], [bL["xi"][i]])
            S.op("dve", lambda e: e.tensor_tensor(out=L["xi"][i][:], in0=L["xi"][i][:], in1=L["a2"][i][:], op=ALU.mult),
                 [bL["a2"][i]], [bL["xi"][i]])
            init = hz[:, 0:1] if blk == 0 else L["h"][1 - i][:, BLK - 1:BLK]
            b_init = b_hz if blk == 0 else bL["h"][1 - i]
            S.op("dve", lambda e: e.tensor_tensor_scan(out=L["h"][i][:], data0=L["a"][i][:], data1=L["xi"][i][:], initial=init,
                                                       op0=ALU.mult, op1=ALU.add),
                 [bL["a"][i], bL["xi"][i], b_init], [bL["h"][i]])
            g = gpre[i]; bg = b_gpre[i]
            S.op("pool", lambda e: e.tensor_tensor(out=L["gl"][i][:], in0=L["sg"][i][:], in1=g[:], op=ALU.mult),
                 [bL["sg"][i], bg], [bL["gl"][i]])
            S.op("dve", lambda e: e.tensor_tensor(out=L["yr"][i][:], in0=L["h"][i][:], in1=L["gl"][i][:], op=ALU.mult),
                 [bL["h"][i], bL["gl"][i]], [bL["yr"][i]])
            S.op("act", lambda e: e.activation(out=ysq[i][:], in_=L["yr"][i][:], func=AF.Square), [bL["yr"][i]], [b_ysq[i]])
            p, bp = fm_bank()
            S.op("pe", lambda e: e.matmul(p[:], lhsT=ones_bf[:], rhs=ysq[i][:], start=True, stop=True), [b_ob, b_ysq[i]], [bp])
            S.op("act", lambda e: e.activation(out=L["sd"][i][:], in_=p[:], func=AF.Ln, bias=128.0 * EPS, scale=1.0),
                 [], [bp, bL["sd"][i]])
            S.op("act", lambda e: e.activation(out=L["sd"][i][:], in_=L["sd"][i][:], func=AF.Exp, scale=-0.5), [], [bL["sd"][i]])
            S.op("pool", lambda e: e.tensor_tensor(out=L["yn"][i][:], in0=L["yr"][i][:], in1=L["sd"][i][:], op=ALU.mult),
                 [bL["yr"][i], bL["sd"][i]], [bL["yn"][i]])
            S.op("act", lambda e: e.activation(out=yrT[i][:], in_=L["yn"][i][:], func=AF.Copy, scale=der[:, 2:3]),
                 [bL["yn"][i], b_der], [b_yrT[i]])
            outs.append(Buf())
            S.dma("sp", d["yT"][128:256, t0:t0 + BLK], yrT[i][:], reads=[b_yrT[i]], writes=[outs[-1]])
            pump(len(pieces))

        for f in proj_pieces(0):
            f()
        for blk in range(nblk):
            emit_compute(blk, proj_pieces(blk + 1) if blk + 1 < nblk else [])
        S.barrier()
    return outs


def pack_B_inputs(inp, l, h, uT_full, hc):
    w_in = inp["w_in"][l]
    b_in = inp["b_in"][l]
    hs = slice(h * 128, (h + 1) * 128)
    o_q, o_k, o_v, o_o, o_i, o_f, o_x, o_g = 0, 1024, 2048, 3072, 4096, 4104, 4112, 5136
    cols = np.concatenate([np.arange(o_q + h * 128, o_q + (h + 1) * 128), np.arange(o_k + h * 128, o_k + (h + 1) * 128),
                           np.arange(o_x + h * 128, o_x + (h + 1) * 128), np.arange(o_g + h * 128, o_g + (h + 1) * 128),
                           np.arange(o_v + h * 128, o_v + (h + 1) * 128), np.arange(o_o + h * 128, o_o + (h + 1) * 128),
                           np.array([o_i + h, o_f + h])])
    wh = np.ascontiguousarray(w_in[:, cols])
    bh = b_in[cols]
    pp = np.zeros((128, 24), np.float32)
    pp[:, 0:4] = inp["w_conv_m"][l][:, hs].T
    pp[:, 4:8] = inp["w_conv_m"][l][:, 1024 + h * 128:1024 + (h + 1) * 128].T
    pp[:, 8:12] = inp["w_conv_r"][l][:, hs].T
    pp[:, 12] = inp["b_conv_m"][l][hs]
    pp[:, 13] = inp["b_conv_m"][l][1024 + h * 128:1024 + (h + 1) * 128]
    pp[:, 14] = inp["b_conv_r"][l][hs]
    pp[:, 15] = bh[0:128]
    pp[:, 16] = bh[128:256]
    pp[:, 17] = bh[256:384]
    pp[:, 18] = bh[384:512]
    pp[:, 19] = inp["b_a"][l][hs]
    pp[:, 20] = inp["b_x"][l][hs]
    pp[:, 21] = inp["lru_lambda"][l][hs]
    pp[:, 22] = inp["lru_norm_g"][l][hs]
    pp[:, 23] = inp["mh_norm_g"][l][hs]
    fv = np.ascontiguousarray(bh[512:770][None, :])
    return {"uT": uT_full, "wh": wh, "pp": pp, "fv": fv,
            "wa": np.ascontiguousarray(inp["w_a"][l][h]), "wx": np.ascontiguousarray(inp["w_x"][l][h]),
            "ident_bf": hc["ident_bf"], "tri_f": hc["tri_f"], "ones_f": hc["ones_f"], "ones_bf": hc["ones_bf"]}


NSLOT = NE * CAP
BIGROW = float(NSLOT + 64)


def build_C(debug=False):
    nc = bass.Bass("TRN2", target_bir_lowering=False)
    with ExitStack() as es:
        cx = Ctx(nc, es)
        S = cx.S
        d = {
            "x": cx.dram_in("x", [TPC, D], F32),
            "yT": cx.dram_in("yT", [2 * D, TPC], BF16),
            "ada": cx.dram_in("ada", [1, 6 * D], F32),
            "w_out": cx.dram_in("w_out", [2 * D, D], F32),
            "lnp": cx.dram_in("lnp", [1, 4 * D], F32),
            "w_router": cx.dram_in("w_router", [D, NE], F32),
            "b_router": cx.dram_in("b_router", [1, NE], F32),
            "eoff": cx.dram_in("eoff", [1, NE], F32),
            "w_gate": cx.dram_in("w_gate", [NE, D, DFF], F32),
            "w_up": cx.dram_in("w_up", [NE, D, DFF], F32),
            "w_down": cx.dram_in("w_down", [NE, DFF, D], F32),
            "ident_bf": cx.dram_in("ident_bf", [128, 128], BF16),
            "ident_f": cx.dram_in("ident_f", [128, 128], F32),
            "tris_bf": cx.dram_in("tris_bf", [128, 128], BF16),
            "ones_bf": cx.dram_in("ones_bf", [128, 128], BF16),
            "xout": cx.dram_out("xout", [TPC, D], F32),
        }
        if debug:
            d["xbuf"] = nc.dram_tensor("xbuf", [NSLOT, D], BF16, kind="ExternalOutput")
            d["ybuf"] = nc.dram_tensor("ybuf", [NSLOT, D], F32, kind="ExternalOutput")
            d["dbg_dest"] = cx.dram_out("dbg_dest", [128, NT * 2], I32)
            d["dbg_gate"] = cx.dram_out("dbg_gate", [128, NT, 2], F32)
        else:
            d["xbuf"] = nc.dram_tensor("xbuf", [NSLOT, D], BF16)
            d["ybuf"] = nc.dram_tensor("ybuf", [NSLOT, D], F32)
        adac = cx.sb([128, 4, D], F32)
        b_adac = Buf()
        S.dma("sp", adac[:], d["ada"][:, 2 * D:6 * D].partition_broadcast(128).rearrange("p o (a n) -> p (o a) n", a=4),
              writes=[b_adac])
        S.op("dve", lambda e: e.tensor_scalar_add(out=adac[:, 2, :], in0=adac[:, 2, :], scalar1=1.0), [b_adac], [b_adac])
        d["x1s"] = nc.dram_tensor("x1s", [TPC, D], F32)
        outs = emit_C(cx, d, d["x"], None, None, adac[:, 0, :], adac[:, 1, :], adac[:, 2, :], adac[:, 3, :], b_adac,
                      d["xout"])
        S.finish(outs, "sp")
    return nc


def emit_C(cx, d, x_d, xres, b_xres, g1_ap, sh2_ap, sc2_ap, g2_ap, b_ada, xout_d):
    S = cx.S
    nc = cx.nc
    outs = []
    with ExitStack() as es0:
        ident = cx.sb([128, 128], BF16, es0); b_id = Buf()
        S.dma("sp", ident[:], d["ident_bf"], writes=[b_id])
        scat = []
        bc_reg = nc.gpsimd.alloc_register(f"bc{cx._n}")
        nc.gpsimd.reg_mov(bc_reg, NSLOT - 1)
        zt = cx.sb([128, D], BF16, es0); b_zt = Buf()
        S.op("pool", lambda e: e.memset(zt[:], 0.0), [], [b_zt])
        zfill = []
        for r0 in range(0, NSLOT, 1024):
            zfill.append(Buf())
            S.dma("sp" if (r0 // 1024) % 2 == 0 else "act",
                  d["xbuf"][r0:r0 + 1024, :].rearrange("(p s) n -> p s n", p=128),
                  zt[:].unsqueeze(1).to_broadcast([128, 8, D]), reads=[b_zt], writes=[zfill[-1]])
        NG = NT * NE
        destS = cx.sb([128, 2 * NT], I32, es0)
        destG = cx.sb([128, 2 * NT], I32, es0)
        gate2 = cx.sb([128, NT, 2], F32, es0)
        b_rt = Buf()
        u2b_all = cx.sb([128, NT, D], BF16, es0)
        b_u2b = [Buf() for _ in range(NT)]
        x1w = []
        with ExitStack() as es:
            def sb(shape, dt):
                return cx.sb(shape, dt, es)

            def sbn(shape, dt, n=2):
                return [cx.sb(shape, dt, es) for _ in range(n)], [Buf() for _ in range(n)]

            wout = sb([128, 16, D], BF16); b_wout = Buf()
            wo_v = d["w_out"].rearrange("(m p) n -> p m n", p=128)
            for q4 in range(4):
                S.dma("pool", wout[:, q4 * 4:(q4 + 1) * 4, :], wo_v[:, q4 * 4:(q4 + 1) * 4, :], writes=[b_wout])
            lnp = sb([128, 2, D], F32); b_lnp = Buf()
            S.dma("sp", lnp[:], d["lnp"][:, 0:2 * D].partition_broadcast(128).rearrange("p o (a n) -> p (o a) n", a=2), writes=[b_lnp])
            identf = sb([128, 128], F32); b_idf = Buf()
            S.dma("sp", identf[:], d["ident_f"], writes=[b_idf])
            tris = sb([128, 128], BF16); b_tris = Buf()
            S.dma("sp", tris[:], d["tris_bf"], writes=[b_tris])
            ones = sb([128, 128], BF16); b_ones = Buf()
            S.dma("sp", ones[:], d["ones_bf"], writes=[b_ones])
            wr = sb([128, 8, NE], F32); b_wr = Buf()
            S.dma("sp", wr[:], d["w_router"].rearrange("(k p) n -> p k n", p=128), writes=[b_wr])
            br = sb([128, NE], F32); b_br = Buf()
            S.dma("sp", br[:], d["b_router"].partition_broadcast(128), writes=[b_br])
            eoff = sb([128, NE], F32); b_eoff = Buf()
            S.dma("sp", eoff[:], d["eoff"].partition_broadcast(128), writes=[b_eoff])

            ytb, b_ytb = sbn([128, 16, 256], BF16)
            xin, b_xin = sbn([128, D], F32)
            t1_, b_t1_ = sbn([128, D], F32)
            z_, b_z_ = sbn([128, D], F32)
            x1t_, b_x1t_ = sbn([128, D], F32)
            u2f_, b_u2f_ = sbn([128, D], F32)
            u2T_, b_u2T_ = sbn([128, 8, 128], F32)
            xnA = sb([128, D], F32); b_xnA = Buf()
            tmpA = sb([128, D], F32); b_tmpA = Buf()
            xnB = sb([128, D], F32); b_xnB = Buf()
            tmpB = sb([128, D], F32); b_tmpB = Buf()
            lnsA = LNScratch(cx, es)
            lnsB = LNScratch(cx, es)
            aff_all = sb([128, NT, NE], F32)
            b_aff = [Buf() for _ in range(NT)]

            po = [cx.ps([128, 512], F32, es) for _ in range(2)]
            b_po = [Buf(), Buf()]
            pT = [cx.ps([128, 4, 128], F32, es) for _ in range(2)]
            b_pT = [Buf(), Buf()]
            plog = cx.ps([128, 512], F32, es); b_plog = Buf()
            ppre = cx.ps([128, 512], F32, es); b_ppre = Buf()
            ptot = cx.ps([128, 512], F32, es); b_ptot = Buf()

            yT_v = d["yT"].rearrange("(m p) t -> p m t", p=128)

            def S1(t):
                i = t % 2
                if t % 2 == 0:
                    yi = (t // 2) % 2
                    S.dma("sp", ytb[yi][:], yT_v[:, :, t * 128:t * 128 + 256], writes=[b_ytb[yi]])
                yi = (t // 2) % 2
                tsl = slice((t % 2) * 128, (t % 2) * 128 + 128)
                S.dma("act", xin[i][:], x_d[t * 128:(t + 1) * 128, :], writes=[b_xin[i]])
                for half in range(2):
                    hs = slice(half * 512, (half + 1) * 512)
                    for m in range(16):
                        S.op("pe", lambda e: e.matmul(po[half][:], lhsT=ytb[yi][:, m, tsl], rhs=wout[:, m, hs],
                                                      start=(m == 0), stop=(m == 15)), [b_ytb[yi], b_wout], [b_po[half]])
                    S.op("dve", lambda e: e.tensor_tensor(out=t1_[i][:, hs], in0=po[half][:], in1=g1_ap[:, hs], op=ALU.mult),
                         [b_ada], [b_po[half], b_t1_[i]])
                S.op("dve", lambda e: e.scalar_tensor_tensor(out=z_[i][:], in0=xin[i][:], scalar=ALPHA, in1=t1_[i][:],
                                                             op0=ALU.mult, op1=ALU.add), [b_xin[i], b_t1_[i]], [b_z_[i]])

            def S2(t):
                i = t % 2
                emit_ln_mod(cx, z_[i][:], b_z_[i], lnsA, xnA, b_xnA, lnp[:, 0, :], lnp[:, 1, :], b_lnp, x1t_[i][:], b_x1t_[i], tmpA, b_tmpA)
                x1w.append(Buf())
                S.dma("act", d["x1s"][t * 128:(t + 1) * 128, :], x1t_[i][:], reads=[b_x1t_[i]], writes=[x1w[-1]])

            def S3(t):
                i = t % 2
                emit_ln_mod(cx, x1t_[i][:], b_x1t_[i], lnsB, xnB, b_xnB, sc2_ap, sh2_ap, b_ada, u2f_[i][:], b_u2f_[i], tmpB, b_tmpB)
                S.op("act", lambda e: e.activation(out=u2b_all[:, t, :], in_=u2f_[i][:], func=AF.Copy), [b_u2f_[i]], [b_u2b[t]])

            def S4(t):
                i = t % 2
                u2f, b_u2f, u2T, b_u2T = u2f_[i], b_u2f_[i], u2T_[i], b_u2T_[i]
                for k in range(8):
                    S.op("pe", lambda e: e.transpose(pT[k // 4][:, k % 4, :], u2f[:, k * 128:(k + 1) * 128], identf[:]),
                         [b_u2f, b_idf], [b_pT[k // 4]])
                S.op("act", lambda e: e.activation(out=u2T[:, 0:4, :], in_=pT[0][:], func=AF.Copy), [], [b_pT[0], b_u2T])
                S.op("dve", lambda e: e.tensor_copy(out=u2T[:, 4:8, :], in_=pT[1][:]), [], [b_pT[1], b_u2T])
                for k in range(8):
                    S.op("pe", lambda e: e.matmul(plog[:, 0:NE], lhsT=u2T[:, k, :], rhs=wr[:, k, :], start=(k == 0), stop=(k == 7)),
                         [b_u2T, b_wr], [b_plog])
                S.op("act", lambda e: e.activation(out=aff_all[:, t, :], in_=plog[:, 0:NE], func=AF.Sigmoid), [], [b_plog, b_aff[t]])

            for kk in range(NT + 3):
                if kk < NT:
                    S1(kk)
                if 0 <= kk - 1 < NT:
                    S2(kk - 1)
                if 0 <= kk - 2 < NT:
                    S3(kk - 2)
                if 0 <= kk - 3 < NT:
                    S4(kk - 3)

            RR = sb([128, 12, NG], F32); b_R = Buf()
            aff = aff_all[:].rearrange("p t e -> p (t e)")
            sel, eq, selm, ge, msk, gv, pos, val, vld, dd, cntf = (RR[:, n, :] for n in range(11))
            q3 = lambda ap: ap.rearrange("p (a j) -> p a j", j=4)
            t3 = lambda ap: ap.rearrange("p (t e) -> p t e", e=NE)
            r128 = sb([128, 4, NT * 8], F32)
            m1, m2, gsc, gone = (r128[:, n, :] for n in range(4))
            r16 = sb([128, 8, NT], F32)
            gmax, gsum, rgs, first, second, g1s, gts = (r16[:, n, :] for n in range(7))
            mk = sb([128, NG], BF16); b_mk = Buf()
            S.op("dve", lambda e: e.tensor_tensor(out=t3(sel), in0=aff_all[:], in1=br[:].unsqueeze(1).to_broadcast([128, NT, NE]), op=ALU.add),
                 b_aff + [b_br], [b_R])
            S.op("dve", lambda e: e.tensor_reduce(out=m1, in_=q3(sel), axis=AX.X, op=ALU.max), [], [b_R])
            S.op("dve", lambda e: e.tensor_tensor(out=q3(eq), in0=q3(sel), in1=m1.unsqueeze(2).to_broadcast([128, NT * 8, 4]), op=ALU.is_equal), [], [b_R])
            S.op("dve", lambda e: e.scalar_tensor_tensor(out=selm, in0=eq, scalar=-1e9, in1=sel, op0=ALU.mult, op1=ALU.add), [], [b_R])
            S.op("dve", lambda e: e.tensor_reduce(out=m2, in_=q3(selm), axis=AX.X, op=ALU.max), [], [b_R])
            S.op("dve", lambda e: e.tensor_tensor(out=gsc, in0=m1, in1=m2, op=ALU.add), [], [b_R])
            g8 = lambda ap: ap.rearrange("p (t g) -> p t g", g=8)
            S.op("dve", lambda e: e.tensor_reduce(out=gmax, in_=g8(gsc), axis=AX.X, op=ALU.max), [], [b_R])
            S.op("dve", lambda e: e.tensor_tensor(out=g8(gone), in0=g8(gsc), in1=gmax.unsqueeze(2).to_broadcast([128, NT, 8]), op=ALU.is_equal), [], [b_R])
            S.op("dve", lambda e: e.tensor_tensor(out=q3(ge), in0=q3(sel), in1=m2.unsqueeze(2).to_broadcast([128, NT * 8, 4]), op=ALU.is_ge), [], [b_R])
            S.op("dve", lambda e: e.tensor_tensor(out=q3(msk), in0=q3(ge), in1=gone.unsqueeze(2).to_broadcast([128, NT * 8, 4]), op=ALU.mult), [], [b_R])
            S.op("dve", lambda e: e.tensor_tensor(out=gv, in0=aff, in1=msk, op=ALU.mult), [], [b_R])
            S.op("dve", lambda e: e.tensor_reduce(out=gsum, in_=t3(gv), axis=AX.X, op=ALU.add), [], [b_R])
            S.op("dve", lambda e: e.reciprocal(out=rgs, in_=gsum), [], [b_R])
            S.op("dve", lambda e: e.tensor_tensor(out=t3(gv), in0=t3(gv), in1=rgs.unsqueeze(2).to_broadcast([128, NT, NE]), op=ALU.mult), [], [b_R])
            S.op("dve", lambda e: e.tensor_copy(out=mk[:], in_=msk), [b_R], [b_mk])
            S.op("pe", lambda e: e.matmul(ppre[:], lhsT=tris[:], rhs=mk[:], start=True, stop=True), [b_tris, b_mk], [b_ppre])
            S.op("pe", lambda e: e.matmul(ptot[:], lhsT=ones[:], rhs=mk[:], start=True, stop=True), [b_ones, b_mk], [b_ptot])
            S.op("dve", lambda e: e.tensor_copy(out=dd, in_=ptot[:]), [], [b_ptot, b_R])
            S.op("dve", lambda e: e.memset(cntf[:, 0:NE], 0.0), [], [b_R])
            for t in range(1, NT):
                S.op("dve", lambda e: e.tensor_tensor(out=cntf[:, t * NE:(t + 1) * NE], in0=cntf[:, (t - 1) * NE:t * NE],
                                                      in1=dd[:, (t - 1) * NE:t * NE], op=ALU.add), [], [b_R])
            S.op("dve", lambda e: e.tensor_tensor(out=pos, in0=ppre[:], in1=cntf, op=ALU.add), [], [b_ppre, b_R])
            S.op("dve", lambda e: e.tensor_scalar(out=vld, in0=pos, scalar1=float(CAP) - 0.5, scalar2=None, op0=ALU.is_lt), [], [b_R])
            S.op("dve", lambda e: e.tensor_tensor(out=vld, in0=vld, in1=msk, op=ALU.mult), [], [b_R])
            S.op("dve", lambda e: e.tensor_tensor(out=t3(val), in0=t3(pos), in1=eoff[:].unsqueeze(1).to_broadcast([128, NT, NE]), op=ALU.add),
                 [b_eoff], [b_R])
            S.op("dve", lambda e: e.scalar_tensor_tensor(out=dd, in0=val, scalar=1.0, in1=vld, op0=ALU.add, op1=ALU.mult), [], [b_R])
            S.op("dve", lambda e: e.tensor_scalar_add(out=dd, in0=dd, scalar1=-1.0), [], [b_R])
            S.op("dve", lambda e: e.tensor_reduce(out=first, in_=t3(dd), axis=AX.X, op=ALU.max), [], [b_R])
            S.op("dve", lambda e: e.tensor_tensor(out=t3(eq), in0=t3(dd), in1=first.unsqueeze(2).to_broadcast([128, NT, NE]), op=ALU.is_equal), [], [b_R])
            S.op("dve", lambda e: e.scalar_tensor_tensor(out=selm, in0=eq, scalar=-1e9, in1=dd, op0=ALU.mult, op1=ALU.add), [], [b_R])
            S.op("dve", lambda e: e.tensor_reduce(out=second, in_=t3(selm), axis=AX.X, op=ALU.max), [], [b_R])
            S.op("dve", lambda e: e.tensor_tensor(out=gv, in0=gv, in1=vld, op=ALU.mult), [], [b_R])
            S.op("dve", lambda e: e.tensor_reduce(out=gts, in_=t3(gv), axis=AX.X, op=ALU.add), [], [b_R])
            S.op("dve", lambda e: e.tensor_tensor(out=ge, in0=gv, in1=eq, op=ALU.mult), [], [b_R])
            S.op("dve", lambda e: e.tensor_reduce(out=g1s, in_=t3(ge), axis=AX.X, op=ALU.add), [], [b_R])
            S.op("dve", lambda e: e.tensor_copy(out=gate2[:, :, 0], in_=g1s), [b_R], [b_rt])
            S.op("dve", lambda e: e.tensor_tensor(out=gate2[:, :, 1], in0=gts, in1=g1s, op=ALU.subtract), [b_R], [b_rt])
            fs = r16[:, 3:5, :]
            neg = r16[:, 5:7, :]
            dS = destS[:].rearrange("p (t s) -> p s t", s=2)
            dG = destG[:].rearrange("p (t s) -> p s t", s=2)
            S.op("dve", lambda e: e.tensor_scalar(out=neg, in0=fs, scalar1=0.0, scalar2=BIGROW + 1.0, op0=ALU.is_lt, op1=ALU.mult), [b_rt], [b_R])
            S.op("dve", lambda e: e.tensor_tensor(out=neg, in0=neg, in1=fs, op=ALU.add), [], [b_R])
            S.op("dve", lambda e: e.tensor_copy(out=dS, in_=neg), [b_R], [b_rt])
            S.op("dve", lambda e: e.tensor_scalar_max(out=neg, in0=fs, scalar1=0.0), [b_rt], [b_R])
            S.op("dve", lambda e: e.tensor_copy(out=dG, in_=neg), [b_R], [b_rt])
            for t in range(NT):
                for sidx in range(2):
                    scat.append(Buf())
                    S.dma_fn("pool", lambda e: e.indirect_dma_start(
                        out=d["xbuf"][:, :], out_offset=bass.IndirectOffsetOnAxis(ap=destS[:, 2 * t + sidx:2 * t + sidx + 1], axis=0),
                        in_=u2b_all[:, t, :], in_offset=None, bounds_check=bc_reg, oob_is_err=False),
                        [b_u2b[t], b_rt] + zfill, [scat[-1]])
        S.barrier()
        if "dbg_dest" in d:
            for nm, src in (("dbg_dest", destS), ("dbg_gate", gate2)):
                outs.append(Buf())
                S.dma("sp", d[nm], src[:], reads=[b_rt], writes=[outs[-1]])
        ysc = []
        with ExitStack() as es:
            def sbn(shape, dt, n=2):
                return [cx.sb(shape, dt, es) for _ in range(n)], [Buf() for _ in range(n)]

            wg, b_wg = sbn([128, 8, DFF], BF16)
            wu, b_wu = sbn([128, 8, DFF], BF16)
            wd, b_wd = sbn([128, 4, D], BF16)
            Xe, b_Xe = sbn([128, CAP // 128, D], BF16)
            XT, b_XT = sbn([128, 8, CAP], BF16)
            hT, b_hT = sbn([128, 4, CAP], BF16)
            sg, b_sg = sbn([128, CAP], F32)
            Ye, b_Ye = sbn([128, D], F32)
            pX = cx.ps([128, 8, 128], BF16, es); b_pX = Buf()
            pg = [cx.ps([128, 512], F32, es) for _ in range(2)]
            b_pg = [Buf(), Buf()]
            pu = [cx.ps([128, 512], F32, es) for _ in range(2)]
            b_pu = [Buf(), Buf()]
            pd = [cx.ps([128, 512], F32, es) for _ in range(2)]
            b_pd = [Buf(), Buf()]
            wg_v = d["w_gate"].rearrange("e (k p) n -> e p k n", p=128)
            wu_v = d["w_up"].rearrange("e (k p) n -> e p k n", p=128)
            wd_v = d["w_down"].rearrange("e (k p) n -> e p k n", p=128)
            nst = CAP // 128
            pcnt = 0
            dcnt = 0
            for ex in range(NE):
                i = ex % 2
                S.dma("pool", wg[i][:], wg_v[ex], writes=[b_wg[i]])
                S.dma("pool", wu[i][:], wu_v[ex], writes=[b_wu[i]])
                S.dma("pool", wd[i][:], wd_v[ex], writes=[b_wd[i]])
                S.dma("sp", Xe[i][:], d["xbuf"][ex * CAP:(ex + 1) * CAP, :].rearrange("(s p) n -> p s n", p=128),
                      reads=scat, writes=[b_Xe[i]])
                for st in range(nst):
                    for k in range(8):
                        S.op("pe", lambda e: e.transpose(pX[:, k, :], Xe[i][:, st, k * 128:(k + 1) * 128], ident[:]),
                             [b_Xe[i], b_id], [b_pX])
                    if st % 2 == 0:
                        S.op("act", lambda e: e.activation(out=XT[i][:, :, st * 128:(st + 1) * 128], in_=pX[:], func=AF.Copy),
                             [], [b_pX, b_XT[i]])
                    else:
                        S.op("dve", lambda e: e.tensor_copy(out=XT[i][:, :, st * 128:(st + 1) * 128], in_=pX[:]),
                             [], [b_pX, b_XT[i]])
                for f in range(4):
                    pi = pcnt % 2
                    pcnt += 1
                    for k in range(8):
                        S.op("pe", lambda e: e.matmul(pg[pi][:, 0:CAP], lhsT=wg[i][:, k, f * 128:(f + 1) * 128], rhs=XT[i][:, k, :],
                                                      start=(k == 0), stop=(k == 7)), [b_wg[i], b_XT[i]], [b_pg[pi]])
                    for k in range(8):
                        S.op("pe", lambda e: e.matmul(pu[pi][:, 0:CAP], lhsT=wu[i][:, k, f * 128:(f + 1) * 128], rhs=XT[i][:, k, :],
                                                      start=(k == 0), stop=(k == 7)), [b_wu[i], b_XT[i]], [b_pu[pi]])
                    S.op("act", lambda e: e.activation(out=sg[pi][:], in_=pg[pi][:, 0:CAP], func=AF.Silu), [], [b_pg[pi], b_sg[pi]])
                    S.op("dve", lambda e: e.tensor_tensor(out=hT[i][:, f, :], in0=pu[pi][:, 0:CAP], in1=sg[pi][:], op=ALU.mult),
                         [b_sg[pi]], [b_pu[pi], b_hT[i]])
                for st in range(nst):
                    yi = dcnt % 2
                    dcnt += 1
                    for half in range(2):
                        hs = slice(half * 512, (half + 1) * 512)
                        for f in range(4):
                            S.op("pe", lambda e: e.matmul(pd[half][:], lhsT=hT[i][:, f, st * 128:(st + 1) * 128], rhs=wd[i][:, f, hs],
                                                          start=(f == 0), stop=(f == 3)), [b_hT[i], b_wd[i]], [b_pd[half]])
                        if half == 0:
                            S.op("act", lambda e: e.activation(out=Ye[yi][:, hs], in_=pd[half][:], func=AF.Copy),
                                 [], [b_pd[half], b_Ye[yi]])
                        else:
                            S.op("dve", lambda e: e.tensor_copy(out=Ye[yi][:, hs], in_=pd[half][:]), [], [b_pd[half], b_Ye[yi]])
                    ysc.append(Buf())
                    S.dma("act", d["ybuf"][ex * CAP + st * 128:ex * CAP + (st + 1) * 128, :], Ye[yi][:],
                          reads=[b_Ye[yi]], writes=[ysc[-1]])
            S.barrier()

        with ExitStack() as es:
            def sbn(shape, dt, n=2):
                return [cx.sb(shape, dt, es) for _ in range(n)], [Buf() for _ in range(n)]

            lnp = cx.sb([128, 2, D], F32, es); b_lnp = Buf()
            S.dma("sp", lnp[:], d["lnp"][:, 2 * D:4 * D].partition_broadcast(128).rearrange("p o (a n) -> p (o a) n", a=2), writes=[b_lnp])
            Yg, b_Yg = sbn([128, 2 * D], F32, 3)
            x1r, b_x1r = sbn([128, D], F32, 3)
            acc, b_acc = sbn([128, D], F32)
            z_, b_z_ = sbn([128, D], F32)
            xn = cx.sb([128, D], F32, es); b_xn = Buf()
            tmp = cx.sb([128, D], F32, es); b_tmp = Buf()
            xo, b_xo = sbn([128, D], F32)
            lns = LNScratch(cx, es)

            def G(t):
                i = t % 3
                S.dma("sp", x1r[i][:], d["x1s"][t * 128:(t + 1) * 128, :], reads=x1w, writes=[b_x1r[i]])
                for sidx in range(2):
                    S.dma_fn("pool", lambda e: e.indirect_dma_start(
                        out=Yg[i][:, sidx * D:(sidx + 1) * D], out_offset=None, in_=d["ybuf"][:, :],
                        in_offset=bass.IndirectOffsetOnAxis(ap=destG[:, 2 * t + sidx:2 * t + sidx + 1], axis=0),
                        bounds_check=bc_reg, oob_is_err=False), [b_rt] + ysc, [b_Yg[i]])

            def Cmb(t):
                i = t % 3
                j = t % 2
                S.op("dve", lambda e: e.tensor_scalar_mul(out=acc[j][:], in0=Yg[i][:, 0:D], scalar1=gate2[:, t, 0:1]),
                     [b_Yg[i], b_rt], [b_acc[j]])
                S.op("dve", lambda e: e.scalar_tensor_tensor(out=acc[j][:], in0=Yg[i][:, D:2 * D], scalar=gate2[:, t, 1:2],
                                                             in1=acc[j][:], op0=ALU.mult, op1=ALU.add),
                     [b_Yg[i], b_rt], [b_acc[j]])
                S.op("pool", lambda e: e.tensor_tensor(out=acc[j][:], in0=acc[j][:], in1=g2_ap, op=ALU.mult), [b_ada], [b_acc[j]])
                S.op("dve", lambda e: e.scalar_tensor_tensor(out=z_[j][:], in0=x1r[i][:], scalar=ALPHA, in1=acc[j][:],
                                                             op0=ALU.mult, op1=ALU.add), [b_x1r[i], b_acc[j]], [b_z_[j]])
                emit_ln_mod(cx, z_[j][:], b_z_[j], lns, xn, b_xn, lnp[:, 0, :], lnp[:, 1, :], b_lnp, xo[j][:], b_xo[j], tmp, b_tmp)
                outs.append(Buf())
                S.dma("sp", xout_d[t * 128:(t + 1) * 128, :], xo[j][:], reads=[b_xo[j]], writes=[outs[-1]])

            G(0)
            G(1)
            for t in range(NT):
                if t + 2 < NT:
                    G(t + 2)
                Cmb(t)
            S.barrier()
        S.barrier()
    return outs


def pack_C_inputs(inp, l, j, x_shard, yT_shard, ada_row, hc):
    lnp = np.concatenate([inp["ln_g"][l, 0], inp["ln_b"][l, 0], inp["ln_g"][l, 1], inp["ln_b"][l, 1]])[None, :]
    return {"x": x_shard, "yT": yT_shard, "ada": ada_row, "w_out": np.ascontiguousarray(inp["w_out"][l]),
            "lnp": np.ascontiguousarray(lnp), "w_router": inp["w_router"], "b_router": inp["b_router"][None, :],
            "eoff": hc["eoff"], "w_gate": np.ascontiguousarray(inp["w_gate"][l]), "w_up": np.ascontiguousarray(inp["w_up"][l]),
            "w_down": np.ascontiguousarray(inp["w_down"][l]), "ident_bf": hc["ident_bf"], "ident_f": hc["ident_f"],
            "tris_bf": hc["tris_bf"], "ones_bf": hc["ones_bf"]}


def _run(nc, in_maps):
    res = run_bass_kernel_spmd(nc, in_maps, core_ids=list(range(NCORES)))
    return res.results


def kernel_unfused(**inputs):
    inp = {k: np.asarray(v) for k, v in inputs.items()}
    hc = host_consts()
    x = np.ascontiguousarray(inp["x"][0], dtype=np.float32)
    cT = np.ascontiguousarray(inp["c"][0].reshape(8, 128).T)
    for l in range(DEPTH):
        ra = _run(build_A(), [{"x": np.ascontiguousarray(x[j * TPC:(j + 1) * TPC]), "cT": cT,
                               "w_ada": np.ascontiguousarray(inp["w_ada"][l]),
                               "b_ada": np.ascontiguousarray(inp["b_ada"][l][None, :]),
                               "ident_bf": hc["ident_bf"]} for j in range(NCORES)])
        uT = np.ascontiguousarray(np.concatenate([r["uT"] for r in ra], axis=1))
        ada = ra[0]["ada"]
        rb = _run(build_B(), [pack_B_inputs(inp, l, h, uT, hc) for h in range(NCORES)])
        yT = np.concatenate([r["yT"][0:128] for r in rb] + [r["yT"][128:256] for r in rb], axis=0)
        rc = _run(build_C(), [pack_C_inputs(inp, l, j, np.ascontiguousarray(x[j * TPC:(j + 1) * TPC]),
                                            np.ascontiguousarray(yT[:, j * TPC:(j + 1) * TPC]), ada, hc)
                              for j in range(NCORES)])
        x = np.concatenate([r["xout"] for r in rc], axis=0)
    return x[None].astype(np.float32)


def kernel(**inputs):
    return kernel_unfused(**inputs)
```

```python
import numpy as np
import ml_dtypes
from contextlib import ExitStack
import concourse.bass as bass
import concourse.mybir as mybir
from concourse.bass_utils import run_bass_kernel_spmd

F32 = mybir.dt.float32
BF16 = mybir.dt.bfloat16
I32 = mybir.dt.int32
AF = mybir.ActivationFunctionType
ALU = mybir.AluOpType
AX = mybir.AxisListType

NCORES = 8
D = 1024
SEQ = 16384
TPC = SEQ // NCORES
NT = TPC // 128
DEPTH = 2
DH = 128
NE = 32
DFF = 512
CAP = 256
ALPHA = (2 * DEPTH) ** 0.25
EPS = 1e-5
BLK = 512
NBLK = SEQ // BLK


class Buf:
    __slots__ = ("w", "r")

    def __init__(self):
        self.w = None
        self.r = {}


class Sched:
    ND = 8

    def __init__(self, nc, es):
        self.nc = nc
        self.engs = {"pe": nc.tensor, "dve": nc.vector, "act": nc.scalar, "pool": nc.gpsimd, "sp": nc.sync}
        self.semh = {}
        for e in self.engs:
            self.semh[e] = es.enter_context(nc.semaphore(f"s_{e}"))
        self.cnt = {e: 0 for e in self.engs}
        self.seen = {e: {} for e in self.engs}
        self.semh["cc"] = es.enter_context(nc.semaphore("s_cc"))
        self.ccn = 0
        self.dqn = {}
        for q in ("sp", "act", "pool"):
            self.dqn[q] = 0
            for i in range(self.ND):
                self.semh[(q, i)] = es.enter_context(nc.semaphore(f"d_{q}{i}"))

    def _wait(self, e, key, val):
        if self.seen[e].get(key, 0) >= val:
            return
        self.engs[e].wait_ge(self.semh[key], val)
        self.seen[e][key] = val

    def _deps(self, e, reads, writes):
        deps = {}
        for b in reads:
            if b.w is not None:
                k, v = b.w
                if deps.get(k, 0) < v:
                    deps[k] = v
        for b in writes:
            if b.w is not None:
                k, v = b.w
                if deps.get(k, 0) < v:
                    deps[k] = v
            for k, v in b.r.items():
                if deps.get(k, 0) < v:
                    deps[k] = v
        for k, v in deps.items():
            if e == "pe" and k == "pe":
                continue
            self._wait(e, k, v)

    def _commit(self, tok, reads, writes):
        k, v = tok
        for b in reads:
            if b.r.get(k, 0) < v:
                b.r[k] = v
        for b in writes:
            b.w = tok
            b.r = {}

    def op(self, e, fn, reads=(), writes=()):
        self._deps(e, reads, writes)
        inst = fn(self.engs[e])
        self.cnt[e] += 1
        inst.then_inc(self.semh[e], 1)
        self._commit((e, self.cnt[e]), reads, writes)
        return inst

    def dma(self, q, out, in_, reads=(), writes=(), **kw):
        return self.dma_fn(q, lambda e: e.dma_start(out=out, in_=in_, **kw), reads, writes)

    def dma_fn(self, q, fn, reads=(), writes=()):
        n = self.dqn[q]
        i = n % self.ND
        rnd = n // self.ND
        self.dqn[q] = n + 1
        key = (q, i)
        if rnd > 0:
            self._wait(q, key, 16 * rnd)
        self._deps(q, reads, writes)
        inst = fn(self.engs[q])
        inst.then_inc(self.semh[key], 16)
        self._commit((key, 16 * (rnd + 1)), reads, writes)
        return inst

    def cc(self, fn, reads=(), writes=()):
        self._deps("pool", reads, writes)
        inst = fn(self.engs["pool"])
        self.ccn += 1
        inst.then_inc(self.semh["cc"], 1)
        self._commit(("cc", self.ccn), reads, writes)
        return inst

    def finish(self, bufs, e="sp"):
        self._deps(e, bufs, bufs)

    def barrier(self):
        for e in self.engs:
            for f in self.engs:
                if f != e and self.cnt[f] > 0:
                    self._wait(e, f, self.cnt[f])
            if self.ccn > 0:
                self._wait(e, "cc", self.ccn)
            for q in ("sp", "act", "pool"):
                n = self.dqn[q]
                for i in range(self.ND):
                    c = (n - i + self.ND - 1) // self.ND if n > i else 0
                    if c > 0:
                        self._wait(e, (q, i), 16 * c)


class Ctx:
    def __init__(self, nc, es):
        self.nc = nc
        self.es = es
        self.S = Sched(nc, es)
        self._n = 0

    def sb(self, shape, dt, es=None, name=None):
        self._n += 1
        return (es or self.es).enter_context(self.nc.sbuf_tensor(name or f"sb{self._n}", list(shape), dt))

    def ps(self, shape, dt, es=None, name=None):
        self._n += 1
        return (es or self.es).enter_context(self.nc.psum_tensor(name or f"ps{self._n}", list(shape), dt))

    def dram_in(self, name, shape, dt):
        return self.nc.dram_tensor(name, list(shape), dt, kind="ExternalInput").ap()

    def dram_out(self, name, shape, dt):
        return self.nc.dram_tensor(name, list(shape), dt, kind="ExternalOutput").ap()


def host_consts():
    c = {}
    c["ident_bf"] = np.eye(128, dtype=np.float32).astype(ml_dtypes.bfloat16)
    c["ident_f"] = np.eye(128, dtype=np.float32)
    r = np.arange(128)
    c["tri_f"] = (r[:, None] <= r[None, :]).astype(np.float32)
    c["ones_f"] = np.ones((128, 128), np.float32)
    c["ones_bf"] = np.ones((128, 128), np.float32).astype(ml_dtypes.bfloat16)
    c["tris_bf"] = (r[:, None] < r[None, :]).astype(np.float32).astype(ml_dtypes.bfloat16)
    c["eoff"] = (np.arange(NE, dtype=np.float32) * CAP)[None, :]
    return c


def emit_ada(cx, cT_d, w_ada_d, b_ada_d, ada_bc, b_ada_bc):
    S = cx.S
    with ExitStack() as es:
        cT = cx.sb([128, 8], F32, es)
        b_cT = Buf()
        cond = cx.sb([128, 8], F32, es)
        b_cond = Buf()
        condB = cx.sb([128, 8, 128], F32, es)
        b_condB = Buf()
        bias = cx.sb([128, 6 * D], F32, es)
        b_bias = Buf()
        wbuf = [cx.sb([128, 8, 512], F32, es) for _ in range(2)]
        b_w = [Buf(), Buf()]
        pp = [cx.ps([128, 512], F32, es) for _ in range(2)]
        b_pp = [Buf(), Buf()]
        S.dma("sp", cT[:], cT_d, writes=[b_cT])
        S.dma("sp", bias[:], b_ada_d.partition_broadcast(128), writes=[b_bias])
        S.op("act", lambda e: e.activation(out=cond[:], in_=cT[:], func=AF.Silu), [b_cT], [b_cond])
        for k in range(8):
            S.op("dve", lambda e: e.tensor_copy(out=condB[:, k, :], in_=cond[:, k:k + 1].to_broadcast([128, 128])),
                 [b_cond], [b_condB])
        wv = w_ada_d.rearrange("(k p) n -> p k n", p=128)
        for j in range(12):
            w = wbuf[j % 2]
            S.dma("sp" if j % 2 == 0 else "act", w[:], wv[:, :, j * 512:(j + 1) * 512], writes=[b_w[j % 2]])
            p = pp[j % 2]
            for k in range(8):
                S.op("pe", lambda e: e.matmul(p[:], lhsT=condB[:, k, :], rhs=w[:, k, :], start=(k == 0), stop=(k == 7)),
                     [b_condB, b_w[j % 2]], [b_pp[j % 2]])
            S.op("dve", lambda e: e.tensor_tensor(out=ada_bc[:, j * 512:(j + 1) * 512], in0=p[:],
                                                  in1=bias[:, j * 512:(j + 1) * 512], op=ALU.add),
                 [b_pp[j % 2], b_bias], [b_ada_bc])
        S.barrier()


def emit_ln_stats(cx, x_ap, b_x, st, mv, rs, nb, b_st):
    S = cx.S
    S.op("dve", lambda e: e.bn_stats(out=st[:, 0:6], in_=x_ap[:, 0:512]), [b_x], [b_st])
    S.op("dve", lambda e: e.bn_stats(out=st[:, 6:12], in_=x_ap[:, 512:1024]), [b_x], [b_st])
    S.op("dve", lambda e: e.bn_aggr(out=mv[:], in_=st[:]), [b_st], [b_st])
    S.op("act", lambda e: e.activation(out=rs[:], in_=mv[:, 1:2], func=AF.Sqrt, bias=EPS, scale=1.0), [b_st], [b_st])
    S.op("dve", lambda e: e.reciprocal(out=rs[:], in_=rs[:]), [b_st], [b_st])
    S.op("dve", lambda e: e.scalar_tensor_tensor(out=nb[:], in0=mv[:, 0:1], scalar=-1.0, in1=rs[:],
                                                 op0=ALU.mult, op1=ALU.mult), [b_st], [b_st])


def g_ln_mod(cx, x_ap, b_x, lns, xn, b_xn, A_ap, B_ap, b_ab, out_ap, b_out, tmp, b_tmp):
    S = cx.S
    st, mv, rs, nb, b_st = lns.st, lns.mv, lns.rs, lns.nb, lns.b
    S.op("dve", lambda e: e.bn_stats(out=st[:, 0:6], in_=x_ap[:, 0:512]), [b_x], [b_st]); yield
    S.op("dve", lambda e: e.bn_stats(out=st[:, 6:12], in_=x_ap[:, 512:1024]), [b_x], [b_st]); yield
    S.op("dve", lambda e: e.bn_aggr(out=mv[:], in_=st[:]), [b_st], [b_st]); yield
    S.op("act", lambda e: e.activation(out=rs[:], in_=mv[:, 1:2], func=AF.Sqrt, bias=EPS, scale=1.0), [b_st], [b_st]); yield
    S.op("dve", lambda e: e.reciprocal(out=rs[:], in_=rs[:]), [b_st], [b_st]); yield
    S.op("dve", lambda e: e.scalar_tensor_tensor(out=nb[:], in0=mv[:, 0:1], scalar=-1.0, in1=rs[:],
                                                 op0=ALU.mult, op1=ALU.mult), [b_st], [b_st]); yield
    S.op("act", lambda e: e.activation(out=xn[:], in_=x_ap, func=AF.Identity, bias=nb[:], scale=rs[:]), [b_x, b_st], [b_xn]); yield
    S.op("dve", lambda e: e.tensor_tensor(out=tmp[:], in0=xn[:], in1=A_ap, op=ALU.mult), [b_xn, b_ab], [b_tmp]); yield
    S.op("pool", lambda e: e.tensor_tensor(out=out_ap, in0=tmp[:], in1=B_ap, op=ALU.add), [b_tmp, b_ab], [b_out]); yield


def round_robin(gens):
    gens = [g for g in gens if g is not None]
    while gens:
        nxt = []
        for g in gens:
            try:
                next(g)
                nxt.append(g)
            except StopIteration:
                pass
        gens = nxt


class LNScratch:
    def __init__(self, cx, es):
        self.st = cx.sb([128, 12], F32, es)
        self.mv = cx.sb([128, 2], F32, es)
        self.rs = cx.sb([128, 1], F32, es)
        self.nb = cx.sb([128, 1], F32, es)
        self.b = Buf()


def emit_ln_mod(cx, x_ap, b_x, lns, xn, b_xn, A_ap, B_ap, b_ab, out_ap, b_out, tmp, b_tmp):
    S = cx.S
    emit_ln_stats(cx, x_ap, b_x, lns.st, lns.mv, lns.rs, lns.nb, lns.b)
    S.op("act", lambda e: e.activation(out=xn[:], in_=x_ap, func=AF.Identity, bias=lns.nb[:], scale=lns.rs[:]),
         [b_x, lns.b], [b_xn])
    S.op("dve", lambda e: e.tensor_tensor(out=tmp[:], in0=xn[:], in1=A_ap, op=ALU.mult), [b_xn, b_ab], [b_tmp])
    S.op("pool", lambda e: e.tensor_tensor(out=out_ap, in0=tmp[:], in1=B_ap, op=ALU.add), [b_tmp, b_ab], [b_out])


def build_A():
    nc = bass.Bass("TRN2", target_bir_lowering=False)
    with ExitStack() as es:
        cx = Ctx(nc, es)
        S = cx.S
        x_d = cx.dram_in("x", [TPC, D], F32)
        cT_d = cx.dram_in("cT", [128, 8], F32)
        w_ada_d = cx.dram_in("w_ada", [D, 6 * D], F32)
        b_ada_d = cx.dram_in("b_ada", [1, 6 * D], F32)
        ident_d = cx.dram_in("ident_bf", [128, 128], BF16)
        uT_d = cx.dram_out("uT", [D, TPC], BF16)
        ada_d = cx.dram_out("ada", [1, 6 * D], F32)

        ada = cx.sb([128, 6 * D], F32)
        b_ada = Buf()
        emit_ada(cx, cT_d, w_ada_d, b_ada_d, ada, b_ada)
        b_adaout = Buf()
        S.dma("sp", ada_d, ada[0:1, :], reads=[b_ada], writes=[b_adaout])
        S.finish([b_adaout], "sp")
        ident = cx.sb([128, 128], BF16)
        b_id = Buf()
        S.dma("sp", ident[:], ident_d, writes=[b_id])
        S.op("dve", lambda e: e.tensor_scalar_add(out=ada[:, D:2 * D], in0=ada[:, D:2 * D], scalar1=1.0), [b_ada], [b_ada])
        emit_A_body(cx, x_d, None, None, ada, b_ada, ident, b_id, uT_d)
    return nc


def emit_A_body(cx, x_d, xres, b_xres, ada, b_ada, ident, b_id, uT_d, sh_off=0, sc_off=D):
    S = cx.S
    with ExitStack() as es:
        lns = LNScratch(cx, es)
        xin = [cx.sb([128, D], F32, es) for _ in range(2)]
        b_xin = [Buf(), Buf()]
        xn = cx.sb([128, D], F32, es)
        b_xn = Buf()
        tmp = cx.sb([128, D], F32, es)
        b_tmp = Buf()
        ub = [cx.sb([128, D], BF16, es) for _ in range(2)]
        b_ub = [Buf(), Buf()]
        pT = [cx.ps([128, 8, 128], BF16, es) for _ in range(2)]
        b_pT = [Buf(), Buf()]
        uTs = [cx.sb([128, 8, 128], BF16, es) for _ in range(2)]
        b_uTs = [Buf(), Buf()]
        outs = []
        uT_v = uT_d.rearrange("(k p) t -> p k t", p=128)
        for t in range(NT):
            i = t % 2
            if x_d is not None:
                S.dma("sp", xin[i][:], x_d[t * 128:(t + 1) * 128, :], writes=[b_xin[i]])
                x_ap, bx = xin[i][:], b_xin[i]
            else:
                x_ap, bx = xres[:, t, :], b_xres[t]
            emit_ln_mod(cx, x_ap, bx, lns, xn, b_xn, ada[:, sc_off:sc_off + D], ada[:, sh_off:sh_off + D], b_ada,
                        ub[i][:], b_ub[i], tmp, b_tmp)
            for k in range(8):
                S.op("pe", lambda e: e.transpose(pT[i][:, k, :], ub[i][:, k * 128:(k + 1) * 128], ident[:]),
                     [b_ub[i], b_id], [b_pT[i]])
            S.op("act", lambda e: e.activation(out=uTs[i][:], in_=pT[i][:], func=AF.Copy), [b_pT[i]], [b_uTs[i]])
            outs.append(Buf())
            S.dma("sp", uT_v[:, :, t * 128:(t + 1) * 128], uTs[i][:], reads=[b_uTs[i]], writes=[outs[-1]])
        S.finish(outs, "sp")
        S.barrier()


LN_INV_SQRT_DH = float(-0.5 * np.log(DH))
GELU_C = 1.5957691216057308


def build_B():
    nc = bass.Bass("TRN2", target_bir_lowering=False)
    with ExitStack() as es:
        cx = Ctx(nc, es)
        d = {
            "uT": cx.dram_in("uT", [D, SEQ], BF16),
            "wh": cx.dram_in("wh", [D, 770], F32),
            "pp": cx.dram_in("pp", [128, 24], F32),
            "fv": cx.dram_in("fv", [1, 258], F32),
            "wa": cx.dram_in("wa", [128, 128], F32),
            "wx": cx.dram_in("wx", [128, 128], F32),
            "ident_bf": cx.dram_in("ident_bf", [128, 128], BF16),
            "tri_f": cx.dram_in("tri_f", [128, 128], F32),
            "ones_f": cx.dram_in("ones_f", [128, 128], F32),
            "ones_bf": cx.dram_in("ones_bf", [128, 128], BF16),
            "yT": cx.dram_out("yT", [256, SEQ], BF16),
        }
        outs = emit_B(cx, d)
        cx.S.finish(outs, "sp")
    return nc


def emit_B(cx, d, nblk=NBLK):
    S = cx.S
    outs = []
    NCH = BLK // 128
    with ExitStack() as es:
        def sb(shape, dt):
            return cx.sb(shape, dt, es)

        def sbn(shape, dt, n=2):
            return [cx.sb(shape, dt, es) for _ in range(n)], [Buf() for _ in range(n)]

        W = sb([128, 8, 770], BF16); b_W = Buf()
        S.dma("pool", W[:], d["wh"].rearrange("(k p) n -> p k n", p=128), writes=[b_W])
        wa = sb([128, 128], BF16); b_wa = Buf()
        S.dma("pool", wa[:], d["wa"], writes=[b_wa])
        wx = sb([128, 128], BF16); b_wx = Buf()
        S.dma("pool", wx[:], d["wx"], writes=[b_wx])
        pp = sb([128, 24], F32); b_pp = Buf()
        S.dma("sp", pp[:], d["pp"], writes=[b_pp])
        fv = sb([128, 258], F32); b_fv = Buf()
        S.dma("sp", fv[:], d["fv"].partition_broadcast(128), writes=[b_fv])
        ident = sb([128, 128], BF16); b_id = Buf()
        S.dma("sp", ident[:], d["ident_bf"], writes=[b_id])
        tri = sb([128, 128], F32); b_tri = Buf()
        S.dma("sp", tri[:], d["tri_f"], writes=[b_tri])
        ones_f = sb([128, 128], F32); b_of = Buf()
        S.dma("sp", ones_f[:], d["ones_f"], writes=[b_of])
        ones_bf = sb([128, 128], BF16); b_ob = Buf()
        S.dma("sp", ones_bf[:], d["ones_bf"], writes=[b_ob])
        der = sb([128, 4], F32); b_der = Buf()
        S.op("act", lambda e: e.activation(out=der[:, 3:4], in_=pp[:, 21:22], func=AF.Exp, scale=-1.0), [b_pp], [b_der])
        S.op("act", lambda e: e.activation(out=der[:, 3:4], in_=der[:, 3:4], func=AF.Ln, bias=1.0, scale=1.0), [b_der], [b_der])
        S.op("dve", lambda e: e.tensor_scalar_mul(out=der[:, 0:1], in0=der[:, 3:4], scalar1=-8.0), [b_der], [b_der])
        S.op("dve", lambda e: e.tensor_scalar_mul(out=der[:, 1:2], in0=der[:, 3:4], scalar1=-16.0), [b_der], [b_der])
        S.op("dve", lambda e: e.tensor_scalar_mul(out=der[:, 2:3], in0=pp[:, 22:23], scalar1=float(np.sqrt(128.0))),
             [b_pp, b_der], [b_der])

        dg = {}
        b_dg = Buf()
        for nm, wc in (("q", 0), ("k", 4), ("x", 8)):
            dg[nm] = sb([128, 4, 128], BF16)
            for k in range(4):
                S.op("dve", lambda e: e.tensor_scalar_mul(out=dg[nm][:, k, :], in0=ident[:], scalar1=pp[:, wc + k:wc + k + 1]),
                     [b_id, b_pp], [b_dg])
        C32 = sb([128, 129], F32); b_C32 = Buf()
        Cbf = sb([128, 129], BF16); b_Cbf = Buf()
        S.op("dve", lambda e: e.memset(C32[:], 0.0), [], [b_C32])
        S.op("dve", lambda e: e.memset(Cbf[:], 0.0), [], [b_Cbf])
        hz = sb([128, 1], F32); b_hz = Buf()
        S.op("dve", lambda e: e.memset(hz[:], 0.0), [], [b_hz])

        uTb, b_uTb = sbn([128, 8, BLK], BF16)
        pre = {}
        b_pre = {}
        for nm in ("q", "k", "x"):
            pre[nm], b_pre[nm] = sbn([128, BLK + 8], BF16)
            for i in range(2):
                S.op("pool", lambda e: e.memset(pre[nm][i][:, 0:3], 0.0), [], [b_pre[nm][i]])
        gpre, b_gpre = sbn([128, BLK], F32)
        acc = {}
        b_acc = {}
        for nm in ("x",):
            acc[nm], b_acc[nm] = sbn([128, BLK], F32)
        qT, b_qT = sbn([128, BLK], BF16)
        kT, b_kT = sbn([128, BLK], BF16)
        vo, b_vo = sbn([128, NCH, 256], F32)
        iff, b_if = sbn([128, NCH, 2], F32)
        sm, b_sm = sbn([128, 64], F32)
        vaug, b_vaug = sbn([128, NCH, 144], BF16)
        kc, b_kc = sbn([128, NCH, 128], BF16)
        PT, b_PT = sbn([128, NCH, 128], BF16)
        hm, b_hm = sbn([128, NCH, 128], F32)
        hq, b_hq = sbn([128, NCH, 128], F32)
        so, b_so = sbn([128, NCH, 128], F32)
        ym, b_ym = sbn([128, NCH, 128], BF16)
        tC, b_tC = sbn([128, 129], F32)
        ymT, b_ymT = sbn([128, BLK], BF16)
        yrT, b_yrT = sbn([128, BLK], BF16)
        xcb, b_xcb = sbn([128, BLK], BF16)
        names = ["r", "ig", "a", "a2", "xi", "h", "g2", "sg", "gl", "yr", "sd", "yn"]
        L = {}
        bL = {}
        for nm in names:
            L[nm], bL[nm] = sbn([128, BLK], F32)
        ysq, b_ysq = sbn([128, BLK], BF16)

        pfm = [cx.ps([128, 512], F32, es) for _ in range(2)]
        b_pfm = [Buf(), Buf()]
        pvo = [cx.ps([128, 2, 256], F32, es) for _ in range(2)]
        b_pvo = [Buf(), Buf()]
        pTr = cx.ps([128, 512], F32, es); b_pTr = Buf()
        psm = cx.ps([128, 512], F32, es); b_psm = Buf()
        pST = cx.ps([128, NCH, 128], F32, es); b_pST = Buf()
        pnum = cx.ps([128, NCH, 128], F32, es); b_pnum = Buf()
        p_kc = pTr[:, 0:256].bitcast(BF16).rearrange("p (c n) -> p c n", c=NCH)
        p_ym = pTr[:, 256:512].bitcast(BF16).rearrange("p (c n) -> p c n", c=NCH)
        p_cs = psm[:, 0:8]
        p_if = psm[:, 8:16].rearrange("p (c n) -> p c n", c=NCH)
        p_den = psm[:, 16:20]
        p_upd = psm[:, 32:161]
        fmstate = {"n": 0}

        def fm_bank():
            n = fmstate["n"] % 2
            fmstate["n"] += 1
            return pfm[n], b_pfm[n]

        uT_v = d["uT"].rearrange("(k p) t -> p k t", p=128)

        def proj_pieces(blk):
            i = blk % 2
            t0 = blk * BLK
            pieces = []

            def p_load():
                S.dma("sp", uTb[i][:], uT_v[:, :, t0:t0 + BLK], writes=[b_uTb[i]])
                if blk > 0:
                    for nm in ("q", "k", "x"):
                        S.op("pool", lambda e: e.tensor_copy(out=pre[nm][i][:, 0:3], in_=pre[nm][1 - i][:, BLK:BLK + 3]),
                             [b_pre[nm][1 - i]], [b_pre[nm][i]])

            def p_fm(gi, nm):
                def f():
                    p, bp = fm_bank()
                    for k in range(8):
                        S.op("pe", lambda e: e.matmul(p[:], lhsT=W[:, k, gi * 128:(gi + 1) * 128], rhs=uTb[i][:, k, :],
                                                      start=(k == 0), stop=(k == 7)), [b_W, b_uTb[i]], [bp])
                    if nm == "g":
                        S.op("act", lambda e: e.activation(out=gpre[i][:], in_=p[:], func=AF.Identity,
                                                           bias=pp[:, 18:19], scale=1.0), [b_pp], [bp, b_gpre[i]])
                    else:
                        S.op("act", lambda e: e.activation(out=pre[nm][i][:, 3:BLK + 3], in_=p[:], func=AF.Identity,
                                                           bias=pp[:, 15 + gi:16 + gi], scale=1.0), [b_pp], [bp, b_pre[nm][i]])
                return f

            def p_vo(cp):
                def f():
                    for cc in range(2):
                        c = cp * 2 + cc
                        for k in range(8):
                            S.op("pe", lambda e: e.matmul(pvo[cp][:, cc, :], lhsT=uTb[i][:, k, c * 128:(c + 1) * 128],
                                                          rhs=W[:, k, 512:768], start=(k == 0), stop=(k == 7)),
                                 [b_W, b_uTb[i]], [b_pvo[cp]])
                    S.op("dve", lambda e: e.tensor_tensor(out=vo[i][:, cp * 2:cp * 2 + 2, :], in0=pvo[cp][:],
                                                          in1=fv[:, 0:256].unsqueeze(1).to_broadcast([128, 2, 256]), op=ALU.add),
                         [b_fv], [b_pvo[cp], b_vo[i]])
                return f

            def p_ifproj():
                for c in range(NCH):
                    for k in range(8):
                        S.op("pe", lambda e: e.matmul(p_if[:, c, :], lhsT=uTb[i][:, k, c * 128:(c + 1) * 128],
                                                      rhs=W[:, k, 768:770], start=(k == 0), stop=(k == 7)),
                             [b_W, b_uTb[i]], [b_psm])
                S.op("dve", lambda e: e.tensor_tensor(out=iff[i][:], in0=p_if, in1=fv[:, 256:258].unsqueeze(1).to_broadcast([128, NCH, 2]),
                                                      op=ALU.add), [b_fv], [b_psm, b_if[i]])

            def p_conv(nm, bc):
                def f():
                    p, bp = fm_bank()
                    for k in range(4):
                        S.op("pe", lambda e: e.matmul(p[:], lhsT=dg[nm][:, k, :], rhs=pre[nm][i][:, k:k + BLK],
                                                      start=(k == 0), stop=(k == 3)), [b_dg, b_pre[nm][i]], [bp])
                    if nm == "q":
                        S.op("act", lambda e: e.activation(out=qT[i][:], in_=p[:], func=AF.Silu, bias=pp[:, bc:bc + 1], scale=1.0),
                             [b_pp], [bp, b_qT[i]])
                    elif nm == "k":
                        S.op("act", lambda e: e.activation(out=kT[i][:], in_=p[:], func=AF.Silu, bias=pp[:, bc:bc + 1], scale=1.0),
                             [b_pp], [bp, b_kT[i]])
                    else:
                        S.op("act", lambda e: e.activation(out=acc["x"][i][:], in_=p[:], func=AF.Identity, bias=pp[:, bc:bc + 1], scale=1.0),
                             [b_pp], [bp, b_acc["x"][i]])
                return f

            pieces = [p_load, p_ifproj, p_fm(0, "q"), p_fm(1, "k"), p_fm(2, "x"), p_fm(3, "g"), p_vo(0), p_vo(1),
                      p_conv("q", 12), p_conv("k", 13), p_conv("x", 14)]
            return pieces

        def emit_compute(blk, pieces):
            def pump(n):
                for _ in range(n):
                    if pieces:
                        pieces.pop(0)()
            i = blk % 2
            t0 = blk * BLK
            s_ = sm[i]; bs = b_sm[i]
            bc4 = lambda ap: ap.unsqueeze(2).to_broadcast([128, NCH, 128])
            S.op("act", lambda e: e.activation(out=s_[:, 0:4], in_=iff[i][:, :, 1], func=AF.Exp, scale=-1.0), [b_if[i]], [bs])
            S.op("act", lambda e: e.activation(out=s_[:, 4:8], in_=s_[:, 0:4], func=AF.Ln, bias=1.0, scale=1.0), [bs], [bs])
            S.op("pe", lambda e: e.matmul(p_cs[:, 0:4], lhsT=tri[:], rhs=s_[:, 4:8], start=True, stop=True), [b_tri, bs], [b_psm])
            S.op("pe", lambda e: e.matmul(p_cs[:, 4:8], lhsT=ones_f[:], rhs=s_[:, 4:8], start=True, stop=True), [b_of, bs], [b_psm])
            S.op("dve", lambda e: e.tensor_tensor(out=s_[:, 8:12], in0=iff[i][:, :, 0], in1=p_cs[:, 0:4], op=ALU.add),
                 [b_if[i]], [b_psm, bs])
            S.op("act", lambda e: e.activation(out=s_[:, 12:16], in_=s_[:, 8:12], func=AF.Exp, bias=LN_INV_SQRT_DH, scale=1.0), [bs], [bs])
            S.op("act", lambda e: e.activation(out=s_[:, 16:24], in_=p_cs, func=AF.Exp, scale=-1.0), [], [b_psm, bs])
            pump(3)
            ws = s_[:, 12:16]
            eb = s_[:, 16:20]
            S.op("dve", lambda e: e.tensor_tensor(out=vaug[i][:, :, 0:128], in0=vo[i][:, :, 0:128], in1=bc4(ws), op=ALU.mult),
                 [b_vo[i], bs], [b_vaug[i]])
            S.op("dve", lambda e: e.tensor_copy(out=vaug[i][:, :, 128], in_=ws), [bs], [b_vaug[i]])
            for c in range(NCH):
                S.op("pe", lambda e: e.transpose(p_kc[:, c, :], kT[i][:, c * 128:(c + 1) * 128], ident[:]), [b_kT[i], b_id], [b_pTr])
            S.op("act", lambda e: e.activation(out=kc[i][:], in_=p_kc, func=AF.Copy), [], [b_pTr, b_kc[i]])
            for c in range(NCH):
                cs = slice(c * 128, (c + 1) * 128)
                S.op("pe", lambda e: e.matmul(pST[:, c, :], lhsT=kT[i][:, cs], rhs=qT[i][:, cs], start=True, stop=True),
                     [b_kT[i], b_qT[i]], [b_pST])
            S.op("dve", lambda e: e.tensor_tensor(out=PT[i][:], in0=pST[:], in1=tri[:].unsqueeze(1).to_broadcast([128, NCH, 128]),
                                                  op=ALU.mult), [b_tri], [b_pST, b_PT[i]])
            for c in range(NCH):
                pump(1)
                cs = slice(c * 128, (c + 1) * 128)
                S.op("pe", lambda e: e.matmul(pnum[:, c, :], lhsT=PT[i][:, c, :], rhs=vaug[i][:, c, 0:128], start=True, stop=False),
                     [b_PT[i], b_vaug[i]], [b_pnum])
                S.op("pe", lambda e: e.matmul(pnum[:, c, :], lhsT=qT[i][:, cs], rhs=Cbf[:, 0:128], start=False, stop=True),
                     [b_qT[i], b_Cbf], [b_pnum])
                S.op("pe", lambda e: e.matmul(p_den[:, c:c + 1], lhsT=PT[i][:, c, :], rhs=vaug[i][:, c, 128:129], start=True, stop=False),
                     [b_PT[i], b_vaug[i]], [b_psm])
                S.op("pe", lambda e: e.matmul(p_den[:, c:c + 1], lhsT=qT[i][:, cs], rhs=Cbf[:, 128:129], start=False, stop=True),
                     [b_qT[i], b_Cbf], [b_psm])
                S.op("pe", lambda e: e.matmul(p_upd, lhsT=kc[i][:, c, :], rhs=vaug[i][:, c, 0:129], start=True, stop=True),
                     [b_kc[i], b_vaug[i]], [b_psm])
                j = c % 2
                S.op("dve", lambda e: e.tensor_tensor(out=tC[j][:], in0=p_upd, in1=C32[:], op=ALU.add), [b_C32], [b_psm, b_tC[j]])
                S.op("dve", lambda e: e.tensor_tensor(out=C32[:], in0=tC[j][:], in1=s_[:, 20 + c:21 + c].to_broadcast([128, 129]), op=ALU.mult),
                     [b_tC[j], bs], [b_C32])
                S.op("act", lambda e: e.activation(out=Cbf[:], in_=tC[j][:], func=AF.Copy, scale=s_[:, 20 + c:21 + c]),
                     [b_tC[j], bs], [b_Cbf])
            pump(1)
            S.op("dve", lambda e: e.tensor_tensor(out=s_[:, 24:28], in0=p_den, in1=eb, op=ALU.mult), [], [b_psm, bs])
            S.op("act", lambda e: e.activation(out=s_[:, 24:28], in_=s_[:, 24:28], func=AF.Abs), [bs], [bs])
            S.op("dve", lambda e: e.tensor_scalar_max(out=s_[:, 24:28], in0=s_[:, 24:28], scalar1=1.0), [bs], [bs])
            S.op("dve", lambda e: e.reciprocal(out=s_[:, 28:32], in_=s_[:, 24:28]), [bs], [bs])
            S.op("dve", lambda e: e.tensor_tensor(out=s_[:, 28:32], in0=s_[:, 28:32], in1=eb, op=ALU.mult), [bs], [bs])
            S.op("dve", lambda e: e.tensor_tensor(out=hm[i][:], in0=pnum[:], in1=bc4(s_[:, 28:32]), op=ALU.mult), [bs], [b_pnum, b_hm[i]])
            S.op("dve", lambda e: e.tensor_reduce(out=s_[:, 32:36], in_=hm[i][:], axis=AX.X, op=ALU.add), [b_hm[i]], [bs])
            S.op("act", lambda e: e.activation(out=hq[i][:], in_=hm[i][:], func=AF.Square), [b_hm[i]], [b_hq[i]])
            S.op("dve", lambda e: e.tensor_reduce(out=s_[:, 36:40], in_=hq[i][:], axis=AX.X, op=ALU.add), [b_hq[i]], [bs])
            S.op("dve", lambda e: e.tensor_scalar_mul(out=s_[:, 32:36], in0=s_[:, 32:36], scalar1=1.0 / 128.0), [bs], [bs])
            S.op("dve", lambda e: e.tensor_tensor(out=s_[:, 40:44], in0=s_[:, 32:36], in1=s_[:, 32:36], op=ALU.mult), [bs], [bs])
            S.op("dve", lambda e: e.scalar_tensor_tensor(out=s_[:, 36:40], in0=s_[:, 36:40], scalar=1.0 / 128.0, in1=s_[:, 40:44],
                                                         op0=ALU.mult, op1=ALU.subtract), [bs], [bs])
            S.op("act", lambda e: e.activation(out=s_[:, 36:40], in_=s_[:, 36:40], func=AF.Ln, bias=EPS, scale=1.0), [bs], [bs])
            S.op("act", lambda e: e.activation(out=s_[:, 36:40], in_=s_[:, 36:40], func=AF.Exp, scale=-0.5), [bs], [bs])
            S.op("dve", lambda e: e.tensor_tensor(out=hm[i][:], in0=hm[i][:], in1=bc4(s_[:, 32:36]), op=ALU.subtract), [bs], [b_hm[i]])
            S.op("dve", lambda e: e.tensor_tensor(out=hm[i][:], in0=hm[i][:], in1=bc4(s_[:, 36:40]), op=ALU.mult), [bs], [b_hm[i]])
            S.op("act", lambda e: e.activation(out=so[i][:], in_=vo[i][:, :, 128:256], func=AF.Sigmoid), [b_vo[i]], [b_so[i]])
            S.op("dve", lambda e: e.tensor_tensor(out=ym[i][:], in0=hm[i][:], in1=so[i][:], op=ALU.mult), [b_hm[i], b_so[i]], [b_ym[i]])
            pump(1)
            for c in range(NCH):
                S.op("pe", lambda e: e.transpose(p_ym[:, c, :], ym[i][:, c, :], ident[:]), [b_ym[i], b_id], [b_pTr])
            S.op("act", lambda e: e.activation(out=ymT[i][:].rearrange("p (c n) -> p c n", c=NCH), in_=p_ym, func=AF.Copy,
                                               scale=pp[:, 23:24]), [b_pp], [b_pTr, b_ymT[i]])
            outs.append(Buf())
            S.dma("sp", d["yT"][0:128, t0:t0 + BLK], ymT[i][:], reads=[b_ymT[i]], writes=[outs[-1]])

            xc = acc["x"][i]; b_xc = b_acc["x"][i]
            S.op("act", lambda e: e.activation(out=xcb[i][:], in_=xc[:], func=AF.Copy), [b_xc], [b_xcb[i]])
            p, bp = fm_bank()
            S.op("pe", lambda e: e.matmul(p[:], lhsT=wa[:], rhs=xcb[i][:], start=True, stop=True), [b_wa, b_xcb[i]], [bp])
            S.op("act", lambda e: e.activation(out=L["r"][i][:], in_=p[:], func=AF.Sigmoid, bias=pp[:, 19:20], scale=1.0),
                 [b_pp], [bp, bL["r"][i]])
            p, bp = fm_bank()
            S.op("pe", lambda e: e.matmul(p[:], lhsT=wx[:], rhs=xcb[i][:], start=True, stop=True), [b_wx, b_xcb[i]], [bp])
            S.op("act", lambda e: e.activation(out=L["ig"][i][:], in_=p[:], func=AF.Sigmoid, bias=pp[:, 20:21], scale=1.0),
                 [b_pp], [bp, bL["ig"][i]])
            g = gpre[i]; bg = b_gpre[i]
            S.op("pool", lambda e: e.tensor_tensor(out=L["g2"][i][:], in0=g[:], in1=g[:], op=ALU.mult), [bg], [bL["g2"][i]])
            S.op("pool", lambda e: e.tensor_scalar(out=L["g2"][i][:], in0=L["g2"][i][:], scalar1=0.044715, scalar2=1.0,
                                                   op0=ALU.mult, op1=ALU.add), [], [bL["g2"][i]])
            S.op("pool", lambda e: e.tensor_tensor(out=L["g2"][i][:], in0=L["g2"][i][:], in1=g[:], op=ALU.mult), [bg], [bL["g2"][i]])
            S.op("act", lambda e: e.activation(out=L["sg"][i][:], in_=L["g2"][i][:], func=AF.Sigmoid, scale=GELU_C),
                 [bL["g2"][i]], [bL["sg"][i]])
            S.op("act", lambda e: e.activation(out=L["a"][i][:], in_=L["r"][i][:], func=AF.Exp, scale=der[:, 0:1]),
                 [bL["r"][i], b_der], [bL["a"][i]])
            S.op("act", lambda e: e.activation(out=L["a2"][i][:], in_=L["r"][i][:], func=AF.Exp, scale=der[:, 1:2]),
                 [bL["r"][i], b_der], [bL["a2"][i]])
            S.op("act", lambda e: e.activation(out=L["a2"][i][:], in_=L["a2"][i][:], func=AF.Ln, bias=1.0, scale=-1.0),
                 [bL["a2"][i]], [bL["a2"][i]])
            S.op("act", lambda e: e.activation(out=L["a2"][i][:], in_=L["a2"][i][:], func=AF.Exp, scale=0.5),
                 [bL["a2"][i]], [bL["a2"][i]])
            S.op("pool", lambda e: e.tensor_tensor(out=L["xi"][i][:], in0=L["ig"][i][:], in1=xc[:], op=ALU.mult),
                 [bL["ig"][i], b_xc], [bL["xi"][i]])
            S.op("dve", lambda e: e.tensor_tensor(out=L["xi"][i][:], in0=L["xi"][i][:], in1=L["a2"][i][:], op=ALU.mult),
                 [bL["a2"][i]], [bL["xi"][i]])
            init = hz[:, 0:1] if blk == 0 else L["h"][1 - i][:, BLK - 1:BLK]
            b_init = b_hz if blk == 0 else bL["h"][1 - i]
            S.op("dve", lambda e: e.tensor_tensor_scan(out=L["h"][i][:], data0=L["a"][i][:], data1=L["xi"][i][:], initial=init,
                                                       op0=ALU.mult, op1=ALU.add),
                 [bL["a"][i], bL["xi"][i], b_init], [bL["h"][i]])
            g = gpre[i]; bg = b_gpre[i]
            S.op("pool", lambda e: e.tensor_tensor(out=L["gl"][i][:], in0=L["sg"][i][:], in1=g[:], op=ALU.mult),
                 [bL["sg"][i], bg], [bL["gl"][i]])
            S.op("dve", lambda e: e.tensor_tensor(out=L["yr"][i][:], in0=L["h"][i][:], in1=L["gl"][i][:], op=ALU.mult),
                 [bL["h"][i], bL["gl"][i]], [bL["yr"][i]])
            S.op("act", lambda e: e.activation(out=ysq[i][:], in_=L["yr"][i][:], func=AF.Square), [bL["yr"][i]], [b_ysq[i]])
            p, bp = fm_bank()
            S.op("pe", lambda e: e.matmul(p[:], lhsT=ones_bf[:], rhs=ysq[i][:], start=True, stop=True), [b_ob, b_ysq[i]], [bp])
            S.op("act", lambda e: e.activation(out=L["sd"][i][:], in_=p[:], func=AF.Ln, bias=128.0 * EPS, scale=1.0),
                 [], [bp, bL["sd"][i]])
            S.op("act", lambda e: e.activation(out=L["sd"][i][:], in_=L["sd"][i][:], func=AF.Exp, scale=-0.5), [], [bL["sd"][i]])
            S.op("pool", lambda e: e.tensor_tensor(out=L["yn"][i][:], in0=L["yr"][i][:], in1=L["sd"][i][:], op=ALU.mult),
                 [bL["yr"][i], bL["sd"][i]], [bL["yn"][i]])
            S.op("act", lambda e: e.activation(out=yrT[i][:], in_=L["yn"][i][:], func=AF.Copy, scale=der[:, 2:3]),
                 [bL["yn"][i], b_der], [b_yrT[i]])
            outs.append(Buf())
            S.dma("sp", d["yT"][128:256, t0:t0 + BLK], yrT[i][:], reads=[b_yrT[i]], writes=[outs[-1]])
            pump(len(pieces))

        for f in proj_pieces(0):
            f()
        for blk in range(nblk):
            emit_compute(blk, proj_pieces(blk + 1) if blk + 1 < nblk else [])
        S.barrier()
    return outs


def pack_B_inputs(inp, l, h, uT_full, hc):
    w_in = inp["w_in"][l]
    b_in = inp["b_in"][l]
    hs = slice(h * 128, (h + 1) * 128)
    o_q, o_k, o_v, o_o, o_i, o_f, o_x, o_g = 0, 1024, 2048, 3072, 4096, 4104, 4112, 5136
    cols = np.concatenate([np.arange(o_q + h * 128, o_q + (h + 1) * 128), np.arange(o_k + h * 128, o_k + (h + 1) * 128),
                           np.arange(o_x + h * 128, o_x + (h + 1) * 128), np.arange(o_g + h * 128, o_g + (h + 1) * 128),
                           np.arange(o_v + h * 128, o_v + (h + 1) * 128), np.arange(o_o + h * 128, o_o + (h + 1) * 128),
                           np.array([o_i + h, o_f + h])])
    wh = np.ascontiguousarray(w_in[:, cols])
    bh = b_in[cols]
    pp = np.zeros((128, 24), np.float32)
    pp[:, 0:4] = inp["w_conv_m"][l][:, hs].T
    pp[:, 4:8] = inp["w_conv_m"][l][:, 1024 + h * 128:1024 + (h + 1) * 128].T
    pp[:, 8:12] = inp["w_conv_r"][l][:, hs].T
    pp[:, 12] = inp["b_conv_m"][l][hs]
    pp[:, 13] = inp["b_conv_m"][l][1024 + h * 128:1024 + (h + 1) * 128]
    pp[:, 14] = inp["b_conv_r"][l][hs]
    pp[:, 15] = bh[0:128]
    pp[:, 16] = bh[128:256]
    pp[:, 17] = bh[256:384]
    pp[:, 18] = bh[384:512]
    pp[:, 19] = inp["b_a"][l][hs]
    pp[:, 20] = inp["b_x"][l][hs]
    pp[:, 21] = inp["lru_lambda"][l][hs]
    pp[:, 22] = inp["lru_norm_g"][l][hs]
    pp[:, 23] = inp["mh_norm_g"][l][hs]
    fv = np.ascontiguousarray(bh[512:770][None, :])
    return {"uT": uT_full, "wh": wh, "pp": pp, "fv": fv,
            "wa": np.ascontiguousarray(inp["w_a"][l][h]), "wx": np.ascontiguousarray(inp["w_x"][l][h]),
            "ident_bf": hc["ident_bf"], "tri_f": hc["tri_f"], "ones_f": hc["ones_f"], "ones_bf": hc["ones_bf"]}


NSLOT = NE * CAP
BIGROW = float(NSLOT + 64)


def build_C(debug=False):
    nc = bass.Bass("TRN2", target_bir_lowering=False)
    with ExitStack() as es:
        cx = Ctx(nc, es)
        S = cx.S
        d = {
            "x": cx.dram_in("x", [TPC, D], F32),
            "yT": cx.dram_in("yT", [2 * D, TPC], BF16),
            "ada": cx.dram_in("ada", [1, 6 * D], F32),
            "w_out": cx.dram_in("w_out", [2 * D, D], F32),
            "lnp": cx.dram_in("lnp", [1, 4 * D], F32),
            "w_router": cx.dram_in("w_router", [D, NE], F32),
            "b_router": cx.dram_in("b_router", [1, NE], F32),
            "eoff": cx.dram_in("eoff", [1, NE], F32),
            "w_gate": cx.dram_in("w_gate", [NE, D, DFF], F32),
            "w_up": cx.dram_in("w_up", [NE, D, DFF], F32),
            "w_down": cx.dram_in("w_down", [NE, DFF, D], F32),
            "ident_bf": cx.dram_in("ident_bf", [128, 128], BF16),
            "ident_f": cx.dram_in("ident_f", [128, 128], F32),
            "tris_bf": cx.dram_in("tris_bf", [128, 128], BF16),
            "ones_bf": cx.dram_in("ones_bf", [128, 128], BF16),
            "xout": cx.dram_out("xout", [TPC, D], F32),
        }
        if debug:
            d["xbuf"] = nc.dram_tensor("xbuf", [NSLOT, D], BF16, kind="ExternalOutput")
            d["ybuf"] = nc.dram_tensor("ybuf", [NSLOT, D], F32, kind="ExternalOutput")
            d["dbg_dest"] = cx.dram_out("dbg_dest", [128, NT * 2], I32)
            d["dbg_gate"] = cx.dram_out("dbg_gate", [128, NT, 2], F32)
        else:
            d["xbuf"] = nc.dram_tensor("xbuf", [NSLOT, D], BF16)
            d["ybuf"] = nc.dram_tensor("ybuf", [NSLOT, D], F32)
        adac = cx.sb([128, 4, D], F32)
        b_adac = Buf()
        S.dma("sp", adac[:], d["ada"][:, 2 * D:6 * D].partition_broadcast(128).rearrange("p o (a n) -> p (o a) n", a=4),
              writes=[b_adac])
        S.op("dve", lambda e: e.tensor_scalar_add(out=adac[:, 2, :], in0=adac[:, 2, :], scalar1=1.0), [b_adac], [b_adac])
        d["x1s"] = nc.dram_tensor("x1s", [TPC, D], F32)
        outs = emit_C(cx, d, d["x"], None, None, adac[:, 0, :], adac[:, 1, :], adac[:, 2, :], adac[:, 3, :], b_adac,
                      d["xout"])
        S.finish(outs, "sp")
    return nc


def emit_C(cx, d, x_d, xres, b_xres, g1_ap, sh2_ap, sc2_ap, g2_ap, b_ada, xout_d):
    S = cx.S
    nc = cx.nc
    outs = []
    with ExitStack() as es0:
        ident = cx.sb([128, 128], BF16, es0); b_id = Buf()
        S.dma("sp", ident[:], d["ident_bf"], writes=[b_id])
        scat = []
        bc_reg = nc.gpsimd.alloc_register(f"bc{cx._n}")
        nc.gpsimd.reg_mov(bc_reg, NSLOT - 1)
        zt = cx.sb([128, D], BF16, es0); b_zt = Buf()
        S.op("pool", lambda e: e.memset(zt[:], 0.0), [], [b_zt])
        zfill = []
        for r0 in range(0, NSLOT, 1024):
            zfill.append(Buf())
            S.dma("sp" if (r0 // 1024) % 2 == 0 else "act",
                  d["xbuf"][r0:r0 + 1024, :].rearrange("(p s) n -> p s n", p=128),
                  zt[:].unsqueeze(1).to_broadcast([128, 8, D]), reads=[b_zt], writes=[zfill[-1]])
        NG = NT * NE
        destS = cx.sb([128, 2 * NT], I32, es0)
        destG = cx.sb([128, 2 * NT], I32, es0)
        gate2 = cx.sb([128, NT, 2], F32, es0)
        b_rt = Buf()
        u2b_all = cx.sb([128, NT, D], BF16, es0)
        b_u2b = [Buf() for _ in range(NT)]
        x1w = []
        with ExitStack() as es:
            def sb(shape, dt):
                return cx.sb(shape, dt, es)

            def sbn(shape, dt, n=2):
                return [cx.sb(shape, dt, es) for _ in range(n)], [Buf() for _ in range(n)]

            wout = sb([128, 16, D], BF16); b_wout = Buf()
            wo_v = d["w_out"].rearrange("(m p) n -> p m n", p=128)
            for q4 in range(4):
                S.dma("pool", wout[:, q4 * 4:(q4 + 1) * 4, :], wo_v[:, q4 * 4:(q4 + 1) * 4, :], writes=[b_wout])
            lnp = sb([128, 2, D], F32); b_lnp = Buf()
            S.dma("sp", lnp[:], d["lnp"][:, 0:2 * D].partition_broadcast(128).rearrange("p o (a n) -> p (o a) n", a=2), writes=[b_lnp])
            identf = sb([128, 128], F32); b_idf = Buf()
            S.dma("sp", identf[:], d["ident_f"], writes=[b_idf])
            tris = sb([128, 128], BF16); b_tris = Buf()
            S.dma("sp", tris[:], d["tris_bf"], writes=[b_tris])
            ones = sb([128, 128], BF16); b_ones = Buf()
            S.dma("sp", ones[:], d["ones_bf"], writes=[b_ones])
            wr = sb([128, 8, NE], F32); b_wr = Buf()
            S.dma("sp", wr[:], d["w_router"].rearrange("(k p) n -> p k n", p=128), writes=[b_wr])
            br = sb([128, NE], F32); b_br = Buf()
            S.dma("sp", br[:], d["b_router"].partition_broadcast(128), writes=[b_br])
            eoff = sb([128, NE], F32); b_eoff = Buf()
            S.dma("sp", eoff[:], d["eoff"].partition_broadcast(128), writes=[b_eoff])

            ytb, b_ytb = sbn([128, 16, 256], BF16)
            xin, b_xin = sbn([128, D], F32)
            t1_, b_t1_ = sbn([128, D], F32)
            z_, b_z_ = sbn([128, D], F32)
            x1t_, b_x1t_ = sbn([128, D], F32)
            u2f_, b_u2f_ = sbn([128, D], F32)
            u2T_, b_u2T_ = sbn([128, 8, 128], F32)
            xnA = sb([128, D], F32); b_xnA = Buf()
            tmpA = sb([128, D], F32); b_tmpA = Buf()
            xnB = sb([128, D], F32); b_xnB = Buf()
            tmpB = sb([128, D], F32); b_tmpB = Buf()
            lnsA = LNScratch(cx, es)
            lnsB = LNScratch(cx, es)
            aff_all = sb([128, NT, NE], F32)
            b_aff = [Buf() for _ in range(NT)]

            po = [cx.ps([128, 512], F32, es) for _ in range(2)]
            b_po = [Buf(), Buf()]
            pT = [cx.ps([128, 4, 128], F32, es) for _ in range(2)]
            b_pT = [Buf(), Buf()]
            plog = cx.ps([128, 512], F32, es); b_plog = Buf()
            ppre = cx.ps([128, 512], F32, es); b_ppre = Buf()
            ptot = cx.ps([128, 512], F32, es); b_ptot = Buf()

            yT_v = d["yT"].rearrange("(m p) t -> p m t", p=128)

            logit_all = sb([128, NT, NE], F32)

            def S1(t):
                i = t % 2
                if t % 2 == 0:
                    yi = (t // 2) % 2
                    S.dma("sp", ytb[yi][:], yT_v[:, :, t * 128:t * 128 + 256], writes=[b_ytb[yi]])
                yi = (t // 2) % 2
                tsl = slice((t % 2) * 128, (t % 2) * 128 + 128)
                S.dma("act", xin[i][:], x_d[t * 128:(t + 1) * 128, :], writes=[b_xin[i]])
                for half in range(2):
                    hs = slice(half * 512, (half + 1) * 512)
                    for m in range(16):
                        S.op("pe", lambda e: e.matmul(po[half][:], lhsT=ytb[yi][:, m, tsl], rhs=wout[:, m, hs],
                                                      start=(m == 0), stop=(m == 15)), [b_ytb[yi], b_wout], [b_po[half]])
                        if m % 4 == 3:
                            yield
                    S.op("dve", lambda e: e.tensor_tensor(out=t1_[i][:, hs], in0=po[half][:], in1=g1_ap[:, hs], op=ALU.mult),
                         [b_ada], [b_po[half], b_t1_[i]])
                    yield
                S.op("dve", lambda e: e.scalar_tensor_tensor(out=z_[i][:], in0=xin[i][:], scalar=ALPHA, in1=t1_[i][:],
                                                             op0=ALU.mult, op1=ALU.add), [b_xin[i], b_t1_[i]], [b_z_[i]])
                yield

            def S2(t):
                i = t % 2
                yield from g_ln_mod(cx, z_[i][:], b_z_[i], lnsA, xnA, b_xnA, lnp[:, 0, :], lnp[:, 1, :], b_lnp, x1t_[i][:], b_x1t_[i], tmpA, b_tmpA)
                x1w.append(Buf())
                S.dma("act", d["x1s"][t * 128:(t + 1) * 128, :], x1t_[i][:], reads=[b_x1t_[i]], writes=[x1w[-1]])
                yield

            def S3(t):
                i = t % 2
                yield from g_ln_mod(cx, x1t_[i][:], b_x1t_[i], lnsB, xnB, b_xnB, sc2_ap, sh2_ap, b_ada, u2f_[i][:], b_u2f_[i], tmpB, b_tmpB)
                S.op("act", lambda e: e.activation(out=u2b_all[:, t, :], in_=u2f_[i][:], func=AF.Copy), [b_u2f_[i]], [b_u2b[t]])
                yield

            def S4(t):
                i = t % 2
                u2f, b_u2f, u2T, b_u2T = u2f_[i], b_u2f_[i], u2T_[i], b_u2T_[i]
                for k in range(8):
                    S.op("pe", lambda e: e.transpose(pT[k // 4][:, k % 4, :], u2f[:, k * 128:(k + 1) * 128], identf[:]),
                         [b_u2f, b_idf], [b_pT[k // 4]])
                    if k % 4 == 3:
                        yield
                S.op("act", lambda e: e.activation(out=u2T[:, 0:4, :], in_=pT[0][:], func=AF.Copy), [], [b_pT[0], b_u2T])
                yield
                S.op("dve", lambda e: e.tensor_copy(out=u2T[:, 4:8, :], in_=pT[1][:]), [], [b_pT[1], b_u2T])
                yield
                for k in range(8):
                    S.op("pe", lambda e: e.matmul(plog[:, 0:NE], lhsT=u2T[:, k, :], rhs=wr[:, k, :], start=(k == 0), stop=(k == 7)),
                         [b_u2T, b_wr], [b_plog])
                yield
                S.op("dve", lambda e: e.tensor_copy(out=logit_all[:, t, :], in_=plog[:, 0:NE]), [], [b_plog, b_aff[t]])
                yield

            for kk in range(NT + 3):
                round_robin([S1(kk) if kk < NT else None,
                             S2(kk - 1) if 0 <= kk - 1 < NT else None,
                             S3(kk - 2) if 0 <= kk - 2 < NT else None,
                             S4(kk - 3) if 0 <= kk - 3 < NT else None])
            b_affall = Buf()
            S.op("act", lambda e: e.activation(out=aff_all[:], in_=logit_all[:], func=AF.Sigmoid), b_aff, [b_affall])

            RR = sb([128, 12, NG], F32); b_R = Buf()
            aff = aff_all[:].rearrange("p t e -> p (t e)")
            sel, eq, selm, ge, msk, gv, pos, val, vld, dd, cntf = (RR[:, n, :] for n in range(11))
            q3 = lambda ap: ap.rearrange("p (a j) -> p a j", j=4)
            t3 = lambda ap: ap.rearrange("p (t e) -> p t e", e=NE)
            r128 = sb([128, 4, NT * 8], F32)
            m1, m2, gsc, gone = (r128[:, n, :] for n in range(4))
            r16 = sb([128, 8, NT], F32)
            gmax, gsum, rgs, first, second, g1s, gts = (r16[:, n, :] for n in range(7))
            mk = sb([128, NG], BF16); b_mk = Buf()
            S.op("dve", lambda e: e.tensor_tensor(out=t3(sel), in0=aff_all[:], in1=br[:].unsqueeze(1).to_broadcast([128, NT, NE]), op=ALU.add),
                 [b_affall, b_br], [b_R])
            S.op("dve", lambda e: e.tensor_reduce(out=m1, in_=q3(sel), axis=AX.X, op=ALU.max), [], [b_R])
            S.op("dve", lambda e: e.tensor_tensor(out=q3(eq), in0=q3(sel), in1=m1.unsqueeze(2).to_broadcast([128, NT * 8, 4]), op=ALU.is_equal), [], [b_R])
            S.op("dve", lambda e: e.scalar_tensor_tensor(out=selm, in0=eq, scalar=-1e9, in1=sel, op0=ALU.mult, op1=ALU.add), [], [b_R])
            S.op("dve", lambda e: e.tensor_reduce(out=m2, in_=q3(selm), axis=AX.X, op=ALU.max), [], [b_R])
            S.op("dve", lambda e: e.tensor_tensor(out=gsc, in0=m1, in1=m2, op=ALU.add), [], [b_R])
            g8 = lambda ap: ap.rearrange("p (t g) -> p t g", g=8)
            S.op("dve", lambda e: e.tensor_reduce(out=gmax, in_=g8(gsc), axis=AX.X, op=ALU.max), [], [b_R])
            S.op("dve", lambda e: e.tensor_tensor(out=g8(gone), in0=g8(gsc), in1=gmax.unsqueeze(2).to_broadcast([128, NT, 8]), op=ALU.is_equal), [], [b_R])
            S.op("dve", lambda e: e.tensor_tensor(out=q3(ge), in0=q3(sel), in1=m2.unsqueeze(2).to_broadcast([128, NT * 8, 4]), op=ALU.is_ge), [], [b_R])
            S.op("dve", lambda e: e.tensor_tensor(out=q3(msk), in0=q3(ge), in1=gone.unsqueeze(2).to_broadcast([128, NT * 8, 4]), op=ALU.mult), [], [b_R])
            S.op("dve", lambda e: e.tensor_tensor(out=gv, in0=aff, in1=msk, op=ALU.mult), [b_affall], [b_R])
            S.op("dve", lambda e: e.tensor_reduce(out=gsum, in_=t3(gv), axis=AX.X, op=ALU.add), [], [b_R])
            S.op("dve", lambda e: e.reciprocal(out=rgs, in_=gsum), [], [b_R])
            S.op("dve", lambda e: e.tensor_tensor(out=t3(gv), in0=t3(gv), in1=rgs.unsqueeze(2).to_broadcast([128, NT, NE]), op=ALU.mult), [], [b_R])
            S.op("dve", lambda e: e.tensor_copy(out=mk[:], in_=msk), [b_R], [b_mk])
            S.op("pe", lambda e: e.matmul(ppre[:], lhsT=tris[:], rhs=mk[:], start=True, stop=True), [b_tris, b_mk], [b_ppre])
            S.op("pe", lambda e: e.matmul(ptot[:], lhsT=ones[:], rhs=mk[:], start=True, stop=True), [b_ones, b_mk], [b_ptot])
            S.op("dve", lambda e: e.tensor_copy(out=dd, in_=ptot[:]), [], [b_ptot, b_R])
            S.op("dve", lambda e: e.memset(cntf[:, 0:NE], 0.0), [], [b_R])
            for t in range(1, NT):
                S.op("dve", lambda e: e.tensor_tensor(out=cntf[:, t * NE:(t + 1) * NE], in0=cntf[:, (t - 1) * NE:t * NE],
                                                      in1=dd[:, (t - 1) * NE:t * NE], op=ALU.add), [], [b_R])
            S.op("dve", lambda e: e.tensor_tensor(out=pos, in0=ppre[:], in1=cntf, op=ALU.add), [], [b_ppre, b_R])
            S.op("dve", lambda e: e.tensor_scalar(out=vld, in0=pos, scalar1=float(CAP) - 0.5, scalar2=None, op0=ALU.is_lt), [], [b_R])
            S.op("dve", lambda e: e.tensor_tensor(out=vld, in0=vld, in1=msk, op=ALU.mult), [], [b_R])
            S.op("dve", lambda e: e.tensor_tensor(out=t3(val), in0=t3(pos), in1=eoff[:].unsqueeze(1).to_broadcast([128, NT, NE]), op=ALU.add),
                 [b_eoff], [b_R])
            S.op("dve", lambda e: e.scalar_tensor_tensor(out=dd, in0=val, scalar=1.0, in1=vld, op0=ALU.add, op1=ALU.mult), [], [b_R])
            S.op("dve", lambda e: e.tensor_scalar_add(out=dd, in0=dd, scalar1=-1.0), [], [b_R])
            S.op("dve", lambda e: e.tensor_reduce(out=first, in_=t3(dd), axis=AX.X, op=ALU.max), [], [b_R])
            S.op("dve", lambda e: e.tensor_tensor(out=t3(eq), in0=t3(dd), in1=first.unsqueeze(2).to_broadcast([128, NT, NE]), op=ALU.is_equal), [], [b_R])
            S.op("dve", lambda e: e.scalar_tensor_tensor(out=selm, in0=eq, scalar=-1e9, in1=dd, op0=ALU.mult, op1=ALU.add), [], [b_R])
            S.op("dve", lambda e: e.tensor_reduce(out=second, in_=t3(selm), axis=AX.X, op=ALU.max), [], [b_R])
            S.op("dve", lambda e: e.tensor_tensor(out=gv, in0=gv, in1=vld, op=ALU.mult), [], [b_R])
            S.op("dve", lambda e: e.tensor_reduce(out=gts, in_=t3(gv), axis=AX.X, op=ALU.add), [], [b_R])
            S.op("dve", lambda e: e.tensor_tensor(out=ge, in0=gv, in1=eq, op=ALU.mult), [], [b_R])
            S.op("dve", lambda e: e.tensor_reduce(out=g1s, in_=t3(ge), axis=AX.X, op=ALU.add), [], [b_R])
            S.op("dve", lambda e: e.tensor_copy(out=gate2[:, :, 0], in_=g1s), [b_R], [b_rt])
            S.op("dve", lambda e: e.tensor_tensor(out=gate2[:, :, 1], in0=gts, in1=g1s, op=ALU.subtract), [b_R], [b_rt])
            fs = r16[:, 3:5, :]
            neg = r16[:, 5:7, :]
            dS = destS[:].rearrange("p (t s) -> p s t", s=2)
            dG = destG[:].rearrange("p (t s) -> p s t", s=2)
            S.op("dve", lambda e: e.tensor_scalar(out=neg, in0=fs, scalar1=0.0, scalar2=BIGROW + 1.0, op0=ALU.is_lt, op1=ALU.mult), [b_rt], [b_R])
            S.op("dve", lambda e: e.tensor_tensor(out=neg, in0=neg, in1=fs, op=ALU.add), [], [b_R])
            S.op("dve", lambda e: e.tensor_copy(out=dS, in_=neg), [b_R], [b_rt])
            S.op("dve", lambda e: e.tensor_scalar_max(out=neg, in0=fs, scalar1=0.0), [b_rt], [b_R])
            S.op("dve", lambda e: e.tensor_copy(out=dG, in_=neg), [b_R], [b_rt])
            for t in range(NT):
                for sidx in range(2):
                    scat.append(Buf())
                    S.dma_fn("pool", lambda e: e.indirect_dma_start(
                        out=d["xbuf"][:, :], out_offset=bass.IndirectOffsetOnAxis(ap=destS[:, 2 * t + sidx:2 * t + sidx + 1], axis=0),
                        in_=u2b_all[:, t, :], in_offset=None, bounds_check=bc_reg, oob_is_err=False),
                        [b_u2b[t], b_rt] + zfill, [scat[-1]])
        S.barrier()
        if "dbg_dest" in d:
            for nm, src in (("dbg_dest", destS), ("dbg_gate", gate2)):
                outs.append(Buf())
                S.dma("sp", d[nm], src[:], reads=[b_rt], writes=[outs[-1]])
        ysc = []
        with ExitStack() as es:
            def sbn(shape, dt, n=2):
                return [cx.sb(shape, dt, es) for _ in range(n)], [Buf() for _ in range(n)]

            wg, b_wg = sbn([128, 8, DFF], BF16)
            wu, b_wu = sbn([128, 8, DFF], BF16)
            wd, b_wd = sbn([128, 4, D], BF16)
            Xe, b_Xe = sbn([128, CAP // 128, D], BF16)
            XT, b_XT = sbn([128, 8, CAP], BF16)
            hT, b_hT = sbn([128, 4, CAP], BF16)
            sg, b_sg = sbn([128, CAP], F32)
            Ye, b_Ye = sbn([128, D], F32)
            pX = cx.ps([128, 8, 128], BF16, es); b_pX = Buf()
            pg = [cx.ps([128, 512], F32, es) for _ in range(2)]
            b_pg = [Buf(), Buf()]
            pu = [cx.ps([128, 512], F32, es) for _ in range(2)]
            b_pu = [Buf(), Buf()]
            pd = [cx.ps([128, 512], F32, es) for _ in range(2)]
            b_pd = [Buf(), Buf()]
            wg_v = d["w_gate"].rearrange("e (k p) n -> e p k n", p=128)
            wu_v = d["w_up"].rearrange("e (k p) n -> e p k n", p=128)
            wd_v = d["w_down"].rearrange("e (k p) n -> e p k n", p=128)
            nst = CAP // 128
            pcnt = 0
            dcnt = 0
            for ex in range(NE):
                i = ex % 2
                S.dma("pool", wg[i][:], wg_v[ex], writes=[b_wg[i]])
                S.dma("pool", wu[i][:], wu_v[ex], writes=[b_wu[i]])
                S.dma("pool", wd[i][:], wd_v[ex], writes=[b_wd[i]])
                S.dma("sp", Xe[i][:], d["xbuf"][ex * CAP:(ex + 1) * CAP, :].rearrange("(s p) n -> p s n", p=128),
                      reads=scat, writes=[b_Xe[i]])
                for st in range(nst):
                    for k in range(8):
                        S.op("pe", lambda e: e.transpose(pX[:, k, :], Xe[i][:, st, k * 128:(k + 1) * 128], ident[:]),
                             [b_Xe[i], b_id], [b_pX])
                    if st % 2 == 0:
                        S.op("act", lambda e: e.activation(out=XT[i][:, :, st * 128:(st + 1) * 128], in_=pX[:], func=AF.Copy),
                             [], [b_pX, b_XT[i]])
                    else:
                        S.op("dve", lambda e: e.tensor_copy(out=XT[i][:, :, st * 128:(st + 1) * 128], in_=pX[:]),
                             [], [b_pX, b_XT[i]])
                for f in range(4):
                    pi = pcnt % 2
                    pcnt += 1
                    for k in range(8):
                        S.op("pe", lambda e: e.matmul(pg[pi][:, 0:CAP], lhsT=wg[i][:, k, f * 128:(f + 1) * 128], rhs=XT[i][:, k, :],
                                                      start=(k == 0), stop=(k == 7)), [b_wg[i], b_XT[i]], [b_pg[pi]])
                    for k in range(8):
                        S.op("pe", lambda e: e.matmul(pu[pi][:, 0:CAP], lhsT=wu[i][:, k, f * 128:(f + 1) * 128], rhs=XT[i][:, k, :],
                                                      start=(k == 0), stop=(k == 7)), [b_wu[i], b_XT[i]], [b_pu[pi]])
                    S.op("act", lambda e: e.activation(out=sg[pi][:], in_=pg[pi][:, 0:CAP], func=AF.Silu), [], [b_pg[pi], b_sg[pi]])
                    S.op("dve", lambda e: e.tensor_tensor(out=hT[i][:, f, :], in0=pu[pi][:, 0:CAP], in1=sg[pi][:], op=ALU.mult),
                         [b_sg[pi]], [b_pu[pi], b_hT[i]])
                for st in range(nst):
                    yi = dcnt % 2
                    dcnt += 1
                    for half in range(2):
                        hs = slice(half * 512, (half + 1) * 512)
                        for f in range(4):
                            S.op("pe", lambda e: e.matmul(pd[half][:], lhsT=hT[i][:, f, st * 128:(st + 1) * 128], rhs=wd[i][:, f, hs],
                                                          start=(f == 0), stop=(f == 3)), [b_hT[i], b_wd[i]], [b_pd[half]])
                        if half == 0:
                            S.op("act", lambda e: e.activation(out=Ye[yi][:, hs], in_=pd[half][:], func=AF.Copy),
                                 [], [b_pd[half], b_Ye[yi]])
                        else:
                            S.op("dve", lambda e: e.tensor_copy(out=Ye[yi][:, hs], in_=pd[half][:]), [], [b_pd[half], b_Ye[yi]])
                    ysc.append(Buf())
                    S.dma("act", d["ybuf"][ex * CAP + st * 128:ex * CAP + (st + 1) * 128, :], Ye[yi][:],
                          reads=[b_Ye[yi]], writes=[ysc[-1]])
            S.barrier()

        with ExitStack() as es:
            def sbn(shape, dt, n=2):
                return [cx.sb(shape, dt, es) for _ in range(n)], [Buf() for _ in range(n)]

            lnp = cx.sb([128, 2, D], F32, es); b_lnp = Buf()
            S.dma("sp", lnp[:], d["lnp"][:, 2 * D:4 * D].partition_broadcast(128).rearrange("p o (a n) -> p (o a) n", a=2), writes=[b_lnp])
            Yg, b_Yg = sbn([128, 2 * D], F32, 3)
            x1r, b_x1r = sbn([128, D], F32, 3)
            acc, b_acc = sbn([128, D], F32)
            z_, b_z_ = sbn([128, D], F32)
            xn = cx.sb([128, D], F32, es); b_xn = Buf()
            tmp = cx.sb([128, D], F32, es); b_tmp = Buf()
            xo, b_xo = sbn([128, D], F32)
            lns = LNScratch(cx, es)

            def G(t):
                i = t % 3
                S.dma("sp", x1r[i][:], d["x1s"][t * 128:(t + 1) * 128, :], reads=x1w, writes=[b_x1r[i]])
                for sidx in range(2):
                    S.dma_fn("pool", lambda e: e.indirect_dma_start(
                        out=Yg[i][:, sidx * D:(sidx + 1) * D], out_offset=None, in_=d["ybuf"][:, :],
                        in_offset=bass.IndirectOffsetOnAxis(ap=destG[:, 2 * t + sidx:2 * t + sidx + 1], axis=0),
                        bounds_check=bc_reg, oob_is_err=False), [b_rt] + ysc, [b_Yg[i]])

            lns2 = [lns, LNScratch(cx, es)]
            xn2 = [xn, cx.sb([128, D], F32, es)]
            b_xn2 = [b_xn, Buf()]
            tmp2 = [tmp, cx.sb([128, D], F32, es)]
            b_tmp2 = [b_tmp, Buf()]

            def Cmb(t):
                i = t % 3
                j = t % 2
                S.op("dve", lambda e: e.tensor_scalar_mul(out=acc[j][:], in0=Yg[i][:, 0:D], scalar1=gate2[:, t, 0:1]),
                     [b_Yg[i], b_rt], [b_acc[j]])
                yield
                S.op("dve", lambda e: e.scalar_tensor_tensor(out=acc[j][:], in0=Yg[i][:, D:2 * D], scalar=gate2[:, t, 1:2],
                                                             in1=acc[j][:], op0=ALU.mult, op1=ALU.add),
                     [b_Yg[i], b_rt], [b_acc[j]])
                yield
                S.op("pool", lambda e: e.tensor_tensor(out=acc[j][:], in0=acc[j][:], in1=g2_ap, op=ALU.mult), [b_ada], [b_acc[j]])
                yield
                S.op("dve", lambda e: e.scalar_tensor_tensor(out=z_[j][:], in0=x1r[i][:], scalar=ALPHA, in1=acc[j][:],
                                                             op0=ALU.mult, op1=ALU.add), [b_x1r[i], b_acc[j]], [b_z_[j]])
                yield

            def Fin(t):
                j = t % 2
                yield from g_ln_mod(cx, z_[j][:], b_z_[j], lns2[j], xn2[j], b_xn2[j], lnp[:, 0, :], lnp[:, 1, :], b_lnp,
                                    xo[j][:], b_xo[j], tmp2[j], b_tmp2[j])
                outs.append(Buf())
                S.dma("sp", xout_d[t * 128:(t + 1) * 128, :], xo[j][:], reads=[b_xo[j]], writes=[outs[-1]])
                yield

            G(0)
            G(1)
            for kk in range(NT + 1):
                if kk + 2 < NT:
                    G(kk + 2)
                round_robin([Cmb(kk) if kk < NT else None, Fin(kk - 1) if 0 <= kk - 1 < NT else None])
            S.barrier()
        S.barrier()
    return outs


def pack_C_inputs(inp, l, j, x_shard, yT_shard, ada_row, hc):
    lnp = np.concatenate([inp["ln_g"][l, 0], inp["ln_b"][l, 0], inp["ln_g"][l, 1], inp["ln_b"][l, 1]])[None, :]
    return {"x": x_shard, "yT": yT_shard, "ada": ada_row, "w_out": np.ascontiguousarray(inp["w_out"][l]),
            "lnp": np.ascontiguousarray(lnp), "w_router": inp["w_router"], "b_router": inp["b_router"][None, :],
            "eoff": hc["eoff"], "w_gate": np.ascontiguousarray(inp["w_gate"][l]), "w_up": np.ascontiguousarray(inp["w_up"][l]),
            "w_down": np.ascontiguousarray(inp["w_down"][l]), "ident_bf": hc["ident_bf"], "ident_f": hc["ident_f"],
            "tris_bf": hc["tris_bf"], "ones_bf": hc["ones_bf"]}


def _run(nc, in_maps):
    res = run_bass_kernel_spmd(nc, in_maps, core_ids=list(range(NCORES)))
    return res.results


def kernel_unfused(**inputs):
    inp = {k: np.asarray(v) for k, v in inputs.items()}
    hc = host_consts()
    x = np.ascontiguousarray(inp["x"][0], dtype=np.float32)
    cT = np.ascontiguousarray(inp["c"][0].reshape(8, 128).T)
    for l in range(DEPTH):
        ra = _run(build_A(), [{"x": np.ascontiguousarray(x[j * TPC:(j + 1) * TPC]), "cT": cT,
                               "w_ada": np.ascontiguousarray(inp["w_ada"][l]),
                               "b_ada": np.ascontiguousarray(inp["b_ada"][l][None, :]),
                               "ident_bf": hc["ident_bf"]} for j in range(NCORES)])
        uT = np.ascontiguousarray(np.concatenate([r["uT"] for r in ra], axis=1))
        ada = ra[0]["ada"]
        rb = _run(build_B(), [pack_B_inputs(inp, l, h, uT, hc) for h in range(NCORES)])
        yT = np.concatenate([r["yT"][0:128] for r in rb] + [r["yT"][128:256] for r in rb], axis=0)
        rc = _run(build_C(), [pack_C_inputs(inp, l, j, np.ascontiguousarray(x[j * TPC:(j + 1) * TPC]),
                                            np.ascontiguousarray(yT[:, j * TPC:(j + 1) * TPC]), ada, hc)
                              for j in range(NCORES)])
        x = np.concatenate([r["xout"] for r in rc], axis=0)
    return x[None].astype(np.float32)


def kernel(**inputs):
    return kernel_unfused(**inputs)
```

```python
import numpy as np
import ml_dtypes
from contextlib import ExitStack
import concourse.bass as bass
import concourse.mybir as mybir
from concourse.bass_utils import run_bass_kernel_spmd

F32 = mybir.dt.float32
BF16 = mybir.dt.bfloat16
I32 = mybir.dt.int32
AF = mybir.ActivationFunctionType
ALU = mybir.AluOpType
AX = mybir.AxisListType

NCORES = 8
D = 1024
SEQ = 16384
TPC = SEQ // NCORES
NT = TPC // 128
DEPTH = 2
DH = 128
NE = 32
DFF = 512
CAP = 256
ALPHA = (2 * DEPTH) ** 0.25
EPS = 1e-5
BLK = 512
NBLK = SEQ // BLK


class Buf:
    __slots__ = ("w", "r")

    def __init__(self):
        self.w = None
        self.r = {}


class Sched:
    ND = 8

    def __init__(self, nc, es):
        self.nc = nc
        self.engs = {"pe": nc.tensor, "dve": nc.vector, "act": nc.scalar, "pool": nc.gpsimd, "sp": nc.sync}
        self.semh = {}
        for e in self.engs:
            self.semh[e] = es.enter_context(nc.semaphore(f"s_{e}"))
        self.cnt = {e: 0 for e in self.engs}
        self.seen = {e: {} for e in self.engs}
        self.semh["cc"] = es.enter_context(nc.semaphore("s_cc"))
        self.ccn = 0
        self.dqn = {}
        for q in ("sp", "act", "pool"):
            self.dqn[q] = 0
            for i in range(self.ND):
                self.semh[(q, i)] = es.enter_context(nc.semaphore(f"d_{q}{i}"))

    def _wait(self, e, key, val):
        if self.seen[e].get(key, 0) >= val:
            return
        self.engs[e].wait_ge(self.semh[key], val)
        self.seen[e][key] = val

    def _deps(self, e, reads, writes):
        deps = {}
        for b in reads:
            if b.w is not None:
                k, v = b.w
                if deps.get(k, 0) < v:
                    deps[k] = v
        for b in writes:
            if b.w is not None:
                k, v = b.w
                if deps.get(k, 0) < v:
                    deps[k] = v
            for k, v in b.r.items():
                if deps.get(k, 0) < v:
                    deps[k] = v
        for k, v in deps.items():
            if e == "pe" and k == "pe":
                continue
            self._wait(e, k, v)

    def _commit(self, tok, reads, writes):
        k, v = tok
        for b in reads:
            if b.r.get(k, 0) < v:
                b.r[k] = v
        for b in writes:
            b.w = tok
            b.r = {}

    def op(self, e, fn, reads=(), writes=()):
        self._deps(e, reads, writes)
        inst = fn(self.engs[e])
        self.cnt[e] += 1
        inst.then_inc(self.semh[e], 1)
        self._commit((e, self.cnt[e]), reads, writes)
        return inst

    def dma(self, q, out, in_, reads=(), writes=(), **kw):
        return self.dma_fn(q, lambda e: e.dma_start(out=out, in_=in_, **kw), reads, writes)

    def dma_fn(self, q, fn, reads=(), writes=()):
        n = self.dqn[q]
        i = n % self.ND
        rnd = n // self.ND
        self.dqn[q] = n + 1
        key = (q, i)
        if rnd > 0:
            self._wait(q, key, 16 * rnd)
        self._deps(q, reads, writes)
        inst = fn(self.engs[q])
        inst.then_inc(self.semh[key], 16)
        self._commit((key, 16 * (rnd + 1)), reads, writes)
        return inst

    def cc(self, fn, reads=(), writes=()):
        self._deps("pool", reads, writes)
        inst = fn(self.engs["pool"])
        self.ccn += 1
        inst.then_inc(self.semh["cc"], 1)
        self._commit(("cc", self.ccn), reads, writes)
        return inst

    def finish(self, bufs, e="sp"):
        self._deps(e, bufs, bufs)

    def barrier(self):
        for e in self.engs:
            for f in self.engs:
                if f != e and self.cnt[f] > 0:
                    self._wait(e, f, self.cnt[f])
            if self.ccn > 0:
                self._wait(e, "cc", self.ccn)
            for q in ("sp", "act", "pool"):
                n = self.dqn[q]
                for i in range(self.ND):
                    c = (n - i + self.ND - 1) // self.ND if n > i else 0
                    if c > 0:
                        self._wait(e, (q, i), 16 * c)


class Ctx:
    def __init__(self, nc, es):
        self.nc = nc
        self.es = es
        self.S = Sched(nc, es)
        self._n = 0

    def sb(self, shape, dt, es=None, name=None):
        self._n += 1
        return (es or self.es).enter_context(self.nc.sbuf_tensor(name or f"sb{self._n}", list(shape), dt))

    def ps(self, shape, dt, es=None, name=None):
        self._n += 1
        return (es or self.es).enter_context(self.nc.psum_tensor(name or f"ps{self._n}", list(shape), dt))

    def dram_in(self, name, shape, dt):
        return self.nc.dram_tensor(name, list(shape), dt, kind="ExternalInput").ap()

    def dram_out(self, name, shape, dt):
        return self.nc.dram_tensor(name, list(shape), dt, kind="ExternalOutput").ap()


def host_consts():
    c = {}
    c["ident_bf"] = np.eye(128, dtype=np.float32).astype(ml_dtypes.bfloat16)
    c["ident_f"] = np.eye(128, dtype=np.float32)
    r = np.arange(128)
    c["tri_f"] = (r[:, None] <= r[None, :]).astype(np.float32)
    c["ones_f"] = np.ones((128, 128), np.float32)
    c["ones_bf"] = np.ones((128, 128), np.float32).astype(ml_dtypes.bfloat16)
    c["tris_bf"] = (r[:, None] < r[None, :]).astype(np.float32).astype(ml_dtypes.bfloat16)
    c["eoff"] = (np.arange(NE, dtype=np.float32) * CAP)[None, :]
    return c


def emit_ada(cx, cT_d, w_ada_d, b_ada_d, ada_bc, b_ada_bc):
    S = cx.S
    with ExitStack() as es:
        cT = cx.sb([128, 8], F32, es)
        b_cT = Buf()
        cond = cx.sb([128, 8], F32, es)
        b_cond = Buf()
        condB = cx.sb([128, 8, 128], F32, es)
        b_condB = Buf()
        bias = cx.sb([128, 6 * D], F32, es)
        b_bias = Buf()
        wbuf = [cx.sb([128, 8, 512], F32, es) for _ in range(2)]
        b_w = [Buf(), Buf()]
        pp = [cx.ps([128, 512], F32, es) for _ in range(2)]
        b_pp = [Buf(), Buf()]
        S.dma("sp", cT[:], cT_d, writes=[b_cT])
        S.dma("sp", bias[:], b_ada_d.partition_broadcast(128), writes=[b_bias])
        S.op("act", lambda e: e.activation(out=cond[:], in_=cT[:], func=AF.Silu), [b_cT], [b_cond])
        for k in range(8):
            S.op("dve", lambda e: e.tensor_copy(out=condB[:, k, :], in_=cond[:, k:k + 1].to_broadcast([128, 128])),
                 [b_cond], [b_condB])
        wv = w_ada_d.rearrange("(k p) n -> p k n", p=128)
        for j in range(12):
            w = wbuf[j % 2]
            S.dma("sp" if j % 2 == 0 else "act", w[:], wv[:, :, j * 512:(j + 1) * 512], writes=[b_w[j % 2]])
            p = pp[j % 2]
            for k in range(8):
                S.op("pe", lambda e: e.matmul(p[:], lhsT=condB[:, k, :], rhs=w[:, k, :], start=(k == 0), stop=(k == 7)),
                     [b_condB, b_w[j % 2]], [b_pp[j % 2]])
            S.op("dve", lambda e: e.tensor_tensor(out=ada_bc[:, j * 512:(j + 1) * 512], in0=p[:],
                                                  in1=bias[:, j * 512:(j + 1) * 512], op=ALU.add),
                 [b_pp[j % 2], b_bias], [b_ada_bc])
        S.barrier()


def emit_ln_stats(cx, x_ap, b_x, st, mv, rs, nb, b_st):
    S = cx.S
    S.op("dve", lambda e: e.bn_stats(out=st[:, 0:6], in_=x_ap[:, 0:512]), [b_x], [b_st])
    S.op("dve", lambda e: e.bn_stats(out=st[:, 6:12], in_=x_ap[:, 512:1024]), [b_x], [b_st])
    S.op("dve", lambda e: e.bn_aggr(out=mv[:], in_=st[:]), [b_st], [b_st])
    S.op("act", lambda e: e.activation(out=rs[:], in_=mv[:, 1:2], func=AF.Sqrt, bias=EPS, scale=1.0), [b_st], [b_st])
    S.op("dve", lambda e: e.reciprocal(out=rs[:], in_=rs[:]), [b_st], [b_st])
    S.op("dve", lambda e: e.scalar_tensor_tensor(out=nb[:], in0=mv[:, 0:1], scalar=-1.0, in1=rs[:],
                                                 op0=ALU.mult, op1=ALU.mult), [b_st], [b_st])


def g_ln_mod(cx, x_ap, b_x, lns, xn, b_xn, A_ap, B_ap, b_ab, out_ap, b_out, tmp, b_tmp):
    S = cx.S
    st, mv, rs, nb, b_st = lns.st, lns.mv, lns.rs, lns.nb, lns.b
    S.op("dve", lambda e: e.bn_stats(out=st[:, 0:6], in_=x_ap[:, 0:512]), [b_x], [b_st]); yield
    S.op("dve", lambda e: e.bn_stats(out=st[:, 6:12], in_=x_ap[:, 512:1024]), [b_x], [b_st]); yield
    S.op("dve", lambda e: e.bn_aggr(out=mv[:], in_=st[:]), [b_st], [b_st]); yield
    S.op("act", lambda e: e.activation(out=rs[:], in_=mv[:, 1:2], func=AF.Sqrt, bias=EPS, scale=1.0), [b_st], [b_st]); yield
    S.op("dve", lambda e: e.reciprocal(out=rs[:], in_=rs[:]), [b_st], [b_st]); yield
    S.op("dve", lambda e: e.scalar_tensor_tensor(out=nb[:], in0=mv[:, 0:1], scalar=-1.0, in1=rs[:],
                                                 op0=ALU.mult, op1=ALU.mult), [b_st], [b_st]); yield
    S.op("act", lambda e: e.activation(out=xn[:], in_=x_ap, func=AF.Identity, bias=nb[:], scale=rs[:]), [b_x, b_st], [b_xn]); yield
    S.op("dve", lambda e: e.tensor_tensor(out=tmp[:], in0=xn[:], in1=A_ap, op=ALU.mult), [b_xn, b_ab], [b_tmp]); yield
    S.op("pool", lambda e: e.tensor_tensor(out=out_ap, in0=tmp[:], in1=B_ap, op=ALU.add), [b_tmp, b_ab], [b_out]); yield


def round_robin(gens):
    gens = [g for g in gens if g is not None]
    while gens:
        nxt = []
        for g in gens:
            try:
                next(g)
                nxt.append(g)
            except StopIteration:
                pass
        gens = nxt


class LNScratch:
    def __init__(self, cx, es):
        self.st = cx.sb([128, 12], F32, es)
        self.mv = cx.sb([128, 2], F32, es)
        self.rs = cx.sb([128, 1], F32, es)
        self.nb = cx.sb([128, 1], F32, es)
        self.b = Buf()


def emit_ln_mod(cx, x_ap, b_x, lns, xn, b_xn, A_ap, B_ap, b_ab, out_ap, b_out, tmp, b_tmp):
    S = cx.S
    emit_ln_stats(cx, x_ap, b_x, lns.st, lns.mv, lns.rs, lns.nb, lns.b)
    S.op("act", lambda e: e.activation(out=xn[:], in_=x_ap, func=AF.Identity, bias=lns.nb[:], scale=lns.rs[:]),
         [b_x, lns.b], [b_xn])
    S.op("dve", lambda e: e.tensor_tensor(out=tmp[:], in0=xn[:], in1=A_ap, op=ALU.mult), [b_xn, b_ab], [b_tmp])
    S.op("pool", lambda e: e.tensor_tensor(out=out_ap, in0=tmp[:], in1=B_ap, op=ALU.add), [b_tmp, b_ab], [b_out])


def build_A():
    nc = bass.Bass("TRN2", target_bir_lowering=False)
    with ExitStack() as es:
        cx = Ctx(nc, es)
        S = cx.S
        x_d = cx.dram_in("x", [TPC, D], F32)
        cT_d = cx.dram_in("cT", [128, 8], F32)
        w_ada_d = cx.dram_in("w_ada", [D, 6 * D], F32)
        b_ada_d = cx.dram_in("b_ada", [1, 6 * D], F32)
        ident_d = cx.dram_in("ident_bf", [128, 128], BF16)
        uT_d = cx.dram_out("uT", [D, TPC], BF16)
        ada_d = cx.dram_out("ada", [1, 6 * D], F32)

        ada = cx.sb([128, 6 * D], F32)
        b_ada = Buf()
        emit_ada(cx, cT_d, w_ada_d, b_ada_d, ada, b_ada)
        b_adaout = Buf()
        S.dma("sp", ada_d, ada[0:1, :], reads=[b_ada], writes=[b_adaout])
        S.finish([b_adaout], "sp")
        ident = cx.sb([128, 128], BF16)
        b_id = Buf()
        S.dma("sp", ident[:], ident_d, writes=[b_id])
        S.op("dve", lambda e: e.tensor_scalar_add(out=ada[:, D:2 * D], in0=ada[:, D:2 * D], scalar1=1.0), [b_ada], [b_ada])
        emit_A_body(cx, x_d, None, None, ada, b_ada, ident, b_id, uT_d)
    return nc


def emit_A_body(cx, x_d, xres, b_xres, ada, b_ada, ident, b_id, uT_d, sh_off=0, sc_off=D):
    S = cx.S
    with ExitStack() as es:
        lns = LNScratch(cx, es)
        xin = [cx.sb([128, D], F32, es) for _ in range(2)]
        b_xin = [Buf(), Buf()]
        xn = cx.sb([128, D], F32, es)
        b_xn = Buf()
        tmp = cx.sb([128, D], F32, es)
        b_tmp = Buf()
        ub = [cx.sb([128, D], BF16, es) for _ in range(2)]
        b_ub = [Buf(), Buf()]
        pT = [cx.ps([128, 8, 128], BF16, es) for _ in range(2)]
        b_pT = [Buf(), Buf()]
        uTs = [cx.sb([128, 8, 128], BF16, es) for _ in range(2)]
        b_uTs = [Buf(), Buf()]
        outs = []
        uT_v = uT_d.rearrange("(k p) t -> p k t", p=128)
        lns2 = [lns, LNScratch(cx, es)]
        xn2 = [xn, cx.sb([128, D], F32, es)]
        b_xn2 = [b_xn, Buf()]
        tmp2 = [tmp, cx.sb([128, D], F32, es)]
        b_tmp2 = [b_tmp, Buf()]

        def Sa(t):
            i = t % 2
            if x_d is not None:
                S.dma("sp", xin[i][:], x_d[t * 128:(t + 1) * 128, :], writes=[b_xin[i]])
                x_ap, bx = xin[i][:], b_xin[i]
            else:
                x_ap, bx = xres[:, t, :], b_xres[t]
            yield from g_ln_mod(cx, x_ap, bx, lns2[i], xn2[i], b_xn2[i], ada[:, sc_off:sc_off + D], ada[:, sh_off:sh_off + D], b_ada,
                                ub[i][:], b_ub[i], tmp2[i], b_tmp2[i])

        def Sb(t):
            i = t % 2
            for k in range(8):
                S.op("pe", lambda e: e.transpose(pT[i][:, k, :], ub[i][:, k * 128:(k + 1) * 128], ident[:]),
                     [b_ub[i], b_id], [b_pT[i]])
                if k % 4 == 3:
                    yield
            S.op("act", lambda e: e.activation(out=uTs[i][:], in_=pT[i][:], func=AF.Copy), [b_pT[i]], [b_uTs[i]])
            yield
            outs.append(Buf())
            S.dma("sp", uT_v[:, :, t * 128:(t + 1) * 128], uTs[i][:], reads=[b_uTs[i]], writes=[outs[-1]])
            yield

        for kk in range(NT + 1):
            round_robin([Sa(kk) if kk < NT else None, Sb(kk - 1) if 0 <= kk - 1 < NT else None])
        S.finish(outs, "sp")
        S.barrier()


LN_INV_SQRT_DH = float(-0.5 * np.log(DH))
GELU_C = 1.5957691216057308


def build_B():
    nc = bass.Bass("TRN2", target_bir_lowering=False)
    with ExitStack() as es:
        cx = Ctx(nc, es)
        d = {
            "uT": cx.dram_in("uT", [D, SEQ], BF16),
            "wh": cx.dram_in("wh", [D, 770], F32),
            "pp": cx.dram_in("pp", [128, 24], F32),
            "fv": cx.dram_in("fv", [1, 258], F32),
            "wa": cx.dram_in("wa", [128, 128], F32),
            "wx": cx.dram_in("wx", [128, 128], F32),
            "ident_bf": cx.dram_in("ident_bf", [128, 128], BF16),
            "tri_f": cx.dram_in("tri_f", [128, 128], F32),
            "ones_f": cx.dram_in("ones_f", [128, 128], F32),
            "ones_bf": cx.dram_in("ones_bf", [128, 128], BF16),
            "yT": cx.dram_out("yT", [256, SEQ], BF16),
        }
        outs = emit_B(cx, d)
        cx.S.finish(outs, "sp")
    return nc


def emit_B(cx, d, nblk=NBLK):
    S = cx.S
    outs = []
    NCH = BLK // 128
    with ExitStack() as es:
        def sb(shape, dt):
            return cx.sb(shape, dt, es)

        def sbn(shape, dt, n=2):
            return [cx.sb(shape, dt, es) for _ in range(n)], [Buf() for _ in range(n)]

        W = sb([128, 8, 770], BF16); b_W = Buf()
        S.dma("pool", W[:], d["wh"].rearrange("(k p) n -> p k n", p=128), writes=[b_W])
        wa = sb([128, 128], BF16); b_wa = Buf()
        S.dma("pool", wa[:], d["wa"], writes=[b_wa])
        wx = sb([128, 128], BF16); b_wx = Buf()
        S.dma("pool", wx[:], d["wx"], writes=[b_wx])
        pp = sb([128, 24], F32); b_pp = Buf()
        S.dma("sp", pp[:], d["pp"], writes=[b_pp])
        fv = sb([128, 258], F32); b_fv = Buf()
        S.dma("sp", fv[:], d["fv"].partition_broadcast(128), writes=[b_fv])
        ident = sb([128, 128], BF16); b_id = Buf()
        S.dma("sp", ident[:], d["ident_bf"], writes=[b_id])
        tri = sb([128, 128], F32); b_tri = Buf()
        S.dma("sp", tri[:], d["tri_f"], writes=[b_tri])
        ones_f = sb([128, 128], F32); b_of = Buf()
        S.dma("sp", ones_f[:], d["ones_f"], writes=[b_of])
        ones_bf = sb([128, 128], BF16); b_ob = Buf()
        S.dma("sp", ones_bf[:], d["ones_bf"], writes=[b_ob])
        der = sb([128, 4], F32); b_der = Buf()
        S.op("act", lambda e: e.activation(out=der[:, 3:4], in_=pp[:, 21:22], func=AF.Exp, scale=-1.0), [b_pp], [b_der])
        S.op("act", lambda e: e.activation(out=der[:, 3:4], in_=der[:, 3:4], func=AF.Ln, bias=1.0, scale=1.0), [b_der], [b_der])
        S.op("dve", lambda e: e.tensor_scalar_mul(out=der[:, 0:1], in0=der[:, 3:4], scalar1=-8.0), [b_der], [b_der])
        S.op("dve", lambda e: e.tensor_scalar_mul(out=der[:, 1:2], in0=der[:, 3:4], scalar1=-16.0), [b_der], [b_der])
        S.op("dve", lambda e: e.tensor_scalar_mul(out=der[:, 2:3], in0=pp[:, 22:23], scalar1=float(np.sqrt(128.0))),
             [b_pp, b_der], [b_der])

        dg = {}
        b_dg = Buf()
        for nm, wc in (("q", 0), ("k", 4), ("x", 8)):
            dg[nm] = sb([128, 4, 128], BF16)
            for k in range(4):
                S.op("dve", lambda e: e.tensor_scalar_mul(out=dg[nm][:, k, :], in0=ident[:], scalar1=pp[:, wc + k:wc + k + 1]),
                     [b_id, b_pp], [b_dg])
        C32 = sb([128, 129], F32); b_C32 = Buf()
        Cbf = sb([128, 129], BF16); b_Cbf = Buf()
        S.op("dve", lambda e: e.memset(C32[:], 0.0), [], [b_C32])
        S.op("dve", lambda e: e.memset(Cbf[:], 0.0), [], [b_Cbf])
        hz = sb([128, 1], F32); b_hz = Buf()
        S.op("dve", lambda e: e.memset(hz[:], 0.0), [], [b_hz])

        uTb, b_uTb = sbn([128, 8, BLK], BF16)
        pre = {}
        b_pre = {}
        for nm in ("q", "k", "x"):
            pre[nm], b_pre[nm] = sbn([128, BLK + 8], BF16)
            for i in range(2):
                S.op("pool", lambda e: e.memset(pre[nm][i][:, 0:3], 0.0), [], [b_pre[nm][i]])
        gpre, b_gpre = sbn([128, BLK], F32)
        acc = {}
        b_acc = {}
        for nm in ("x",):
            acc[nm], b_acc[nm] = sbn([128, BLK], F32)
        qT, b_qT = sbn([128, BLK], BF16)
        kT, b_kT = sbn([128, BLK], BF16)
        vo, b_vo = sbn([128, NCH, 256], F32)
        iff, b_if = sbn([128, NCH, 2], F32)
        sm, b_sm = sbn([128, 64], F32)
        vaug, b_vaug = sbn([128, NCH, 144], BF16)
        kc, b_kc = sbn([128, NCH, 128], BF16)
        PT, b_PT = sbn([128, NCH, 128], BF16)
        hm, b_hm = sbn([128, NCH, 128], F32)
        hq, b_hq = sbn([128, NCH, 128], F32)
        so, b_so = sbn([128, NCH, 128], F32)
        ym, b_ym = sbn([128, NCH, 128], BF16)
        tC, b_tC = sbn([128, 129], F32)
        ymT, b_ymT = sbn([128, BLK], BF16)
        yrT, b_yrT = sbn([128, BLK], BF16)
        xcb, b_xcb = sbn([128, BLK], BF16)
        names = ["r", "ig", "a", "a2", "xi", "h", "g2", "sg", "gl", "yr", "sd", "yn"]
        L = {}
        bL = {}
        for nm in names:
            L[nm], bL[nm] = sbn([128, BLK], F32)
        ysq, b_ysq = sbn([128, BLK], BF16)

        pfm = [cx.ps([128, 512], F32, es) for _ in range(2)]
        b_pfm = [Buf(), Buf()]
        pvo = [cx.ps([128, 2, 256], F32, es) for _ in range(2)]
        b_pvo = [Buf(), Buf()]
        pTr = cx.ps([128, 512], F32, es); b_pTr = Buf()
        psm = cx.ps([128, 512], F32, es); b_psm = Buf()
        pST = cx.ps([128, NCH, 128], F32, es); b_pST = Buf()
        pnum = cx.ps([128, NCH, 128], F32, es); b_pnum = Buf()
        p_kc = pTr[:, 0:256].bitcast(BF16).rearrange("p (c n) -> p c n", c=NCH)
        p_ym = pTr[:, 256:512].bitcast(BF16).rearrange("p (c n) -> p c n", c=NCH)
        p_cs = psm[:, 0:8]
        p_if = psm[:, 8:16].rearrange("p (c n) -> p c n", c=NCH)
        p_den = psm[:, 16:20]
        p_upd = psm[:, 32:161]
        fmstate = {"n": 0}

        def fm_bank():
            n = fmstate["n"] % 2
            fmstate["n"] += 1
            return pfm[n], b_pfm[n]

        uT_v = d["uT"].rearrange("(k p) t -> p k t", p=128)

        def proj_pieces(blk):
            i = blk % 2
            t0 = blk * BLK
            pieces = []

            def p_load():
                S.dma("sp", uTb[i][:], uT_v[:, :, t0:t0 + BLK], writes=[b_uTb[i]])
                if blk > 0:
                    for nm in ("q", "k", "x"):
                        S.op("pool", lambda e: e.tensor_copy(out=pre[nm][i][:, 0:3], in_=pre[nm][1 - i][:, BLK:BLK + 3]),
                             [b_pre[nm][1 - i]], [b_pre[nm][i]])

            def p_fm(gi, nm):
                def f():
                    p, bp = fm_bank()
                    for k in range(8):
                        S.op("pe", lambda e: e.matmul(p[:], lhsT=W[:, k, gi * 128:(gi + 1) * 128], rhs=uTb[i][:, k, :],
                                                      start=(k == 0), stop=(k == 7)), [b_W, b_uTb[i]], [bp])
                    if nm == "g":
                        S.op("act", lambda e: e.activation(out=gpre[i][:], in_=p[:], func=AF.Identity,
                                                           bias=pp[:, 18:19], scale=1.0), [b_pp], [bp, b_gpre[i]])
                    else:
                        S.op("act", lambda e: e.activation(out=pre[nm][i][:, 3:BLK + 3], in_=p[:], func=AF.Identity,
                                                           bias=pp[:, 15 + gi:16 + gi], scale=1.0), [b_pp], [bp, b_pre[nm][i]])
                return f

            def p_vo(cp):
                def f():
                    for cc in range(2):
                        c = cp * 2 + cc
                        for k in range(8):
                            S.op("pe", lambda e: e.matmul(pvo[cp][:, cc, :], lhsT=uTb[i][:, k, c * 128:(c + 1) * 128],
                                                          rhs=W[:, k, 512:768], start=(k == 0), stop=(k == 7)),
                                 [b_W, b_uTb[i]], [b_pvo[cp]])
                    S.op("dve", lambda e: e.tensor_tensor(out=vo[i][:, cp * 2:cp * 2 + 2, :], in0=pvo[cp][:],
                                                          in1=fv[:, 0:256].unsqueeze(1).to_broadcast([128, 2, 256]), op=ALU.add),
                         [b_fv], [b_pvo[cp], b_vo[i]])
                return f

            def p_ifproj():
                for c in range(NCH):
                    for k in range(8):
                        S.op("pe", lambda e: e.matmul(p_if[:, c, :], lhsT=uTb[i][:, k, c * 128:(c + 1) * 128],
                                                      rhs=W[:, k, 768:770], start=(k == 0), stop=(k == 7)),
                             [b_W, b_uTb[i]], [b_psm])
                S.op("dve", lambda e: e.tensor_tensor(out=iff[i][:], in0=p_if, in1=fv[:, 256:258].unsqueeze(1).to_broadcast([128, NCH, 2]),
                                                      op=ALU.add), [b_fv], [b_psm, b_if[i]])

            def p_conv(nm, bc):
                def f():
                    p, bp = fm_bank()
                    for k in range(4):
                        S.op("pe", lambda e: e.matmul(p[:], lhsT=dg[nm][:, k, :], rhs=pre[nm][i][:, k:k + BLK],
                                                      start=(k == 0), stop=(k == 3)), [b_dg, b_pre[nm][i]], [bp])
                    if nm == "q":
                        S.op("act", lambda e: e.activation(out=qT[i][:], in_=p[:], func=AF.Silu, bias=pp[:, bc:bc + 1], scale=1.0),
                             [b_pp], [bp, b_qT[i]])
                    elif nm == "k":
                        S.op("act", lambda e: e.activation(out=kT[i][:], in_=p[:], func=AF.Silu, bias=pp[:, bc:bc + 1], scale=1.0),
                             [b_pp], [bp, b_kT[i]])
                    else:
                        S.op("act", lambda e: e.activation(out=acc["x"][i][:], in_=p[:], func=AF.Identity, bias=pp[:, bc:bc + 1], scale=1.0),
                             [b_pp], [bp, b_acc["x"][i]])
                return f

            pieces = [p_load, p_ifproj, p_fm(0, "q"), p_fm(1, "k"), p_fm(2, "x"), p_fm(3, "g"), p_vo(0), p_vo(1),
                      p_conv("q", 12), p_conv("k", 13), p_conv("x", 14)]
            return pieces

        def emit_compute(blk, pieces):
            def pump(n):
                for _ in range(n):
                    if pieces:
                        pieces.pop(0)()
            i = blk % 2
            t0 = blk * BLK
            s_ = sm[i]; bs = b_sm[i]
            bc4 = lambda ap: ap.unsqueeze(2).to_broadcast([128, NCH, 128])
            S.op("act", lambda e: e.activation(out=s_[:, 0:4], in_=iff[i][:, :, 1], func=AF.Exp, scale=-1.0), [b_if[i]], [bs])
            S.op("act", lambda e: e.activation(out=s_[:, 4:8], in_=s_[:, 0:4], func=AF.Ln, bias=1.0, scale=1.0), [bs], [bs])
            S.op("pe", lambda e: e.matmul(p_cs[:, 0:4], lhsT=tri[:], rhs=s_[:, 4:8], start=True, stop=True), [b_tri, bs], [b_psm])
            S.op("pe", lambda e: e.matmul(p_cs[:, 4:8], lhsT=ones_f[:], rhs=s_[:, 4:8], start=True, stop=True), [b_of, bs], [b_psm])
            S.op("dve", lambda e: e.tensor_tensor(out=s_[:, 8:12], in0=iff[i][:, :, 0], in1=p_cs[:, 0:4], op=ALU.add),
                 [b_if[i]], [b_psm, bs])
            S.op("act", lambda e: e.activation(out=s_[:, 12:16], in_=s_[:, 8:12], func=AF.Exp, bias=LN_INV_SQRT_DH, scale=1.0), [bs], [bs])
            S.op("act", lambda e: e.activation(out=s_[:, 16:24], in_=p_cs, func=AF.Exp, scale=-1.0), [], [b_psm, bs])
            pump(3)
            ws = s_[:, 12:16]
            eb = s_[:, 16:20]
            S.op("dve", lambda e: e.tensor_tensor(out=vaug[i][:, :, 0:128], in0=vo[i][:, :, 0:128], in1=bc4(ws), op=ALU.mult),
                 [b_vo[i], bs], [b_vaug[i]])
            S.op("dve", lambda e: e.tensor_copy(out=vaug[i][:, :, 128], in_=ws), [bs], [b_vaug[i]])
            for c in range(NCH):
                S.op("pe", lambda e: e.transpose(p_kc[:, c, :], kT[i][:, c * 128:(c + 1) * 128], ident[:]), [b_kT[i], b_id], [b_pTr])
            S.op("act", lambda e: e.activation(out=kc[i][:], in_=p_kc, func=AF.Copy), [], [b_pTr, b_kc[i]])
            for c in range(NCH):
                cs = slice(c * 128, (c + 1) * 128)
                S.op("pe", lambda e: e.matmul(pST[:, c, :], lhsT=kT[i][:, cs], rhs=qT[i][:, cs], start=True, stop=True),
                     [b_kT[i], b_qT[i]], [b_pST])
            S.op("dve", lambda e: e.tensor_tensor(out=PT[i][:], in0=pST[:], in1=tri[:].unsqueeze(1).to_broadcast([128, NCH, 128]),
                                                  op=ALU.mult), [b_tri], [b_pST, b_PT[i]])
            for c in range(NCH):
                pump(1)
                cs = slice(c * 128, (c + 1) * 128)
                S.op("pe", lambda e: e.matmul(pnum[:, c, :], lhsT=PT[i][:, c, :], rhs=vaug[i][:, c, 0:128], start=True, stop=False),
                     [b_PT[i], b_vaug[i]], [b_pnum])
                S.op("pe", lambda e: e.matmul(pnum[:, c, :], lhsT=qT[i][:, cs], rhs=Cbf[:, 0:128], start=False, stop=True),
                     [b_qT[i], b_Cbf], [b_pnum])
                S.op("pe", lambda e: e.matmul(p_den[:, c:c + 1], lhsT=PT[i][:, c, :], rhs=vaug[i][:, c, 128:129], start=True, stop=False),
                     [b_PT[i], b_vaug[i]], [b_psm])
                S.op("pe", lambda e: e.matmul(p_den[:, c:c + 1], lhsT=qT[i][:, cs], rhs=Cbf[:, 128:129], start=False, stop=True),
                     [b_qT[i], b_Cbf], [b_psm])
                S.op("pe", lambda e: e.matmul(p_upd, lhsT=kc[i][:, c, :], rhs=vaug[i][:, c, 0:129], start=True, stop=True),
                     [b_kc[i], b_vaug[i]], [b_psm])
                j = c % 2
                S.op("dve", lambda e: e.tensor_tensor(out=tC[j][:], in0=p_upd, in1=C32[:], op=ALU.add), [b_C32], [b_psm, b_tC[j]])
                S.op("dve", lambda e: e.tensor_tensor(out=C32[:], in0=tC[j][:], in1=s_[:, 20 + c:21 + c].to_broadcast([128, 129]), op=ALU.mult),
                     [b_tC[j], bs], [b_C32])
                S.op("act", lambda e: e.activation(out=Cbf[:], in_=tC[j][:], func=AF.Copy, scale=s_[:, 20 + c:21 + c]),
                     [b_tC[j], bs], [b_Cbf])
            pump(1)
            S.op("dve", lambda e: e.tensor_tensor(out=s_[:, 24:28], in0=p_den, in1=eb, op=ALU.mult), [], [b_psm, bs])
            S.op("act", lambda e: e.activation(out=s_[:, 24:28], in_=s_[:, 24:28], func=AF.Abs), [bs], [bs])
            S.op("dve", lambda e: e.tensor_scalar_max(out=s_[:, 24:28], in0=s_[:, 24:28], scalar1=1.0), [bs], [bs])
            S.op("dve", lambda e: e.reciprocal(out=s_[:, 28:32], in_=s_[:, 24:28]), [bs], [bs])
            S.op("dve", lambda e: e.tensor_tensor(out=s_[:, 28:32], in0=s_[:, 28:32], in1=eb, op=ALU.mult), [bs], [bs])
            S.op("dve", lambda e: e.tensor_tensor(out=hm[i][:], in0=pnum[:], in1=bc4(s_[:, 28:32]), op=ALU.mult), [bs], [b_pnum, b_hm[i]])
            S.op("dve", lambda e: e.tensor_reduce(out=s_[:, 32:36], in_=hm[i][:], axis=AX.X, op=ALU.add), [b_hm[i]], [bs])
            S.op("act", lambda e: e.activation(out=hq[i][:], in_=hm[i][:], func=AF.Square), [b_hm[i]], [b_hq[i]])
            S.op("dve", lambda e: e.tensor_reduce(out=s_[:, 36:40], in_=hq[i][:], axis=AX.X, op=ALU.add), [b_hq[i]], [bs])
            S.op("dve", lambda e: e.tensor_scalar_mul(out=s_[:, 32:36], in0=s_[:, 32:36], scalar1=1.0 / 128.0), [bs], [bs])
            S.op("dve", lambda e: e.tensor_tensor(out=s_[:, 40:44], in0=s_[:, 32:36], in1=s_[:, 32:36], op=ALU.mult), [bs], [bs])
            S.op("dve", lambda e: e.scalar_tensor_tensor(out=s_[:, 36:40], in0=s_[:, 36:40], scalar=1.0 / 128.0, in1=s_[:, 40:44],
                                                         op0=ALU.mult, op1=ALU.subtract), [bs], [bs])
            S.op("act", lambda e: e.activation(out=s_[:, 36:40], in_=s_[:, 36:40], func=AF.Ln, bias=EPS, scale=1.0), [bs], [bs])
            S.op("act", lambda e: e.activation(out=s_[:, 36:40], in_=s_[:, 36:40], func=AF.Exp, scale=-0.5), [bs], [bs])
            S.op("dve", lambda e: e.tensor_tensor(out=hm[i][:], in0=hm[i][:], in1=bc4(s_[:, 32:36]), op=ALU.subtract), [bs], [b_hm[i]])
            S.op("dve", lambda e: e.tensor_tensor(out=hm[i][:], in0=hm[i][:], in1=bc4(s_[:, 36:40]), op=ALU.mult), [bs], [b_hm[i]])
            S.op("act", lambda e: e.activation(out=so[i][:], in_=vo[i][:, :, 128:256], func=AF.Sigmoid), [b_vo[i]], [b_so[i]])
            S.op("dve", lambda e: e.tensor_tensor(out=ym[i][:], in0=hm[i][:], in1=so[i][:], op=ALU.mult), [b_hm[i], b_so[i]], [b_ym[i]])
            pump(1)
            for c in range(NCH):
                S.op("pe", lambda e: e.transpose(p_ym[:, c, :], ym[i][:, c, :], ident[:]), [b_ym[i], b_id], [b_pTr])
            S.op("act", lambda e: e.activation(out=ymT[i][:].rearrange("p (c n) -> p c n", c=NCH), in_=p_ym, func=AF.Copy,
                                               scale=pp[:, 23:24]), [b_pp], [b_pTr, b_ymT[i]])
            outs.append(Buf())
            S.dma("sp", d["yT"][0:128, t0:t0 + BLK], ymT[i][:], reads=[b_ymT[i]], writes=[outs[-1]])

            xc = acc["x"][i]; b_xc = b_acc["x"][i]
            S.op("act", lambda e: e.activation(out=xcb[i][:], in_=xc[:], func=AF.Copy), [b_xc], [b_xcb[i]])
            p, bp = fm_bank()
            S.op("pe", lambda e: e.matmul(p[:], lhsT=wa[:], rhs=xcb[i][:], start=True, stop=True), [b_wa, b_xcb[i]], [bp])
            S.op("act", lambda e: e.activation(out=L["r"][i][:], in_=p[:], func=AF.Sigmoid, bias=pp[:, 19:20], scale=1.0),
                 [b_pp], [bp, bL["r"][i]])
            p, bp = fm_bank()
            S.op("pe", lambda e: e.matmul(p[:], lhsT=wx[:], rhs=xcb[i][:], start=True, stop=True), [b_wx, b_xcb[i]], [bp])
            S.op("act", lambda e: e.activation(out=L["ig"][i][:], in_=p[:], func=AF.Sigmoid, bias=pp[:, 20:21], scale=1.0),
                 [b_pp], [bp, bL["ig"][i]])
            g = gpre[i]; bg = b_gpre[i]
            S.op("pool", lambda e: e.tensor_tensor(out=L["g2"][i][:], in0=g[:], in1=g[:], op=ALU.mult), [bg], [bL["g2"][i]])
            S.op("pool", lambda e: e.tensor_scalar(out=L["g2"][i][:], in0=L["g2"][i][:], scalar1=0.044715, scalar2=1.0,
                                                   op0=ALU.mult, op1=ALU.add), [], [bL["g2"][i]])
            S.op("pool", lambda e: e.tensor_tensor(out=L["g2"][i][:], in0=L["g2"][i][:], in1=g[:], op=ALU.mult), [bg], [bL["g2"][i]])
            S.op("act", lambda e: e.activation(out=L["sg"][i][:], in_=L["g2"][i][:], func=AF.Sigmoid, scale=GELU_C),
                 [bL["g2"][i]], [bL["sg"][i]])
            S.op("act", lambda e: e.activation(out=L["a"][i][:], in_=L["r"][i][:], func=AF.Exp, scale=der[:, 0:1]),
                 [bL["r"][i], b_der], [bL["a"][i]])
            S.op("act", lambda e: e.activation(out=L["a2"][i][:], in_=L["r"][i][:], func=AF.Exp, scale=der[:, 1:2]),
                 [bL["r"][i], b_der], [bL["a2"][i]])
            S.op("act", lambda e: e.activation(out=L["a2"][i][:], in_=L["a2"][i][:], func=AF.Ln, bias=1.0, scale=-1.0),
                 [bL["a2"][i]], [bL["a2"][i]])
            S.op("act", lambda e: e.activation(out=L["a2"][i][:], in_=L["a2"][i][:], func=AF.Exp, scale=0.5),
                 [bL["a2"][i]], [bL["a2"][i]])
            S.op("pool", lambda e: e.tensor_tensor(out=L["xi"][i][:], in0=L["ig"][i][:], in1=xc[:], op=ALU.mult),
                 [bL["ig"][i], b_xc], [bL["xi"][i]])
            S.op("dve", lambda e: e.tensor_tensor(out=L["xi"][i][:], in0=L["xi"][i][:], in1=L["a2"][i][:], op=ALU.mult),
                 [bL["a2"][i]], [bL["xi"][i]])
            init = hz[:, 0:1] if blk == 0 else L["h"][1 - i][:, BLK - 1:BLK]
            b_init = b_hz if blk == 0 else bL["h"][1 - i]
            S.op("dve", lambda e: e.tensor_tensor_scan(out=L["h"][i][:], data0=L["a"][i][:], data1=L["xi"][i][:], initial=init,
                                                       op0=ALU.mult, op1=ALU.add),
                 [bL["a"][i], bL["xi"][i], b_init], [bL["h"][i]])
            g = gpre[i]; bg = b_gpre[i]
            S.op("pool", lambda e: e.tensor_tensor(out=L["gl"][i][:], in0=L["sg"][i][:], in1=g[:], op=ALU.mult),
                 [bL["sg"][i], bg], [bL["gl"][i]])
            S.op("dve", lambda e: e.tensor_tensor(out=L["yr"][i][:], in0=L["h"][i][:], in1=L["gl"][i][:], op=ALU.mult),
                 [bL["h"][i], bL["gl"][i]], [bL["yr"][i]])
            S.op("act", lambda e: e.activation(out=ysq[i][:], in_=L["yr"][i][:], func=AF.Square), [bL["yr"][i]], [b_ysq[i]])
            p, bp = fm_bank()
            S.op("pe", lambda e: e.matmul(p[:], lhsT=ones_bf[:], rhs=ysq[i][:], start=True, stop=True), [b_ob, b_ysq[i]], [bp])
            S.op("act", lambda e: e.activation(out=L["sd"][i][:], in_=p[:], func=AF.Ln, bias=128.0 * EPS, scale=1.0),
                 [], [bp, bL["sd"][i]])
            S.op("act", lambda e: e.activation(out=L["sd"][i][:], in_=L["sd"][i][:], func=AF.Exp, scale=-0.5), [], [bL["sd"][i]])
            S.op("pool", lambda e: e.tensor_tensor(out=L["yn"][i][:], in0=L["yr"][i][:], in1=L["sd"][i][:], op=ALU.mult),
                 [bL["yr"][i], bL["sd"][i]], [bL["yn"][i]])
            S.op("act", lambda e: e.activation(out=yrT[i][:], in_=L["yn"][i][:], func=AF.Copy, scale=der[:, 2:3]),
                 [bL["yn"][i], b_der], [b_yrT[i]])
            outs.append(Buf())
            S.dma("sp", d["yT"][128:256, t0:t0 + BLK], yrT[i][:], reads=[b_yrT[i]], writes=[outs[-1]])
            pump(len(pieces))

        for f in proj_pieces(0):
            f()
        for blk in range(nblk):
            emit_compute(blk, proj_pieces(blk + 1) if blk + 1 < nblk else [])
        S.barrier()
    return outs


def pack_B_inputs(inp, l, h, uT_full, hc):
    w_in = inp["w_in"][l]
    b_in = inp["b_in"][l]
    hs = slice(h * 128, (h + 1) * 128)
    o_q, o_k, o_v, o_o, o_i, o_f, o_x, o_g = 0, 1024, 2048, 3072, 4096, 4104, 4112, 5136
    cols = np.concatenate([np.arange(o_q + h * 128, o_q + (h + 1) * 128), np.arange(o_k + h * 128, o_k + (h + 1) * 128),
                           np.arange(o_x + h * 128, o_x + (h + 1) * 128), np.arange(o_g + h * 128, o_g + (h + 1) * 128),
                           np.arange(o_v + h * 128, o_v + (h + 1) * 128), np.arange(o_o + h * 128, o_o + (h + 1) * 128),
                           np.array([o_i + h, o_f + h])])
    wh = np.ascontiguousarray(w_in[:, cols])
    bh = b_in[cols]
    pp = np.zeros((128, 24), np.float32)
    pp[:, 0:4] = inp["w_conv_m"][l][:, hs].T
    pp[:, 4:8] = inp["w_conv_m"][l][:, 1024 + h * 128:1024 + (h + 1) * 128].T
    pp[:, 8:12] = inp["w_conv_r"][l][:, hs].T
    pp[:, 12] = inp["b_conv_m"][l][hs]
    pp[:, 13] = inp["b_conv_m"][l][1024 + h * 128:1024 + (h + 1) * 128]
    pp[:, 14] = inp["b_conv_r"][l][hs]
    pp[:, 15] = bh[0:128]
    pp[:, 16] = bh[128:256]
    pp[:, 17] = bh[256:384]
    pp[:, 18] = bh[384:512]
    pp[:, 19] = inp["b_a"][l][hs]
    pp[:, 20] = inp["b_x"][l][hs]
    pp[:, 21] = inp["lru_lambda"][l][hs]
    pp[:, 22] = inp["lru_norm_g"][l][hs]
    pp[:, 23] = inp["mh_norm_g"][l][hs]
    fv = np.ascontiguousarray(bh[512:770][None, :])
    return {"uT": uT_full, "wh": wh, "pp": pp, "fv": fv,
            "wa": np.ascontiguousarray(inp["w_a"][l][h]), "wx": np.ascontiguousarray(inp["w_x"][l][h]),
            "ident_bf": hc["ident_bf"], "tri_f": hc["tri_f"], "ones_f": hc["ones_f"], "ones_bf": hc["ones_bf"]}


NSLOT = NE * CAP
BIGROW = float(NSLOT + 64)


def build_C(debug=False):
    nc = bass.Bass("TRN2", target_bir_lowering=False)
    with ExitStack() as es:
        cx = Ctx(nc, es)
        S = cx.S
        d = {
            "x": cx.dram_in("x", [TPC, D], F32),
            "yT": cx.dram_in("yT", [2 * D, TPC], BF16),
            "ada": cx.dram_in("ada", [1, 6 * D], F32),
            "w_out": cx.dram_in("w_out", [2 * D, D], F32),
            "lnp": cx.dram_in("lnp", [1, 4 * D], F32),
            "w_router": cx.dram_in("w_router", [D, NE], F32),
            "b_router": cx.dram_in("b_router", [1, NE], F32),
            "eoff": cx.dram_in("eoff", [1, NE], F32),
            "w_gate": cx.dram_in("w_gate", [NE, D, DFF], F32),
            "w_up": cx.dram_in("w_up", [NE, D, DFF], F32),
            "w_down": cx.dram_in("w_down", [NE, DFF, D], F32),
            "ident_bf": cx.dram_in("ident_bf", [128, 128], BF16),
            "ident_f": cx.dram_in("ident_f", [128, 128], F32),
            "tris_bf": cx.dram_in("tris_bf", [128, 128], BF16),
            "ones_bf": cx.dram_in("ones_bf", [128, 128], BF16),
            "xout": cx.dram_out("xout", [TPC, D], F32),
        }
        if debug:
            d["xbuf"] = nc.dram_tensor("xbuf", [NSLOT, D], BF16, kind="ExternalOutput")
            d["ybuf"] = nc.dram_tensor("ybuf", [NSLOT, D], F32, kind="ExternalOutput")
            d["dbg_dest"] = cx.dram_out("dbg_dest", [128, NT * 2], I32)
            d["dbg_gate"] = cx.dram_out("dbg_gate", [128, NT, 2], F32)
        else:
            d["xbuf"] = nc.dram_tensor("xbuf", [NSLOT, D], BF16)
            d["ybuf"] = nc.dram_tensor("ybuf", [NSLOT, D], F32)
        adac = cx.sb([128, 4, D], F32)
        b_adac = Buf()
        S.dma("sp", adac[:], d["ada"][:, 2 * D:6 * D].partition_broadcast(128).rearrange("p o (a n) -> p (o a) n", a=4),
              writes=[b_adac])
        S.op("dve", lambda e: e.tensor_scalar_add(out=adac[:, 2, :], in0=adac[:, 2, :], scalar1=1.0), [b_adac], [b_adac])
        d["x1s"] = nc.dram_tensor("x1s", [TPC, D], F32)
        outs = emit_C(cx, d, d["x"], None, None, adac[:, 0, :], adac[:, 1, :], adac[:, 2, :], adac[:, 3, :], b_adac,
                      d["xout"])
        S.finish(outs, "sp")
    return nc


def emit_C(cx, d, x_d, xres, b_xres, g1_ap, sh2_ap, sc2_ap, g2_ap, b_ada, xout_d):
    S = cx.S
    nc = cx.nc
    outs = []
    with ExitStack() as es0:
        ident = cx.sb([128, 128], BF16, es0); b_id = Buf()
        S.dma("sp", ident[:], d["ident_bf"], writes=[b_id])
        scat = []
        bc_reg = nc.gpsimd.alloc_register(f"bc{cx._n}")
        nc.gpsimd.reg_mov(bc_reg, NSLOT - 1)
        zt = cx.sb([128, D], BF16, es0); b_zt = Buf()
        S.op("pool", lambda e: e.memset(zt[:], 0.0), [], [b_zt])
        zfill = []
        for r0 in range(0, NSLOT, 1024):
            zfill.append(Buf())
            S.dma("sp" if (r0 // 1024) % 2 == 0 else "act",
                  d["xbuf"][r0:r0 + 1024, :].rearrange("(p s) n -> p s n", p=128),
                  zt[:].unsqueeze(1).to_broadcast([128, 8, D]), reads=[b_zt], writes=[zfill[-1]])
        NG = NT * NE
        destS = cx.sb([128, 2 * NT], I32, es0)
        destG = cx.sb([128, 2 * NT], I32, es0)
        gate2 = cx.sb([128, NT, 2], F32, es0)
        b_rt = Buf()
        u2b_all = cx.sb([128, NT, D], BF16, es0)
        b_u2b = [Buf() for _ in range(NT)]
        x1w = []
        with ExitStack() as es:
            def sb(shape, dt):
                return cx.sb(shape, dt, es)

            def sbn(shape, dt, n=2):
                return [cx.sb(shape, dt, es) for _ in range(n)], [Buf() for _ in range(n)]

            wout = sb([128, 16, D], BF16); b_wout = Buf()
            wo_v = d["w_out"].rearrange("(m p) n -> p m n", p=128)
            for q4 in range(4):
                S.dma("pool", wout[:, q4 * 4:(q4 + 1) * 4, :], wo_v[:, q4 * 4:(q4 + 1) * 4, :], writes=[b_wout])
            lnp = sb([128, 2, D], F32); b_lnp = Buf()
            S.dma("sp", lnp[:], d["lnp"][:, 0:2 * D].partition_broadcast(128).rearrange("p o (a n) -> p (o a) n", a=2), writes=[b_lnp])
            identf = sb([128, 128], F32); b_idf = Buf()
            S.dma("sp", identf[:], d["ident_f"], writes=[b_idf])
            tris = sb([128, 128], BF16); b_tris = Buf()
            S.dma("sp", tris[:], d["tris_bf"], writes=[b_tris])
            ones = sb([128, 128], BF16); b_ones = Buf()
            S.dma("sp", ones[:], d["ones_bf"], writes=[b_ones])
            wr = sb([128, 8, NE], F32); b_wr = Buf()
            S.dma("sp", wr[:], d["w_router"].rearrange("(k p) n -> p k n", p=128), writes=[b_wr])
            br = sb([128, NE], F32); b_br = Buf()
            S.dma("sp", br[:], d["b_router"].partition_broadcast(128), writes=[b_br])
            eoff = sb([128, NE], F32); b_eoff = Buf()
            S.dma("sp", eoff[:], d["eoff"].partition_broadcast(128), writes=[b_eoff])

            ytb, b_ytb = sbn([128, 16, 256], BF16)
            xin, b_xin = sbn([128, D], F32)
            t1_, b_t1_ = sbn([128, D], F32)
            z_, b_z_ = sbn([128, D], F32)
            x1t_, b_x1t_ = sbn([128, D], F32)
            u2f_, b_u2f_ = sbn([128, D], F32)
            u2T_, b_u2T_ = sbn([128, 8, 128], F32)
            xnA = sb([128, D], F32); b_xnA = Buf()
            tmpA = sb([128, D], F32); b_tmpA = Buf()
            xnB = sb([128, D], F32); b_xnB = Buf()
            tmpB = sb([128, D], F32); b_tmpB = Buf()
            lnsA = LNScratch(cx, es)
            lnsB = LNScratch(cx, es)
            aff_all = sb([128, NT, NE], F32)
            b_aff = [Buf() for _ in range(NT)]

            po = [cx.ps([128, 512], F32, es) for _ in range(2)]
            b_po = [Buf(), Buf()]
            pT = [cx.ps([128, 4, 128], F32, es) for _ in range(2)]
            b_pT = [Buf(), Buf()]
            plog = cx.ps([128, 512], F32, es); b_plog = Buf()
            ppre = cx.ps([128, 512], F32, es); b_ppre = Buf()
            ptot = cx.ps([128, 512], F32, es); b_ptot = Buf()

            yT_v = d["yT"].rearrange("(m p) t -> p m t", p=128)

            logit_all = sb([128, NT, NE], F32)

            def S1(t):
                i = t % 2
                if t % 2 == 0:
                    yi = (t // 2) % 2
                    S.dma("sp", ytb[yi][:], yT_v[:, :, t * 128:t * 128 + 256], writes=[b_ytb[yi]])
                yi = (t // 2) % 2
                tsl = slice((t % 2) * 128, (t % 2) * 128 + 128)
                S.dma("act", xin[i][:], x_d[t * 128:(t + 1) * 128, :], writes=[b_xin[i]])
                for half in range(2):
                    hs = slice(half * 512, (half + 1) * 512)
                    for m in range(16):
                        S.op("pe", lambda e: e.matmul(po[half][:], lhsT=ytb[yi][:, m, tsl], rhs=wout[:, m, hs],
                                                      start=(m == 0), stop=(m == 15)), [b_ytb[yi], b_wout], [b_po[half]])
                        if m % 4 == 3:
                            yield
                    S.op("dve", lambda e: e.tensor_tensor(out=t1_[i][:, hs], in0=po[half][:], in1=g1_ap[:, hs], op=ALU.mult),
                         [b_ada], [b_po[half], b_t1_[i]])
                    yield
                S.op("dve", lambda e: e.scalar_tensor_tensor(out=z_[i][:], in0=xin[i][:], scalar=ALPHA, in1=t1_[i][:],
                                                             op0=ALU.mult, op1=ALU.add), [b_xin[i], b_t1_[i]], [b_z_[i]])
                yield

            def S2(t):
                i = t % 2
                yield from g_ln_mod(cx, z_[i][:], b_z_[i], lnsA, xnA, b_xnA, lnp[:, 0, :], lnp[:, 1, :], b_lnp, x1t_[i][:], b_x1t_[i], tmpA, b_tmpA)
                x1w.append(Buf())
                S.dma("act", d["x1s"][t * 128:(t + 1) * 128, :], x1t_[i][:], reads=[b_x1t_[i]], writes=[x1w[-1]])
                yield

            def S3(t):
                i = t % 2
                yield from g_ln_mod(cx, x1t_[i][:], b_x1t_[i], lnsB, xnB, b_xnB, sc2_ap, sh2_ap, b_ada, u2f_[i][:], b_u2f_[i], tmpB, b_tmpB)
                S.op("act", lambda e: e.activation(out=u2b_all[:, t, :], in_=u2f_[i][:], func=AF.Copy), [b_u2f_[i]], [b_u2b[t]])
                yield

            def S4(t):
                i = t % 2
                u2f, b_u2f, u2T, b_u2T = u2f_[i], b_u2f_[i], u2T_[i], b_u2T_[i]
                for k in range(8):
                    S.op("pe", lambda e: e.transpose(pT[k // 4][:, k % 4, :], u2f[:, k * 128:(k + 1) * 128], identf[:]),
                         [b_u2f, b_idf], [b_pT[k // 4]])
                    if k % 4 == 3:
                        yield
                S.op("act", lambda e: e.activation(out=u2T[:, 0:4, :], in_=pT[0][:], func=AF.Copy), [], [b_pT[0], b_u2T])
                yield
                S.op("dve", lambda e: e.tensor_copy(out=u2T[:, 4:8, :], in_=pT[1][:]), [], [b_pT[1], b_u2T])
                yield
                for k in range(8):
                    S.op("pe", lambda e: e.matmul(plog[:, 0:NE], lhsT=u2T[:, k, :], rhs=wr[:, k, :], start=(k == 0), stop=(k == 7)),
                         [b_u2T, b_wr], [b_plog])
                yield
                S.op("dve", lambda e: e.tensor_copy(out=logit_all[:, t, :], in_=plog[:, 0:NE]), [], [b_plog, b_aff[t]])
                yield

            for kk in range(NT + 3):
                round_robin([S1(kk) if kk < NT else None,
                             S2(kk - 1) if 0 <= kk - 1 < NT else None,
                             S3(kk - 2) if 0 <= kk - 2 < NT else None,
                             S4(kk - 3) if 0 <= kk - 3 < NT else None])
            b_affall = Buf()
            S.op("act", lambda e: e.activation(out=aff_all[:], in_=logit_all[:], func=AF.Sigmoid), b_aff, [b_affall])

            RR = sb([128, 12, NG], F32); b_R = Buf()
            aff = aff_all[:].rearrange("p t e -> p (t e)")
            sel, eq, selm, ge, msk, gv, pos, val, vld, dd, cntf = (RR[:, n, :] for n in range(11))
            q3 = lambda ap: ap.rearrange("p (a j) -> p a j", j=4)
            t3 = lambda ap: ap.rearrange("p (t e) -> p t e", e=NE)
            r128 = sb([128, 4, NT * 8], F32)
            m1, m2, gsc, gone = (r128[:, n, :] for n in range(4))
            r16 = sb([128, 8, NT], F32)
            gmax, gsum, rgs, first, second, g1s, gts = (r16[:, n, :] for n in range(7))
            mk = sb([128, NG], BF16); b_mk = Buf()
            S.op("dve", lambda e: e.tensor_tensor(out=t3(sel), in0=aff_all[:], in1=br[:].unsqueeze(1).to_broadcast([128, NT, NE]), op=ALU.add),
                 [b_affall, b_br], [b_R])
            S.op("dve", lambda e: e.tensor_reduce(out=m1, in_=q3(sel), axis=AX.X, op=ALU.max), [], [b_R])
            S.op("dve", lambda e: e.tensor_tensor(out=q3(eq), in0=q3(sel), in1=m1.unsqueeze(2).to_broadcast([128, NT * 8, 4]), op=ALU.is_equal), [], [b_R])
            S.op("dve", lambda e: e.scalar_tensor_tensor(out=selm, in0=eq, scalar=-1e9, in1=sel, op0=ALU.mult, op1=ALU.add), [], [b_R])
            S.op("dve", lambda e: e.tensor_reduce(out=m2, in_=q3(selm), axis=AX.X, op=ALU.max), [], [b_R])
            S.op("dve", lambda e: e.tensor_tensor(out=gsc, in0=m1, in1=m2, op=ALU.add), [], [b_R])
            g8 = lambda ap: ap.rearrange("p (t g) -> p t g", g=8)
            S.op("dve", lambda e: e.tensor_reduce(out=gmax, in_=g8(gsc), axis=AX.X, op=ALU.max), [], [b_R])
            S.op("dve", lambda e: e.tensor_tensor(out=g8(gone), in0=g8(gsc), in1=gmax.unsqueeze(2).to_broadcast([128, NT, 8]), op=ALU.is_equal), [], [b_R])
            S.op("dve", lambda e: e.tensor_tensor(out=q3(ge), in0=q3(sel), in1=m2.unsqueeze(2).to_broadcast([128, NT * 8, 4]), op=ALU.is_ge), [], [b_R])
            S.op("dve", lambda e: e.tensor_tensor(out=q3(msk), in0=q3(ge), in1=gone.unsqueeze(2).to_broadcast([128, NT * 8, 4]), op=ALU.mult), [], [b_R])
            S.op("dve", lambda e: e.tensor_tensor(out=gv, in0=aff, in1=msk, op=ALU.mult), [b_affall], [b_R])
            S.op("dve", lambda e: e.tensor_reduce(out=gsum, in_=t3(gv), axis=AX.X, op=ALU.add), [], [b_R])
            S.op("dve", lambda e: e.reciprocal(out=rgs, in_=gsum), [], [b_R])
            S.op("dve", lambda e: e.tensor_tensor(out=t3(gv), in0=t3(gv), in1=rgs.unsqueeze(2).to_broadcast([128, NT, NE]), op=ALU.mult), [], [b_R])
            S.op("dve", lambda e: e.tensor_copy(out=mk[:], in_=msk), [b_R], [b_mk])
            S.op("pe", lambda e: e.matmul(ppre[:], lhsT=tris[:], rhs=mk[:], start=True, stop=True), [b_tris, b_mk], [b_ppre])
            S.op("pe", lambda e: e.matmul(ptot[:], lhsT=ones[:], rhs=mk[:], start=True, stop=True), [b_ones, b_mk], [b_ptot])
            S.op("dve", lambda e: e.tensor_copy(out=dd, in_=ptot[:]), [], [b_ptot, b_R])
            S.op("dve", lambda e: e.memset(cntf[:, 0:NE], 0.0), [], [b_R])
            for t in range(1, NT):
                S.op("dve", lambda e: e.tensor_tensor(out=cntf[:, t * NE:(t + 1) * NE], in0=cntf[:, (t - 1) * NE:t * NE],
                                                      in1=dd[:, (t - 1) * NE:t * NE], op=ALU.add), [], [b_R])
            S.op("dve", lambda e: e.tensor_tensor(out=pos, in0=ppre[:], in1=cntf, op=ALU.add), [], [b_ppre, b_R])
            S.op("dve", lambda e: e.tensor_scalar(out=vld, in0=pos, scalar1=float(CAP) - 0.5, scalar2=None, op0=ALU.is_lt), [], [b_R])
            S.op("dve", lambda e: e.tensor_tensor(out=vld, in0=vld, in1=msk, op=ALU.mult), [], [b_R])
            S.op("dve", lambda e: e.tensor_tensor(out=t3(val), in0=t3(pos), in1=eoff[:].unsqueeze(1).to_broadcast([128, NT, NE]), op=ALU.add),
                 [b_eoff], [b_R])
            S.op("dve", lambda e: e.scalar_tensor_tensor(out=dd, in0=val, scalar=1.0, in1=vld, op0=ALU.add, op1=ALU.mult), [], [b_R])
            S.op("dve", lambda e: e.tensor_scalar_add(out=dd, in0=dd, scalar1=-1.0), [], [b_R])
            S.op("dve", lambda e: e.tensor_reduce(out=first, in_=t3(dd), axis=AX.X, op=ALU.max), [], [b_R])
            S.op("dve", lambda e: e.tensor_tensor(out=t3(eq), in0=t3(dd), in1=first.unsqueeze(2).to_broadcast([128, NT, NE]), op=ALU.is_equal), [], [b_R])
            S.op("dve", lambda e: e.scalar_tensor_tensor(out=selm, in0=eq, scalar=-1e9, in1=dd, op0=ALU.mult, op1=ALU.add), [], [b_R])
            S.op("dve", lambda e: e.tensor_reduce(out=second, in_=t3(selm), axis=AX.X, op=ALU.max), [], [b_R])
            S.op("dve", lambda e: e.tensor_tensor(out=gv, in0=gv, in1=vld, op=ALU.mult), [], [b_R])
            S.op("dve", lambda e: e.tensor_reduce(out=gts, in_=t3(gv), axis=AX.X, op=ALU.add), [], [b_R])
            S.op("dve", lambda e: e.tensor_tensor(out=ge, in0=gv, in1=eq, op=ALU.mult), [], [b_R])
            S.op("dve", lambda e: e.tensor_reduce(out=g1s, in_=t3(ge), axis=AX.X, op=ALU.add), [], [b_R])
            S.op("dve", lambda e: e.tensor_copy(out=gate2[:, :, 0], in_=g1s), [b_R], [b_rt])
            S.op("dve", lambda e: e.tensor_tensor(out=gate2[:, :, 1], in0=gts, in1=g1s, op=ALU.subtract), [b_R], [b_rt])
            fs = r16[:, 3:5, :]
            neg = r16[:, 5:7, :]
            dS = destS[:].rearrange("p (t s) -> p s t", s=2)
            dG = destG[:].rearrange("p (t s) -> p s t", s=2)
            S.op("dve", lambda e: e.tensor_scalar(out=neg, in0=fs, scalar1=0.0, scalar2=BIGROW + 1.0, op0=ALU.is_lt, op1=ALU.mult), [b_rt], [b_R])
            S.op("dve", lambda e: e.tensor_tensor(out=neg, in0=neg, in1=fs, op=ALU.add), [], [b_R])
            S.op("dve", lambda e: e.tensor_copy(out=dS, in_=neg), [b_R], [b_rt])
            S.op("dve", lambda e: e.tensor_scalar_max(out=neg, in0=fs, scalar1=0.0), [b_rt], [b_R])
            S.op("dve", lambda e: e.tensor_copy(out=dG, in_=neg), [b_R], [b_rt])
            for t in range(NT):
                for sidx in range(2):
                    scat.append(Buf())
                    S.dma_fn("pool", lambda e: e.indirect_dma_start(
                        out=d["xbuf"][:, :], out_offset=bass.IndirectOffsetOnAxis(ap=destS[:, 2 * t + sidx:2 * t + sidx + 1], axis=0),
                        in_=u2b_all[:, t, :], in_offset=None, bounds_check=bc_reg, oob_is_err=False),
                        [b_u2b[t], b_rt] + zfill, [scat[-1]])
        S.barrier()
        if "dbg_dest" in d:
            for nm, src in (("dbg_dest", destS), ("dbg_gate", gate2)):
                outs.append(Buf())
                S.dma("sp", d[nm], src[:], reads=[b_rt], writes=[outs[-1]])
        ysc = []
        with ExitStack() as es:
            def sbn(shape, dt, n=2):
                return [cx.sb(shape, dt, es) for _ in range(n)], [Buf() for _ in range(n)]

            wg, b_wg = sbn([128, 8, DFF], BF16)
            wu, b_wu = sbn([128, 8, DFF], BF16)
            wd, b_wd = sbn([128, 4, D], BF16)
            Xe, b_Xe = sbn([128, CAP // 128, D], BF16)
            XT, b_XT = sbn([128, 8, CAP], BF16)
            hT, b_hT = sbn([128, 4, CAP], BF16)
            sg, b_sg = sbn([128, CAP], F32)
            Ye, b_Ye = sbn([128, D], F32)
            pX = cx.ps([128, 8, 128], BF16, es); b_pX = Buf()
            pg = [cx.ps([128, 512], F32, es) for _ in range(2)]
            b_pg = [Buf(), Buf()]
            pu = [cx.ps([128, 512], F32, es) for _ in range(2)]
            b_pu = [Buf(), Buf()]
            pd = [cx.ps([128, 512], F32, es) for _ in range(2)]
            b_pd = [Buf(), Buf()]
            wg_v = d["w_gate"].rearrange("e (k p) n -> e p k n", p=128)
            wu_v = d["w_up"].rearrange("e (k p) n -> e p k n", p=128)
            wd_v = d["w_down"].rearrange("e (k p) n -> e p k n", p=128)
            nst = CAP // 128
            pcnt = 0
            dcnt = 0
            for ex in range(NE):
                i = ex % 2
                S.dma("pool", wg[i][:], wg_v[ex], writes=[b_wg[i]])
                S.dma("pool", wu[i][:], wu_v[ex], writes=[b_wu[i]])
                S.dma("pool", wd[i][:], wd_v[ex], writes=[b_wd[i]])
                S.dma("sp", Xe[i][:], d["xbuf"][ex * CAP:(ex + 1) * CAP, :].rearrange("(s p) n -> p s n", p=128),
                      reads=scat, writes=[b_Xe[i]])
                for st in range(nst):
                    for k in range(8):
                        S.op("pe", lambda e: e.transpose(pX[:, k, :], Xe[i][:, st, k * 128:(k + 1) * 128], ident[:]),
                             [b_Xe[i], b_id], [b_pX])
                    if st % 2 == 0:
                        S.op("act", lambda e: e.activation(out=XT[i][:, :, st * 128:(st + 1) * 128], in_=pX[:], func=AF.Copy),
                             [], [b_pX, b_XT[i]])
                    else:
                        S.op("dve", lambda e: e.tensor_copy(out=XT[i][:, :, st * 128:(st + 1) * 128], in_=pX[:]),
                             [], [b_pX, b_XT[i]])
                for f in range(4):
                    pi = pcnt % 2
                    pcnt += 1
                    for k in range(8):
                        S.op("pe", lambda e: e.matmul(pg[pi][:, 0:CAP], lhsT=wg[i][:, k, f * 128:(f + 1) * 128], rhs=XT[i][:, k, :],
                                                      start=(k == 0), stop=(k == 7)), [b_wg[i], b_XT[i]], [b_pg[pi]])
                    for k in range(8):
                        S.op("pe", lambda e: e.matmul(pu[pi][:, 0:CAP], lhsT=wu[i][:, k, f * 128:(f + 1) * 128], rhs=XT[i][:, k, :],
                                                      start=(k == 0), stop=(k == 7)), [b_wu[i], b_XT[i]], [b_pu[pi]])
                    S.op("act", lambda e: e.activation(out=sg[pi][:], in_=pg[pi][:, 0:CAP], func=AF.Silu), [], [b_pg[pi], b_sg[pi]])
                    S.op("dve", lambda e: e.tensor_tensor(out=hT[i][:, f, :], in0=pu[pi][:, 0:CAP], in1=sg[pi][:], op=ALU.mult),
                         [b_sg[pi]], [b_pu[pi], b_hT[i]])
                for st in range(nst):
                    yi = dcnt % 2
                    dcnt += 1
                    for half in range(2):
                        hs = slice(half * 512, (half + 1) * 512)
                        for f in range(4):
                            S.op("pe", lambda e: e.matmul(pd[half][:], lhsT=hT[i][:, f, st * 128:(st + 1) * 128], rhs=wd[i][:, f, hs],
                                                          start=(f == 0), stop=(f == 3)), [b_hT[i], b_wd[i]], [b_pd[half]])
                        if half == 0:
                            S.op("act", lambda e: e.activation(out=Ye[yi][:, hs], in_=pd[half][:], func=AF.Copy),
                                 [], [b_pd[half], b_Ye[yi]])
                        else:
                            S.op("dve", lambda e: e.tensor_copy(out=Ye[yi][:, hs], in_=pd[half][:]), [], [b_pd[half], b_Ye[yi]])
                    ysc.append(Buf())
                    S.dma("act", d["ybuf"][ex * CAP + st * 128:ex * CAP + (st + 1) * 128, :], Ye[yi][:],
                          reads=[b_Ye[yi]], writes=[ysc[-1]])
            S.barrier()

        with ExitStack() as es:
            def sbn(shape, dt, n=2):
                return [cx.sb(shape, dt, es) for _ in range(n)], [Buf() for _ in range(n)]

            lnp = cx.sb([128, 2, D], F32, es); b_lnp = Buf()
            S.dma("sp", lnp[:], d["lnp"][:, 2 * D:4 * D].partition_broadcast(128).rearrange("p o (a n) -> p (o a) n", a=2), writes=[b_lnp])
            Yg, b_Yg = sbn([128, 2 * D], F32, 3)
            x1r, b_x1r = sbn([128, D], F32, 3)
            acc, b_acc = sbn([128, D], F32)
            z_, b_z_ = sbn([128, D], F32)
            xn = cx.sb([128, D], F32, es); b_xn = Buf()
            tmp = cx.sb([128, D], F32, es); b_tmp = Buf()
            xo, b_xo = sbn([128, D], F32)
            lns = LNScratch(cx, es)

            def G(t):
                i = t % 3
                S.dma("sp", x1r[i][:], d["x1s"][t * 128:(t + 1) * 128, :], reads=x1w, writes=[b_x1r[i]])
                for sidx in range(2):
                    S.dma_fn("pool", lambda e: e.indirect_dma_start(
                        out=Yg[i][:, sidx * D:(sidx + 1) * D], out_offset=None, in_=d["ybuf"][:, :],
                        in_offset=bass.IndirectOffsetOnAxis(ap=destG[:, 2 * t + sidx:2 * t + sidx + 1], axis=0),
                        bounds_check=bc_reg, oob_is_err=False), [b_rt] + ysc, [b_Yg[i]])

            lns2 = [lns, LNScratch(cx, es)]
            xn2 = [xn, cx.sb([128, D], F32, es)]
            b_xn2 = [b_xn, Buf()]
            tmp2 = [tmp, cx.sb([128, D], F32, es)]
            b_tmp2 = [b_tmp, Buf()]

            def Cmb(t):
                i = t % 3
                j = t % 2
                S.op("dve", lambda e: e.tensor_scalar_mul(out=acc[j][:], in0=Yg[i][:, 0:D], scalar1=gate2[:, t, 0:1]),
                     [b_Yg[i], b_rt], [b_acc[j]])
                yield
                S.op("dve", lambda e: e.scalar_tensor_tensor(out=acc[j][:], in0=Yg[i][:, D:2 * D], scalar=gate2[:, t, 1:2],
                                                             in1=acc[j][:], op0=ALU.mult, op1=ALU.add),
                     [b_Yg[i], b_rt], [b_acc[j]])
                yield
                S.op("pool", lambda e: e.tensor_tensor(out=acc[j][:], in0=acc[j][:], in1=g2_ap, op=ALU.mult), [b_ada], [b_acc[j]])
                yield
                S.op("dve", lambda e: e.scalar_tensor_tensor(out=z_[j][:], in0=x1r[i][:], scalar=ALPHA, in1=acc[j][:],
                                                             op0=ALU.mult, op1=ALU.add), [b_x1r[i], b_acc[j]], [b_z_[j]])
                yield

            def Fin(t):
                j = t % 2
                yield from g_ln_mod(cx, z_[j][:], b_z_[j], lns2[j], xn2[j], b_xn2[j], lnp[:, 0, :], lnp[:, 1, :], b_lnp,
                                    xo[j][:], b_xo[j], tmp2[j], b_tmp2[j])
                outs.append(Buf())
                S.dma("sp", xout_d[t * 128:(t + 1) * 128, :], xo[j][:], reads=[b_xo[j]], writes=[outs[-1]])
                yield

            G(0)
            G(1)
            for kk in range(NT + 1):
                if kk + 2 < NT:
                    G(kk + 2)
                round_robin([Cmb(kk) if kk < NT else None, Fin(kk - 1) if 0 <= kk - 1 < NT else None])
            S.barrier()
        S.barrier()
    return outs


def pack_C_inputs(inp, l, j, x_shard, yT_shard, ada_row, hc):
    lnp = np.concatenate([inp["ln_g"][l, 0], inp["ln_b"][l, 0], inp["ln_g"][l, 1], inp["ln_b"][l, 1]])[None, :]
    return {"x": x_shard, "yT": yT_shard, "ada": ada_row, "w_out": np.ascontiguousarray(inp["w_out"][l]),
            "lnp": np.ascontiguousarray(lnp), "w_router": inp["w_router"], "b_router": inp["b_router"][None, :],
            "eoff": hc["eoff"], "w_gate": np.ascontiguousarray(inp["w_gate"][l]), "w_up": np.ascontiguousarray(inp["w_up"][l]),
            "w_down": np.ascontiguousarray(inp["w_down"][l]), "ident_bf": hc["ident_bf"], "ident_f": hc["ident_f"],
            "tris_bf": hc["tris_bf"], "ones_bf": hc["ones_bf"]}


def _run(nc, in_maps):
    res = run_bass_kernel_spmd(nc, in_maps, core_ids=list(range(NCORES)))
    return res.results


def kernel_unfused(**inputs):
    inp = {k: np.asarray(v) for k, v in inputs.items()}
    hc = host_consts()
    x = np.ascontiguousarray(inp["x"][0], dtype=np.float32)
    cT = np.ascontiguousarray(inp["c"][0].reshape(8, 128).T)
    for l in range(DEPTH):
        ra = _run(build_A(), [{"x": np.ascontiguousarray(x[j * TPC:(j + 1) * TPC]), "cT": cT,
                               "w_ada": np.ascontiguousarray(inp["w_ada"][l]),
                               "b_ada": np.ascontiguousarray(inp["b_ada"][l][None, :]),
                               "ident_bf": hc["ident_bf"]} for j in range(NCORES)])
        uT = np.ascontiguousarray(np.concatenate([r["uT"] for r in ra], axis=1))
        ada = ra[0]["ada"]
        rb = _run(build_B(), [pack_B_inputs(inp, l, h, uT, hc) for h in range(NCORES)])
        yT = np.concatenate([r["yT"][0:128] for r in rb] + [r["yT"][128:256] for r in rb], axis=0)
        rc = _run(build_C(), [pack_C_inputs(inp, l, j, np.ascontiguousarray(x[j * TPC:(j + 1) * TPC]),
                                            np.ascontiguousarray(yT[:, j * TPC:(j + 1) * TPC]), ada, hc)
                              for j in range(NCORES)])
        x = np.concatenate([r["xout"] for r in rc], axis=0)
    return x[None].astype(np.float32)


def kernel(**inputs):
    return kernel_unfused(**inputs)
```

```python
import numpy as np
import ml_dtypes
from contextlib import ExitStack
import concourse.bass as bass
import concourse.mybir as mybir
from concourse.bass_utils import run_bass_kernel_spmd

F32 = mybir.dt.float32
BF16 = mybir.dt.bfloat16
I32 = mybir.dt.int32
AF = mybir.ActivationFunctionType
ALU = mybir.AluOpType
AX = mybir.AxisListType

NCORES = 8
D = 1024
SEQ = 16384
TPC = SEQ // NCORES
NT = TPC // 128
DEPTH = 2
DH = 128
NE = 32
DFF = 512
CAP = 256
ALPHA = (2 * DEPTH) ** 0.25
EPS = 1e-5
BLK = 512
NBLK = SEQ // BLK


class Buf:
    __slots__ = ("w", "r")

    def __init__(self):
        self.w = None
        self.r = {}


class Sched:
    ND = 8

    def __init__(self, nc, es):
        self.nc = nc
        self.engs = {"pe": nc.tensor, "dve": nc.vector, "act": nc.scalar, "pool": nc.gpsimd, "sp": nc.sync}
        self.semh = {}
        for e in self.engs:
            self.semh[e] = es.enter_context(nc.semaphore(f"s_{e}"))
        self.cnt = {e: 0 for e in self.engs}
        self.seen = {e: {} for e in self.engs}
        self.semh["cc"] = es.enter_context(nc.semaphore("s_cc"))
        self.ccn = 0
        self.dqn = {}
        for q in ("sp", "act", "pool"):
            self.dqn[q] = 0
            for i in range(self.ND):
                self.semh[(q, i)] = es.enter_context(nc.semaphore(f"d_{q}{i}"))

    def _wait(self, e, key, val):
        if self.seen[e].get(key, 0) >= val:
            return
        self.engs[e].wait_ge(self.semh[key], val)
        self.seen[e][key] = val

    def _deps(self, e, reads, writes):
        deps = {}
        for b in reads:
            if b.w is not None:
                k, v = b.w
                if deps.get(k, 0) < v:
                    deps[k] = v
        for b in writes:
            if b.w is not None:
                k, v = b.w
                if deps.get(k, 0) < v:
                    deps[k] = v
            for k, v in b.r.items():
                if deps.get(k, 0) < v:
                    deps[k] = v
        for k, v in deps.items():
            if e == "pe" and k == "pe":
                continue
            self._wait(e, k, v)

    def _commit(self, tok, reads, writes):
        k, v = tok
        for b in reads:
            if b.r.get(k, 0) < v:
                b.r[k] = v
        for b in writes:
            b.w = tok
            b.r = {}

    def op(self, e, fn, reads=(), writes=()):
        self._deps(e, reads, writes)
        inst = fn(self.engs[e])
        self.cnt[e] += 1
        inst.then_inc(self.semh[e], 1)
        self._commit((e, self.cnt[e]), reads, writes)
        return inst

    def dma(self, q, out, in_, reads=(), writes=(), **kw):
        return self.dma_fn(q, lambda e: e.dma_start(out=out, in_=in_, **kw), reads, writes)

    def dma_fn(self, q, fn, reads=(), writes=()):
        n = self.dqn[q]
        i = n % self.ND
        rnd = n // self.ND
        self.dqn[q] = n + 1
        key = (q, i)
        if rnd > 0:
            self._wait(q, key, 16 * rnd)
        self._deps(q, reads, writes)
        inst = fn(self.engs[q])
        inst.then_inc(self.semh[key], 16)
        self._commit((key, 16 * (rnd + 1)), reads, writes)
        return inst

    def cc(self, fn, reads=(), writes=()):
        self._deps("pool", reads, writes)
        inst = fn(self.engs["pool"])
        self.ccn += 1
        inst.then_inc(self.semh["cc"], 1)
        self._commit(("cc", self.ccn), reads, writes)
        return inst

    def finish(self, bufs, e="sp"):
        self._deps(e, bufs, bufs)

    def barrier(self):
        for e in self.engs:
            for f in self.engs:
                if f != e and self.cnt[f] > 0:
                    self._wait(e, f, self.cnt[f])
            if self.ccn > 0:
                self._wait(e, "cc", self.ccn)
            for q in ("sp", "act", "pool"):
                n = self.dqn[q]
                for i in range(self.ND):
                    c = (n - i + self.ND - 1) // self.ND if n > i else 0
                    if c > 0:
                        self._wait(e, (q, i), 16 * c)


class Ctx:
    def __init__(self, nc, es):
        self.nc = nc
        self.es = es
        self.S = Sched(nc, es)
        self._n = 0

    def sb(self, shape, dt, es=None, name=None):
        self._n += 1
        return (es or self.es).enter_context(self.nc.sbuf_tensor(name or f"sb{self._n}", list(shape), dt))

    def ps(self, shape, dt, es=None, name=None):
        self._n += 1
        return (es or self.es).enter_context(self.nc.psum_tensor(name or f"ps{self._n}", list(shape), dt))

    def dram_in(self, name, shape, dt):
        return self.nc.dram_tensor(name, list(shape), dt, kind="ExternalInput").ap()

    def dram_out(self, name, shape, dt):
        return self.nc.dram_tensor(name, list(shape), dt, kind="ExternalOutput").ap()


def host_consts():
    c = {}
    c["ident_bf"] = np.eye(128, dtype=np.float32).astype(ml_dtypes.bfloat16)
    c["ident_f"] = np.eye(128, dtype=np.float32)
    r = np.arange(128)
    c["tri_f"] = (r[:, None] <= r[None, :]).astype(np.float32)
    c["ones_f"] = np.ones((128, 128), np.float32)
    c["ones_bf"] = np.ones((128, 128), np.float32).astype(ml_dtypes.bfloat16)
    c["tris_bf"] = (r[:, None] < r[None, :]).astype(np.float32).astype(ml_dtypes.bfloat16)
    c["eoff"] = (np.arange(NE, dtype=np.float32) * CAP)[None, :]
    return c


def emit_ada(cx, cT_d, w_ada_d, b_ada_d, ada_bc, b_ada_bc):
    S = cx.S
    with ExitStack() as es:
        cT = cx.sb([128, 8], F32, es)
        b_cT = Buf()
        cond = cx.sb([128, 8], F32, es)
        b_cond = Buf()
        condB = cx.sb([128, 8, 128], F32, es)
        b_condB = Buf()
        bias = cx.sb([128, 6 * D], F32, es)
        b_bias = Buf()
        wbuf = [cx.sb([128, 8, 512], F32, es) for _ in range(2)]
        b_w = [Buf(), Buf()]
        pp = [cx.ps([128, 512], F32, es) for _ in range(2)]
        b_pp = [Buf(), Buf()]
        S.dma("sp", cT[:], cT_d, writes=[b_cT])
        S.dma("sp", bias[:], b_ada_d.partition_broadcast(128), writes=[b_bias])
        S.op("act", lambda e: e.activation(out=cond[:], in_=cT[:], func=AF.Silu), [b_cT], [b_cond])
        for k in range(8):
            S.op("dve", lambda e: e.tensor_copy(out=condB[:, k, :], in_=cond[:, k:k + 1].to_broadcast([128, 128])),
                 [b_cond], [b_condB])
        wv = w_ada_d.rearrange("(k p) n -> p k n", p=128)
        for j in range(12):
            w = wbuf[j % 2]
            S.dma("sp" if j % 2 == 0 else "act", w[:], wv[:, :, j * 512:(j + 1) * 512], writes=[b_w[j % 2]])
            p = pp[j % 2]
            for k in range(8):
                S.op("pe", lambda e: e.matmul(p[:], lhsT=condB[:, k, :], rhs=w[:, k, :], start=(k == 0), stop=(k == 7)),
                     [b_condB, b_w[j % 2]], [b_pp[j % 2]])
            S.op("dve", lambda e: e.tensor_tensor(out=ada_bc[:, j * 512:(j + 1) * 512], in0=p[:],
                                                  in1=bias[:, j * 512:(j + 1) * 512], op=ALU.add),
                 [b_pp[j % 2], b_bias], [b_ada_bc])
        S.barrier()


def emit_ln_stats(cx, x_ap, b_x, st, mv, rs, nb, b_st):
    S = cx.S
    S.op("dve", lambda e: e.bn_stats(out=st[:, 0:6], in_=x_ap[:, 0:512]), [b_x], [b_st])
    S.op("dve", lambda e: e.bn_stats(out=st[:, 6:12], in_=x_ap[:, 512:1024]), [b_x], [b_st])
    S.op("dve", lambda e: e.bn_aggr(out=mv[:], in_=st[:]), [b_st], [b_st])
    S.op("act", lambda e: e.activation(out=rs[:], in_=mv[:, 1:2], func=AF.Sqrt, bias=EPS, scale=1.0), [b_st], [b_st])
    S.op("dve", lambda e: e.reciprocal(out=rs[:], in_=rs[:]), [b_st], [b_st])
    S.op("dve", lambda e: e.scalar_tensor_tensor(out=nb[:], in0=mv[:, 0:1], scalar=-1.0, in1=rs[:],
                                                 op0=ALU.mult, op1=ALU.mult), [b_st], [b_st])


def g_ln_mod(cx, x_ap, b_x, lns, xn, b_xn, A_ap, B_ap, b_ab, out_ap, b_out, tmp, b_tmp):
    S = cx.S
    st, mv, rs, nb, b_st = lns.st, lns.mv, lns.rs, lns.nb, lns.b
    S.op("dve", lambda e: e.bn_stats(out=st[:, 0:6], in_=x_ap[:, 0:512]), [b_x], [b_st]); yield
    S.op("dve", lambda e: e.bn_stats(out=st[:, 6:12], in_=x_ap[:, 512:1024]), [b_x], [b_st]); yield
    S.op("dve", lambda e: e.bn_aggr(out=mv[:], in_=st[:]), [b_st], [b_st]); yield
    S.op("act", lambda e: e.activation(out=rs[:], in_=mv[:, 1:2], func=AF.Sqrt, bias=EPS, scale=1.0), [b_st], [b_st]); yield
    S.op("dve", lambda e: e.reciprocal(out=rs[:], in_=rs[:]), [b_st], [b_st]); yield
    S.op("dve", lambda e: e.scalar_tensor_tensor(out=nb[:], in0=mv[:, 0:1], scalar=-1.0, in1=rs[:],
                                                 op0=ALU.mult, op1=ALU.mult), [b_st], [b_st]); yield
    S.op("act", lambda e: e.activation(out=xn[:], in_=x_ap, func=AF.Identity, bias=nb[:], scale=rs[:]), [b_x, b_st], [b_xn]); yield
    S.op("dve", lambda e: e.tensor_tensor(out=tmp[:], in0=xn[:], in1=A_ap, op=ALU.mult), [b_xn, b_ab], [b_tmp]); yield
    S.op("pool", lambda e: e.tensor_tensor(out=out_ap, in0=tmp[:], in1=B_ap, op=ALU.add), [b_tmp, b_ab], [b_out]); yield


def round_robin(gens):
    gens = [g for g in gens if g is not None]
    while gens:
        nxt = []
        for g in gens:
            try:
                next(g)
                nxt.append(g)
            except StopIteration:
                pass
        gens = nxt


class LNScratch:
    def __init__(self, cx, es):
        self.st = cx.sb([128, 12], F32, es)
        self.mv = cx.sb([128, 2], F32, es)
        self.rs = cx.sb([128, 1], F32, es)
        self.nb = cx.sb([128, 1], F32, es)
        self.b = Buf()


def emit_ln_mod(cx, x_ap, b_x, lns, xn, b_xn, A_ap, B_ap, b_ab, out_ap, b_out, tmp, b_tmp):
    S = cx.S
    emit_ln_stats(cx, x_ap, b_x, lns.st, lns.mv, lns.rs, lns.nb, lns.b)
    S.op("act", lambda e: e.activation(out=xn[:], in_=x_ap, func=AF.Identity, bias=lns.nb[:], scale=lns.rs[:]),
         [b_x, lns.b], [b_xn])
    S.op("dve", lambda e: e.tensor_tensor(out=tmp[:], in0=xn[:], in1=A_ap, op=ALU.mult), [b_xn, b_ab], [b_tmp])
    S.op("pool", lambda e: e.tensor_tensor(out=out_ap, in0=tmp[:], in1=B_ap, op=ALU.add), [b_tmp, b_ab], [b_out])


def build_A():
    nc = bass.Bass("TRN2", target_bir_lowering=False)
    with ExitStack() as es:
        cx = Ctx(nc, es)
        S = cx.S
        x_d = cx.dram_in("x", [TPC, D], F32)
        cT_d = cx.dram_in("cT", [128, 8], F32)
        w_ada_d = cx.dram_in("w_ada", [D, 6 * D], F32)
        b_ada_d = cx.dram_in("b_ada", [1, 6 * D], F32)
        ident_d = cx.dram_in("ident_bf", [128, 128], BF16)
        uT_d = cx.dram_out("uT", [D, TPC], BF16)
        ada_d = cx.dram_out("ada", [1, 6 * D], F32)

        ada = cx.sb([128, 6 * D], F32)
        b_ada = Buf()
        emit_ada(cx, cT_d, w_ada_d, b_ada_d, ada, b_ada)
        b_adaout = Buf()
        S.dma("sp", ada_d, ada[0:1, :], reads=[b_ada], writes=[b_adaout])
        S.finish([b_adaout], "sp")
        ident = cx.sb([128, 128], BF16)
        b_id = Buf()
        S.dma("sp", ident[:], ident_d, writes=[b_id])
        S.op("dve", lambda e: e.tensor_scalar_add(out=ada[:, D:2 * D], in0=ada[:, D:2 * D], scalar1=1.0), [b_ada], [b_ada])
        emit_A_body(cx, x_d, None, None, ada, b_ada, ident, b_id, uT_d)
    return nc


def emit_A_body(cx, x_d, xres, b_xres, ada, b_ada, ident, b_id, uT_d, sh_off=0, sc_off=D):
    S = cx.S
    with ExitStack() as es:
        lns = LNScratch(cx, es)
        xin = [cx.sb([128, D], F32, es) for _ in range(2)]
        b_xin = [Buf(), Buf()]
        xn = cx.sb([128, D], F32, es)
        b_xn = Buf()
        tmp = cx.sb([128, D], F32, es)
        b_tmp = Buf()
        ub = [cx.sb([128, D], BF16, es) for _ in range(2)]
        b_ub = [Buf(), Buf()]
        pT = [cx.ps([128, 8, 128], BF16, es) for _ in range(2)]
        b_pT = [Buf(), Buf()]
        uTs = [cx.sb([128, 8, 128], BF16, es) for _ in range(2)]
        b_uTs = [Buf(), Buf()]
        outs = []
        uT_v = uT_d.rearrange("(k p) t -> p k t", p=128)
        lns2 = [lns, LNScratch(cx, es)]
        xn2 = [xn, cx.sb([128, D], F32, es)]
        b_xn2 = [b_xn, Buf()]
        tmp2 = [tmp, cx.sb([128, D], F32, es)]
        b_tmp2 = [b_tmp, Buf()]

        def Sa(t):
            i = t % 2
            if x_d is not None:
                S.dma("sp", xin[i][:], x_d[t * 128:(t + 1) * 128, :], writes=[b_xin[i]])
                x_ap, bx = xin[i][:], b_xin[i]
            else:
                x_ap, bx = xres[:, t, :], b_xres[t]
            yield from g_ln_mod(cx, x_ap, bx, lns2[i], xn2[i], b_xn2[i], ada[:, sc_off:sc_off + D], ada[:, sh_off:sh_off + D], b_ada,
                                ub[i][:], b_ub[i], tmp2[i], b_tmp2[i])

        def Sb(t):
            i = t % 2
            for k in range(8):
                S.op("pe", lambda e: e.transpose(pT[i][:, k, :], ub[i][:, k * 128:(k + 1) * 128], ident[:]),
                     [b_ub[i], b_id], [b_pT[i]])
                if k % 4 == 3:
                    yield
            S.op("act", lambda e: e.activation(out=uTs[i][:], in_=pT[i][:], func=AF.Copy), [b_pT[i]], [b_uTs[i]])
            yield
            outs.append(Buf())
            S.dma("sp", uT_v[:, :, t * 128:(t + 1) * 128], uTs[i][:], reads=[b_uTs[i]], writes=[outs[-1]])
            yield

        for kk in range(NT + 1):
            round_robin([Sa(kk) if kk < NT else None, Sb(kk - 1) if 0 <= kk - 1 < NT else None])
        S.finish(outs, "sp")
        S.barrier()


LN_INV_SQRT_DH = float(-0.5 * np.log(DH))
GELU_C = 1.5957691216057308


def build_B():
    nc = bass.Bass("TRN2", target_bir_lowering=False)
    with ExitStack() as es:
        cx = Ctx(nc, es)
        d = {
            "uT": cx.dram_in("uT", [D, SEQ], BF16),
            "wh": cx.dram_in("wh", [D, 770], F32),
            "pp": cx.dram_in("pp", [128, 24], F32),
            "fv": cx.dram_in("fv", [1, 258], F32),
            "wa": cx.dram_in("wa", [128, 128], F32),
            "wx": cx.dram_in("wx", [128, 128], F32),
            "ident_bf": cx.dram_in("ident_bf", [128, 128], BF16),
            "tri_f": cx.dram_in("tri_f", [128, 128], F32),
            "ones_f": cx.dram_in("ones_f", [128, 128], F32),
            "ones_bf": cx.dram_in("ones_bf", [128, 128], BF16),
            "yT": cx.dram_out("yT", [256, SEQ], BF16),
        }
        outs = emit_B(cx, d)
        cx.S.finish(outs, "sp")
    return nc


def emit_B(cx, d, nblk=NBLK):
    S = cx.S
    outs = []
    NCH = BLK // 128
    with ExitStack() as es:
        def sb(shape, dt):
            return cx.sb(shape, dt, es)

        def sbn(shape, dt, n=2):
            return [cx.sb(shape, dt, es) for _ in range(n)], [Buf() for _ in range(n)]

        W = sb([128, 8, 770], BF16); b_W = Buf()
        S.dma("pool", W[:], d["wh"].rearrange("(k p) n -> p k n", p=128), writes=[b_W])
        wa = sb([128, 128], BF16); b_wa = Buf()
        S.dma("pool", wa[:], d["wa"], writes=[b_wa])
        wx = sb([128, 128], BF16); b_wx = Buf()
        S.dma("pool", wx[:], d["wx"], writes=[b_wx])
        pp = sb([128, 24], F32); b_pp = Buf()
        S.dma("sp", pp[:], d["pp"], writes=[b_pp])
        fv = sb([128, 258], F32); b_fv = Buf()
        S.dma("sp", fv[:], d["fv"].partition_broadcast(128), writes=[b_fv])
        ident = sb([128, 128], BF16); b_id = Buf()
        S.dma("sp", ident[:], d["ident_bf"], writes=[b_id])
        tri = sb([128, 128], F32); b_tri = Buf()
        S.dma("sp", tri[:], d["tri_f"], writes=[b_tri])
        ones_f = sb([128, 128], F32); b_of = Buf()
        S.dma("sp", ones_f[:], d["ones_f"], writes=[b_of])
        ones_bf = sb([128, 128], BF16); b_ob = Buf()
        S.dma("sp", ones_bf[:], d["ones_bf"], writes=[b_ob])
        der = sb([128, 4], F32); b_der = Buf()
        S.op("act", lambda e: e.activation(out=der[:, 3:4], in_=pp[:, 21:22], func=AF.Exp, scale=-1.0), [b_pp], [b_der])
        S.op("act", lambda e: e.activation(out=der[:, 3:4], in_=der[:, 3:4], func=AF.Ln, bias=1.0, scale=1.0), [b_der], [b_der])
        S.op("dve", lambda e: e.tensor_scalar_mul(out=der[:, 0:1], in0=der[:, 3:4], scalar1=-8.0), [b_der], [b_der])
        S.op("dve", lambda e: e.tensor_scalar_mul(out=der[:, 1:2], in0=der[:, 3:4], scalar1=-16.0), [b_der], [b_der])
        S.op("dve", lambda e: e.tensor_scalar_mul(out=der[:, 2:3], in0=pp[:, 22:23], scalar1=float(np.sqrt(128.0))),
             [b_pp, b_der], [b_der])

        dg = {}
        b_dg = Buf()
        for nm, wc in (("q", 0), ("k", 4), ("x", 8)):
            dg[nm] = sb([128, 4, 128], BF16)
            for k in range(4):
                S.op("dve", lambda e: e.tensor_scalar_mul(out=dg[nm][:, k, :], in0=ident[:], scalar1=pp[:, wc + k:wc + k + 1]),
                     [b_id, b_pp], [b_dg])
        C32 = sb([128, 129], F32); b_C32 = Buf()
        Cbf = sb([128, 129], BF16); b_Cbf = Buf()
        S.op("dve", lambda e: e.memset(C32[:], 0.0), [], [b_C32])
        S.op("dve", lambda e: e.memset(Cbf[:], 0.0), [], [b_Cbf])
        hz = sb([128, 1], F32); b_hz = Buf()
        S.op("dve", lambda e: e.memset(hz[:], 0.0), [], [b_hz])

        uTb, b_uTb = sbn([128, 8, BLK], BF16)
        pre = {}
        b_pre = {}
        for nm in ("q", "k", "x"):
            pre[nm], b_pre[nm] = sbn([128, BLK + 8], BF16)
            for i in range(2):
                S.op("pool", lambda e: e.memset(pre[nm][i][:, 0:3], 0.0), [], [b_pre[nm][i]])
        gpre, b_gpre = sbn([128, BLK], F32, 3)
        acc = {}
        b_acc = {}
        for nm in ("x",):
            acc[nm], b_acc[nm] = sbn([128, BLK], F32, 3)
        qT, b_qT = sbn([128, BLK], BF16)
        kT, b_kT = sbn([128, BLK], BF16)
        vo, b_vo = sbn([128, NCH, 256], F32, 3)
        iff, b_if = sbn([128, NCH, 2], F32)
        sm, b_sm = sbn([128, 64], F32)
        vaug, b_vaug = sbn([128, NCH, 144], BF16)
        kc, b_kc = sbn([128, NCH, 128], BF16)
        PT, b_PT = sbn([128, NCH, 128], BF16)
        hm, b_hm = sbn([128, NCH, 128], F32)
        hq, b_hq = sbn([128, NCH, 128], F32)
        so, b_so = sbn([128, NCH, 128], F32)
        ym, b_ym = sbn([128, NCH, 128], BF16)
        tC, b_tC = sbn([128, 129], F32)
        ymT, b_ymT = sbn([128, BLK], BF16)
        yrT, b_yrT = sbn([128, BLK], BF16)
        xcb, b_xcb = sbn([128, BLK], BF16)
        names = ["r", "ig", "a", "a2", "xi", "h", "g2", "sg", "gl", "yr", "sd", "yn"]
        L = {}
        bL = {}
        for nm in names:
            L[nm], bL[nm] = sbn([128, BLK], F32)
        ysq, b_ysq = sbn([128, BLK], BF16)

        pfm = [cx.ps([128, 512], F32, es) for _ in range(2)]
        b_pfm = [Buf(), Buf()]
        pvo = [cx.ps([128, 2, 256], F32, es) for _ in range(2)]
        b_pvo = [Buf(), Buf()]
        pTr = cx.ps([128, 512], F32, es); b_pTr = Buf()
        psm = cx.ps([128, 512], F32, es); b_psm = Buf()
        pST = cx.ps([128, NCH, 128], F32, es); b_pST = Buf()
        pnum = cx.ps([128, NCH, 128], F32, es); b_pnum = Buf()
        p_kc = pTr[:, 0:256].bitcast(BF16).rearrange("p (c n) -> p c n", c=NCH)
        p_ym = pTr[:, 256:512].bitcast(BF16).rearrange("p (c n) -> p c n", c=NCH)
        p_cs = psm[:, 0:8]
        p_if = psm[:, 8:16].rearrange("p (c n) -> p c n", c=NCH)
        p_den = psm[:, 16:20]
        p_upd = psm[:, 32:161]
        fmstate = {"n": 0}

        def fm_bank():
            n = fmstate["n"] % 2
            fmstate["n"] += 1
            return pfm[n], b_pfm[n]

        uT_v = d["uT"].rearrange("(k p) t -> p k t", p=128)

        def proj_pieces(blk):
            i = blk % 2
            i3 = blk % 3
            t0 = blk * BLK
            pieces = []

            def p_load():
                S.dma("sp", uTb[i][:], uT_v[:, :, t0:t0 + BLK], writes=[b_uTb[i]])
                if blk > 0:
                    for nm in ("q", "k", "x"):
                        S.op("pool", lambda e: e.tensor_copy(out=pre[nm][i][:, 0:3], in_=pre[nm][1 - i][:, BLK:BLK + 3]),
                             [b_pre[nm][1 - i]], [b_pre[nm][i]])

            def p_fm(gi, nm):
                def f():
                    p, bp = fm_bank()
                    for k in range(8):
                        S.op("pe", lambda e: e.matmul(p[:], lhsT=W[:, k, gi * 128:(gi + 1) * 128], rhs=uTb[i][:, k, :],
                                                      start=(k == 0), stop=(k == 7)), [b_W, b_uTb[i]], [bp])
                    if nm == "g":
                        S.op("act", lambda e: e.activation(out=gpre[i3][:], in_=p[:], func=AF.Identity,
                                                           bias=pp[:, 18:19], scale=1.0), [b_pp], [bp, b_gpre[i3]])
                    else:
                        S.op("act", lambda e: e.activation(out=pre[nm][i][:, 3:BLK + 3], in_=p[:], func=AF.Identity,
                                                           bias=pp[:, 15 + gi:16 + gi], scale=1.0), [b_pp], [bp, b_pre[nm][i]])
                return f

            def p_vo(cp):
                def f():
                    for cc in range(2):
                        c = cp * 2 + cc
                        for k in range(8):
                            S.op("pe", lambda e: e.matmul(pvo[cp][:, cc, :], lhsT=uTb[i][:, k, c * 128:(c + 1) * 128],
                                                          rhs=W[:, k, 512:768], start=(k == 0), stop=(k == 7)),
                                 [b_W, b_uTb[i]], [b_pvo[cp]])
                    S.op("dve", lambda e: e.tensor_tensor(out=vo[i3][:, cp * 2:cp * 2 + 2, :], in0=pvo[cp][:],
                                                          in1=fv[:, 0:256].unsqueeze(1).to_broadcast([128, 2, 256]), op=ALU.add),
                         [b_fv], [b_pvo[cp], b_vo[i3]])
                return f

            def p_ifproj():
                for c in range(NCH):
                    for k in range(8):
                        S.op("pe", lambda e: e.matmul(p_if[:, c, :], lhsT=uTb[i][:, k, c * 128:(c + 1) * 128],
                                                      rhs=W[:, k, 768:770], start=(k == 0), stop=(k == 7)),
                             [b_W, b_uTb[i]], [b_psm])
                S.op("dve", lambda e: e.tensor_tensor(out=iff[i][:], in0=p_if, in1=fv[:, 256:258].unsqueeze(1).to_broadcast([128, NCH, 2]),
                                                      op=ALU.add), [b_fv], [b_psm, b_if[i]])

            def p_conv(nm, bc):
                def f():
                    p, bp = fm_bank()
                    for k in range(4):
                        S.op("pe", lambda e: e.matmul(p[:], lhsT=dg[nm][:, k, :], rhs=pre[nm][i][:, k:k + BLK],
                                                      start=(k == 0), stop=(k == 3)), [b_dg, b_pre[nm][i]], [bp])
                    if nm == "q":
                        S.op("act", lambda e: e.activation(out=qT[i][:], in_=p[:], func=AF.Silu, bias=pp[:, bc:bc + 1], scale=1.0),
                             [b_pp], [bp, b_qT[i]])
                    elif nm == "k":
                        S.op("act", lambda e: e.activation(out=kT[i][:], in_=p[:], func=AF.Silu, bias=pp[:, bc:bc + 1], scale=1.0),
                             [b_pp], [bp, b_kT[i]])
                    else:
                        S.op("act", lambda e: e.activation(out=acc["x"][i3][:], in_=p[:], func=AF.Identity, bias=pp[:, bc:bc + 1], scale=1.0),
                             [b_pp], [bp, b_acc["x"][i3]])
                return f

            pieces = [p_load, p_ifproj, p_fm(0, "q"), p_fm(1, "k"), p_fm(2, "x"), p_fm(3, "g"), p_vo(0), p_vo(1),
                      p_conv("q", 12), p_conv("k", 13), p_conv("x", 14)]
            return pieces

        def head(blk, pieces):
            def pump(n):
                for _ in range(n):
                    if pieces:
                        pieces.pop(0)()
            i = blk % 2
            i3 = blk % 3
            t0 = blk * BLK
            s_ = sm[i]; bs = b_sm[i]
            bc4 = lambda ap: ap.unsqueeze(2).to_broadcast([128, NCH, 128])
            S.op("act", lambda e: e.activation(out=s_[:, 0:4], in_=iff[i][:, :, 1], func=AF.Exp, scale=-1.0), [b_if[i]], [bs])
            yield
            S.op("act", lambda e: e.activation(out=s_[:, 4:8], in_=s_[:, 0:4], func=AF.Ln, bias=1.0, scale=1.0), [bs], [bs])
            yield
            S.op("pe", lambda e: e.matmul(p_cs[:, 0:4], lhsT=tri[:], rhs=s_[:, 4:8], start=True, stop=True), [b_tri, bs], [b_psm])
            yield
            S.op("pe", lambda e: e.matmul(p_cs[:, 4:8], lhsT=ones_f[:], rhs=s_[:, 4:8], start=True, stop=True), [b_of, bs], [b_psm])
            yield
            S.op("dve", lambda e: e.tensor_tensor(out=s_[:, 8:12], in0=iff[i][:, :, 0], in1=p_cs[:, 0:4], op=ALU.add),
                 [b_if[i]], [b_psm, bs])
            yield
            S.op("act", lambda e: e.activation(out=s_[:, 12:16], in_=s_[:, 8:12], func=AF.Exp, bias=LN_INV_SQRT_DH, scale=1.0), [bs], [bs])
            yield
            S.op("act", lambda e: e.activation(out=s_[:, 16:24], in_=p_cs, func=AF.Exp, scale=-1.0), [], [b_psm, bs])
            yield
            pump(3)
            ws = s_[:, 12:16]
            eb = s_[:, 16:20]
            S.op("dve", lambda e: e.tensor_tensor(out=vaug[i][:, :, 0:128], in0=vo[i3][:, :, 0:128], in1=bc4(ws), op=ALU.mult),
                 [b_vo[i3], bs], [b_vaug[i]])
            yield
            S.op("dve", lambda e: e.tensor_copy(out=vaug[i][:, :, 128], in_=ws), [bs], [b_vaug[i]])
            yield
            for c in range(NCH):
                S.op("pe", lambda e: e.transpose(p_kc[:, c, :], kT[i][:, c * 128:(c + 1) * 128], ident[:]), [b_kT[i], b_id], [b_pTr])
                yield
            S.op("act", lambda e: e.activation(out=kc[i][:], in_=p_kc, func=AF.Copy), [], [b_pTr, b_kc[i]])
            yield
            for c in range(NCH):
                cs = slice(c * 128, (c + 1) * 128)
                S.op("pe", lambda e: e.matmul(pST[:, c, :], lhsT=kT[i][:, cs], rhs=qT[i][:, cs], start=True, stop=True),
                     [b_kT[i], b_qT[i]], [b_pST])
                yield
            S.op("dve", lambda e: e.tensor_tensor(out=PT[i][:], in0=pST[:], in1=tri[:].unsqueeze(1).to_broadcast([128, NCH, 128]),
                                                  op=ALU.mult), [b_tri], [b_pST, b_PT[i]])
            yield
            for c in range(NCH):
                pump(1)
                cs = slice(c * 128, (c + 1) * 128)
                S.op("pe", lambda e: e.matmul(pnum[:, c, :], lhsT=PT[i][:, c, :], rhs=vaug[i][:, c, 0:128], start=True, stop=False),
                     [b_PT[i], b_vaug[i]], [b_pnum])
                yield
                S.op("pe", lambda e: e.matmul(pnum[:, c, :], lhsT=qT[i][:, cs], rhs=Cbf[:, 0:128], start=False, stop=True),
                     [b_qT[i], b_Cbf], [b_pnum])
                yield
                S.op("pe", lambda e: e.matmul(p_den[:, c:c + 1], lhsT=PT[i][:, c, :], rhs=vaug[i][:, c, 128:129], start=True, stop=False),
                     [b_PT[i], b_vaug[i]], [b_psm])
                yield
                S.op("pe", lambda e: e.matmul(p_den[:, c:c + 1], lhsT=qT[i][:, cs], rhs=Cbf[:, 128:129], start=False, stop=True),
                     [b_qT[i], b_Cbf], [b_psm])
                yield
                S.op("pe", lambda e: e.matmul(p_upd, lhsT=kc[i][:, c, :], rhs=vaug[i][:, c, 0:129], start=True, stop=True),
                     [b_kc[i], b_vaug[i]], [b_psm])
                yield
                j = c % 2
                S.op("dve", lambda e: e.tensor_tensor(out=tC[j][:], in0=p_upd, in1=C32[:], op=ALU.add), [b_C32], [b_psm, b_tC[j]])
                yield
                S.op("dve", lambda e: e.tensor_tensor(out=C32[:], in0=tC[j][:], in1=s_[:, 20 + c:21 + c].to_broadcast([128, 129]), op=ALU.mult),
                     [b_tC[j], bs], [b_C32])
                yield
                S.op("act", lambda e: e.activation(out=Cbf[:], in_=tC[j][:], func=AF.Copy, scale=s_[:, 20 + c:21 + c]),
                     [b_tC[j], bs], [b_Cbf])
                yield
            pump(1)
            yield

        def tail(blk, pieces):
            def pump(n):
                for _ in range(n):
                    if pieces:
                        pieces.pop(0)()
            i = blk % 2
            i3 = blk % 3
            t0 = blk * BLK
            s_ = sm[i]; bs = b_sm[i]
            bc4 = lambda ap: ap.unsqueeze(2).to_broadcast([128, NCH, 128])
            eb = s_[:, 16:20]
            S.op("dve", lambda e: e.tensor_tensor(out=s_[:, 24:28], in0=p_den, in1=eb, op=ALU.mult), [], [b_psm, bs])
            yield
            S.op("act", lambda e: e.activation(out=s_[:, 24:28], in_=s_[:, 24:28], func=AF.Abs), [bs], [bs])
            yield
            S.op("dve", lambda e: e.tensor_scalar_max(out=s_[:, 24:28], in0=s_[:, 24:28], scalar1=1.0), [bs], [bs])
            yield
            S.op("dve", lambda e: e.reciprocal(out=s_[:, 28:32], in_=s_[:, 24:28]), [bs], [bs])
            yield
            S.op("dve", lambda e: e.tensor_tensor(out=s_[:, 28:32], in0=s_[:, 28:32], in1=eb, op=ALU.mult), [bs], [bs])
            yield
            S.op("dve", lambda e: e.tensor_tensor(out=hm[i][:], in0=pnum[:], in1=bc4(s_[:, 28:32]), op=ALU.mult), [bs], [b_pnum, b_hm[i]])
            yield
            S.op("dve", lambda e: e.tensor_reduce(out=s_[:, 32:36], in_=hm[i][:], axis=AX.X, op=ALU.add), [b_hm[i]], [bs])
            yield
            S.op("act", lambda e: e.activation(out=hq[i][:], in_=hm[i][:], func=AF.Square), [b_hm[i]], [b_hq[i]])
            yield
            S.op("dve", lambda e: e.tensor_reduce(out=s_[:, 36:40], in_=hq[i][:], axis=AX.X, op=ALU.add), [b_hq[i]], [bs])
            yield
            S.op("dve", lambda e: e.tensor_scalar_mul(out=s_[:, 32:36], in0=s_[:, 32:36], scalar1=1.0 / 128.0), [bs], [bs])
            yield
            S.op("dve", lambda e: e.tensor_tensor(out=s_[:, 40:44], in0=s_[:, 32:36], in1=s_[:, 32:36], op=ALU.mult), [bs], [bs])
            yield
            S.op("dve", lambda e: e.scalar_tensor_tensor(out=s_[:, 36:40], in0=s_[:, 36:40], scalar=1.0 / 128.0, in1=s_[:, 40:44],
                                                         op0=ALU.mult, op1=ALU.subtract), [bs], [bs])
            yield
            S.op("act", lambda e: e.activation(out=s_[:, 36:40], in_=s_[:, 36:40], func=AF.Ln, bias=EPS, scale=1.0), [bs], [bs])
            yield
            S.op("act", lambda e: e.activation(out=s_[:, 36:40], in_=s_[:, 36:40], func=AF.Exp, scale=-0.5), [bs], [bs])
            yield
            S.op("dve", lambda e: e.tensor_tensor(out=hm[i][:], in0=hm[i][:], in1=bc4(s_[:, 32:36]), op=ALU.subtract), [bs], [b_hm[i]])
            yield
            S.op("dve", lambda e: e.tensor_tensor(out=hm[i][:], in0=hm[i][:], in1=bc4(s_[:, 36:40]), op=ALU.mult), [bs], [b_hm[i]])
            yield
            S.op("act", lambda e: e.activation(out=so[i][:], in_=vo[i3][:, :, 128:256], func=AF.Sigmoid), [b_vo[i3]], [b_so[i]])
            yield
            S.op("dve", lambda e: e.tensor_tensor(out=ym[i][:], in0=hm[i][:], in1=so[i][:], op=ALU.mult), [b_hm[i], b_so[i]], [b_ym[i]])
            yield
            pump(1)
            for c in range(NCH):
                S.op("pe", lambda e: e.transpose(p_ym[:, c, :], ym[i][:, c, :], ident[:]), [b_ym[i], b_id], [b_pTr])
                yield
            S.op("act", lambda e: e.activation(out=ymT[i][:].rearrange("p (c n) -> p c n", c=NCH), in_=p_ym, func=AF.Copy,
                                               scale=pp[:, 23:24]), [b_pp], [b_pTr, b_ymT[i]])
            yield
            outs.append(Buf())
            S.dma("sp", d["yT"][0:128, t0:t0 + BLK], ymT[i][:], reads=[b_ymT[i]], writes=[outs[-1]])
            yield

            xc = acc["x"][i3]; b_xc = b_acc["x"][i3]
            S.op("act", lambda e: e.activation(out=xcb[i][:], in_=xc[:], func=AF.Copy), [b_xc], [b_xcb[i]])
            yield
            p, bp = fm_bank()
            S.op("pe", lambda e: e.matmul(p[:], lhsT=wa[:], rhs=xcb[i][:], start=True, stop=True), [b_wa, b_xcb[i]], [bp])
            yield
            S.op("act", lambda e: e.activation(out=L["r"][i][:], in_=p[:], func=AF.Sigmoid, bias=pp[:, 19:20], scale=1.0),
                 [b_pp], [bp, bL["r"][i]])
            yield
            p, bp = fm_bank()
            S.op("pe", lambda e: e.matmul(p[:], lhsT=wx[:], rhs=xcb[i][:], start=True, stop=True), [b_wx, b_xcb[i]], [bp])
            yield
            S.op("act", lambda e: e.activation(out=L["ig"][i][:], in_=p[:], func=AF.Sigmoid, bias=pp[:, 20:21], scale=1.0),
                 [b_pp], [bp, bL["ig"][i]])
            yield
            g = gpre[i3]; bg = b_gpre[i3]
            S.op("pool", lambda e: e.tensor_tensor(out=L["g2"][i][:], in0=g[:], in1=g[:], op=ALU.mult), [bg], [bL["g2"][i]])
            yield
            S.op("pool", lambda e: e.tensor_scalar(out=L["g2"][i][:], in0=L["g2"][i][:], scalar1=0.044715, scalar2=1.0,
                                                   op0=ALU.mult, op1=ALU.add), [], [bL["g2"][i]])
            yield
            S.op("pool", lambda e: e.tensor_tensor(out=L["g2"][i][:], in0=L["g2"][i][:], in1=g[:], op=ALU.mult), [bg], [bL["g2"][i]])
            yield
            S.op("act", lambda e: e.activation(out=L["sg"][i][:], in_=L["g2"][i][:], func=AF.Sigmoid, scale=GELU_C),
                 [bL["g2"][i]], [bL["sg"][i]])
            yield
            S.op("act", lambda e: e.activation(out=L["a"][i][:], in_=L["r"][i][:], func=AF.Exp, scale=der[:, 0:1]),
                 [bL["r"][i], b_der], [bL["a"][i]])
            yield
            S.op("act", lambda e: e.activation(out=L["a2"][i][:], in_=L["r"][i][:], func=AF.Exp, scale=der[:, 1:2]),
                 [bL["r"][i], b_der], [bL["a2"][i]])
            yield
            S.op("act", lambda e: e.activation(out=L["a2"][i][:], in_=L["a2"][i][:], func=AF.Ln, bias=1.0, scale=-1.0),
                 [bL["a2"][i]], [bL["a2"][i]])
            yield
            S.op("act", lambda e: e.activation(out=L["a2"][i][:], in_=L["a2"][i][:], func=AF.Exp, scale=0.5),
                 [bL["a2"][i]], [bL["a2"][i]])
            yield
            S.op("pool", lambda e: e.tensor_tensor(out=L["xi"][i][:], in0=L["ig"][i][:], in1=xc[:], op=ALU.mult),
                 [bL["ig"][i], b_xc], [bL["xi"][i]])
            yield
            S.op("dve", lambda e: e.tensor_tensor(out=L["xi"][i][:], in0=L["xi"][i][:], in1=L["a2"][i][:], op=ALU.mult),
                 [bL["a2"][i]], [bL["xi"][i]])
            yield
            init = hz[:, 0:1] if blk == 0 else L["h"][1 - i][:, BLK - 1:BLK]
            b_init = b_hz if blk == 0 else bL["h"][1 - i]
            S.op("dve", lambda e: e.tensor_tensor_scan(out=L["h"][i][:], data0=L["a"][i][:], data1=L["xi"][i][:], initial=init,
                                                       op0=ALU.mult, op1=ALU.add),
                 [bL["a"][i], bL["xi"][i], b_init], [bL["h"][i]])
            yield
            g = gpre[i3]; bg = b_gpre[i3]
            S.op("pool", lambda e: e.tensor_tensor(out=L["gl"][i][:], in0=L["sg"][i][:], in1=g[:], op=ALU.mult),
                 [bL["sg"][i], bg], [bL["gl"][i]])
            yield
            S.op("dve", lambda e: e.tensor_tensor(out=L["yr"][i][:], in0=L["h"][i][:], in1=L["gl"][i][:], op=ALU.mult),
                 [bL["h"][i], bL["gl"][i]], [bL["yr"][i]])
            yield
            S.op("act", lambda e: e.activation(out=ysq[i][:], in_=L["yr"][i][:], func=AF.Square), [bL["yr"][i]], [b_ysq[i]])
            yield
            p, bp = fm_bank()
            S.op("pe", lambda e: e.matmul(p[:], lhsT=ones_bf[:], rhs=ysq[i][:], start=True, stop=True), [b_ob, b_ysq[i]], [bp])
            yield
            S.op("act", lambda e: e.activation(out=L["sd"][i][:], in_=p[:], func=AF.Ln, bias=128.0 * EPS, scale=1.0),
                 [], [bp, bL["sd"][i]])
            yield
            S.op("act", lambda e: e.activation(out=L["sd"][i][:], in_=L["sd"][i][:], func=AF.Exp, scale=-0.5), [], [bL["sd"][i]])
            yield
            S.op("pool", lambda e: e.tensor_tensor(out=L["yn"][i][:], in0=L["yr"][i][:], in1=L["sd"][i][:], op=ALU.mult),
                 [bL["yr"][i], bL["sd"][i]], [bL["yn"][i]])
            yield
            S.op("act", lambda e: e.activation(out=yrT[i][:], in_=L["yn"][i][:], func=AF.Copy, scale=der[:, 2:3]),
                 [bL["yn"][i], b_der], [b_yrT[i]])
            yield
            outs.append(Buf())
            S.dma("sp", d["yT"][128:256, t0:t0 + BLK], yrT[i][:], reads=[b_yrT[i]], writes=[outs[-1]])
            yield

        for f in proj_pieces(0):
            f()
        prev_tail = None
        for blk in range(nblk):
            pcs = proj_pieces(blk + 1) if blk + 1 < nblk else []
            round_robin([prev_tail, head(blk, pcs)])
            for f in pcs:
                f()
            prev_tail = tail(blk, [])
        round_robin([prev_tail])
        S.barrier()
    return outs


def pack_B_inputs(inp, l, h, uT_full, hc):
    w_in = inp["w_in"][l]
    b_in = inp["b_in"][l]
    hs = slice(h * 128, (h + 1) * 128)
    o_q, o_k, o_v, o_o, o_i, o_f, o_x, o_g = 0, 1024, 2048, 3072, 4096, 4104, 4112, 5136
    cols = np.concatenate([np.arange(o_q + h * 128, o_q + (h + 1) * 128), np.arange(o_k + h * 128, o_k + (h + 1) * 128),
                           np.arange(o_x + h * 128, o_x + (h + 1) * 128), np.arange(o_g + h * 128, o_g + (h + 1) * 128),
                           np.arange(o_v + h * 128, o_v + (h + 1) * 128), np.arange(o_o + h * 128, o_o + (h + 1) * 128),
                           np.array([o_i + h, o_f + h])])
    wh = np.ascontiguousarray(w_in[:, cols])
    bh = b_in[cols]
    pp = np.zeros((128, 24), np.float32)
    pp[:, 0:4] = inp["w_conv_m"][l][:, hs].T
    pp[:, 4:8] = inp["w_conv_m"][l][:, 1024 + h * 128:1024 + (h + 1) * 128].T
    pp[:, 8:12] = inp["w_conv_r"][l][:, hs].T
    pp[:, 12] = inp["b_conv_m"][l][hs]
    pp[:, 13] = inp["b_conv_m"][l][1024 + h * 128:1024 + (h + 1) * 128]
    pp[:, 14] = inp["b_conv_r"][l][hs]
    pp[:, 15] = bh[0:128]
    pp[:, 16] = bh[128:256]
    pp[:, 17] = bh[256:384]
    pp[:, 18] = bh[384:512]
    pp[:, 19] = inp["b_a"][l][hs]
    pp[:, 20] = inp["b_x"][l][hs]
    pp[:, 21] = inp["lru_lambda"][l][hs]
    pp[:, 22] = inp["lru_norm_g"][l][hs]
    pp[:, 23] = inp["mh_norm_g"][l][hs]
    fv = np.ascontiguousarray(bh[512:770][None, :])
    return {"uT": uT_full, "wh": wh, "pp": pp, "fv": fv,
            "wa": np.ascontiguousarray(inp["w_a"][l][h]), "wx": np.ascontiguousarray(inp["w_x"][l][h]),
            "ident_bf": hc["ident_bf"], "tri_f": hc["tri_f"], "ones_f": hc["ones_f"], "ones_bf": hc["ones_bf"]}


NSLOT = NE * CAP
BIGROW = float(NSLOT + 64)


def build_C(debug=False):
    nc = bass.Bass("TRN2", target_bir_lowering=False)
    with ExitStack() as es:
        cx = Ctx(nc, es)
        S = cx.S
        d = {
            "x": cx.dram_in("x", [TPC, D], F32),
            "yT": cx.dram_in("yT", [2 * D, TPC], BF16),
            "ada": cx.dram_in("ada", [1, 6 * D], F32),
            "w_out": cx.dram_in("w_out", [2 * D, D], F32),
            "lnp": cx.dram_in("lnp", [1, 4 * D], F32),
            "w_router": cx.dram_in("w_router", [D, NE], F32),
            "b_router": cx.dram_in("b_router", [1, NE], F32),
            "eoff": cx.dram_in("eoff", [1, NE], F32),
            "w_gate": cx.dram_in("w_gate", [NE, D, DFF], F32),
            "w_up": cx.dram_in("w_up", [NE, D, DFF], F32),
            "w_down": cx.dram_in("w_down", [NE, DFF, D], F32),
            "ident_bf": cx.dram_in("ident_bf", [128, 128], BF16),
            "ident_f": cx.dram_in("ident_f", [128, 128], F32),
            "tris_bf": cx.dram_in("tris_bf", [128, 128], BF16),
            "ones_bf": cx.dram_in("ones_bf", [128, 128], BF16),
            "xout": cx.dram_out("xout", [TPC, D], F32),
        }
        if debug:
            d["xbuf"] = nc.dram_tensor("xbuf", [NSLOT, D], BF16, kind="ExternalOutput")
            d["ybuf"] = nc.dram_tensor("ybuf", [NSLOT, D], F32, kind="ExternalOutput")
            d["dbg_dest"] = cx.dram_out("dbg_dest", [128, NT * 2], I32)
            d["dbg_gate"] = cx.dram_out("dbg_gate", [128, NT, 2], F32)
        else:
            d["xbuf"] = nc.dram_tensor("xbuf", [NSLOT, D], BF16)
            d["ybuf"] = nc.dram_tensor("ybuf", [NSLOT, D], F32)
        adac = cx.sb([128, 4, D], F32)
        b_adac = Buf()
        S.dma("sp", adac[:], d["ada"][:, 2 * D:6 * D].partition_broadcast(128).rearrange("p o (a n) -> p (o a) n", a=4),
              writes=[b_adac])
        S.op("dve", lambda e: e.tensor_scalar_add(out=adac[:, 2, :], in0=adac[:, 2, :], scalar1=1.0), [b_adac], [b_adac])
        d["x1s"] = nc.dram_tensor("x1s", [TPC, D], F32)
        outs = emit_C(cx, d, d["x"], None, None, adac[:, 0, :], adac[:, 1, :], adac[:, 2, :], adac[:, 3, :], b_adac,
                      d["xout"])
        S.finish(outs, "sp")
    return nc


def emit_C(cx, d, x_d, xres, b_xres, g1_ap, sh2_ap, sc2_ap, g2_ap, b_ada, xout_d):
    S = cx.S
    nc = cx.nc
    outs = []
    with ExitStack() as es0:
        ident = cx.sb([128, 128], BF16, es0); b_id = Buf()
        S.dma("sp", ident[:], d["ident_bf"], writes=[b_id])
        scat = []
        bc_reg = nc.gpsimd.alloc_register(f"bc{cx._n}")
        nc.gpsimd.reg_mov(bc_reg, NSLOT - 1)
        zt = cx.sb([128, D], BF16, es0); b_zt = Buf()
        S.op("pool", lambda e: e.memset(zt[:], 0.0), [], [b_zt])
        zfill = []
        for r0 in range(0, NSLOT, 1024):
            zfill.append(Buf())
            S.dma("sp" if (r0 // 1024) % 2 == 0 else "act",
                  d["xbuf"][r0:r0 + 1024, :].rearrange("(p s) n -> p s n", p=128),
                  zt[:].unsqueeze(1).to_broadcast([128, 8, D]), reads=[b_zt], writes=[zfill[-1]])
        NG = NT * NE
        destS = cx.sb([128, 2 * NT], I32, es0)
        destG = cx.sb([128, 2 * NT], I32, es0)
        gate2 = cx.sb([128, NT, 2], F32, es0)
        b_rt = Buf()
        u2b_all = cx.sb([128, NT, D], BF16, es0)
        b_u2b = [Buf() for _ in range(NT)]
        x1w = []
        with ExitStack() as es:
            def sb(shape, dt):
                return cx.sb(shape, dt, es)

            def sbn(shape, dt, n=2):
                return [cx.sb(shape, dt, es) for _ in range(n)], [Buf() for _ in range(n)]

            wout = sb([128, 16, D], BF16); b_wout = Buf()
            wo_v = d["w_out"].rearrange("(m p) n -> p m n", p=128)
            for q4 in range(4):
                S.dma("pool", wout[:, q4 * 4:(q4 + 1) * 4, :], wo_v[:, q4 * 4:(q4 + 1) * 4, :], writes=[b_wout])
            lnp = sb([128, 2, D], F32); b_lnp = Buf()
            S.dma("sp", lnp[:], d["lnp"][:, 0:2 * D].partition_broadcast(128).rearrange("p o (a n) -> p (o a) n", a=2), writes=[b_lnp])
            identf = sb([128, 128], F32); b_idf = Buf()
            S.dma("sp", identf[:], d["ident_f"], writes=[b_idf])
            tris = sb([128, 128], BF16); b_tris = Buf()
            S.dma("sp", tris[:], d["tris_bf"], writes=[b_tris])
            ones = sb([128, 128], BF16); b_ones = Buf()
            S.dma("sp", ones[:], d["ones_bf"], writes=[b_ones])
            wr = sb([128, 8, NE], F32); b_wr = Buf()
            S.dma("sp", wr[:], d["w_router"].rearrange("(k p) n -> p k n", p=128), writes=[b_wr])
            br = sb([128, NE], F32); b_br = Buf()
            S.dma("sp", br[:], d["b_router"].partition_broadcast(128), writes=[b_br])
            eoff = sb([128, NE], F32); b_eoff = Buf()
            S.dma("sp", eoff[:], d["eoff"].partition_broadcast(128), writes=[b_eoff])

            ytb, b_ytb = sbn([128, 16, 256], BF16)
            xin, b_xin = sbn([128, D], F32)
            t1_, b_t1_ = sbn([128, D], F32)
            z_, b_z_ = sbn([128, D], F32)
            x1t_, b_x1t_ = sbn([128, D], F32)
            u2f_, b_u2f_ = sbn([128, D], F32)
            u2T_, b_u2T_ = sbn([128, 8, 128], F32)
            xnA = sb([128, D], F32); b_xnA = Buf()
            tmpA = sb([128, D], F32); b_tmpA = Buf()
            xnB = sb([128, D], F32); b_xnB = Buf()
            tmpB = sb([128, D], F32); b_tmpB = Buf()
            lnsA = LNScratch(cx, es)
            lnsB = LNScratch(cx, es)
            aff_all = sb([128, NT, NE], F32)
            b_aff = [Buf() for _ in range(NT)]

            po = [cx.ps([128, 512], F32, es) for _ in range(2)]
            b_po = [Buf(), Buf()]
            pT = [cx.ps([128, 4, 128], F32, es) for _ in range(2)]
            b_pT = [Buf(), Buf()]
            plog = cx.ps([128, 512], F32, es); b_plog = Buf()
            ppre = cx.ps([128, 512], F32, es); b_ppre = Buf()
            ptot = cx.ps([128, 512], F32, es); b_ptot = Buf()

            yT_v = d["yT"].rearrange("(m p) t -> p m t", p=128)

            logit_all = sb([128, NT, NE], F32)

            def S1(t):
                i = t % 2
                if t % 2 == 0:
                    yi = (t // 2) % 2
                    S.dma("sp", ytb[yi][:], yT_v[:, :, t * 128:t * 128 + 256], writes=[b_ytb[yi]])
                yi = (t // 2) % 2
                tsl = slice((t % 2) * 128, (t % 2) * 128 + 128)
                S.dma("act", xin[i][:], x_d[t * 128:(t + 1) * 128, :], writes=[b_xin[i]])
                for half in range(2):
                    hs = slice(half * 512, (half + 1) * 512)
                    for m in range(16):
                        S.op("pe", lambda e: e.matmul(po[half][:], lhsT=ytb[yi][:, m, tsl], rhs=wout[:, m, hs],
                                                      start=(m == 0), stop=(m == 15)), [b_ytb[yi], b_wout], [b_po[half]])
                        if m % 4 == 3:
                            yield
                    S.op("dve", lambda e: e.tensor_tensor(out=t1_[i][:, hs], in0=po[half][:], in1=g1_ap[:, hs], op=ALU.mult),
                         [b_ada], [b_po[half], b_t1_[i]])
                    yield
                S.op("dve", lambda e: e.scalar_tensor_tensor(out=z_[i][:], in0=xin[i][:], scalar=ALPHA, in1=t1_[i][:],
                                                             op0=ALU.mult, op1=ALU.add), [b_xin[i], b_t1_[i]], [b_z_[i]])
                yield

            def S2(t):
                i = t % 2
                yield from g_ln_mod(cx, z_[i][:], b_z_[i], lnsA, xnA, b_xnA, lnp[:, 0, :], lnp[:, 1, :], b_lnp, x1t_[i][:], b_x1t_[i], tmpA, b_tmpA)
                x1w.append(Buf())
                S.dma("act", d["x1s"][t * 128:(t + 1) * 128, :], x1t_[i][:], reads=[b_x1t_[i]], writes=[x1w[-1]])
                yield

            def S3(t):
                i = t % 2
                yield from g_ln_mod(cx, x1t_[i][:], b_x1t_[i], lnsB, xnB, b_xnB, sc2_ap, sh2_ap, b_ada, u2f_[i][:], b_u2f_[i], tmpB, b_tmpB)
                S.op("act", lambda e: e.activation(out=u2b_all[:, t, :], in_=u2f_[i][:], func=AF.Copy), [b_u2f_[i]], [b_u2b[t]])
                yield

            def S4(t):
                i = t % 2
                u2f, b_u2f, u2T, b_u2T = u2f_[i], b_u2f_[i], u2T_[i], b_u2T_[i]
                for k in range(8):
                    S.op("pe", lambda e: e.transpose(pT[k // 4][:, k % 4, :], u2f[:, k * 128:(k + 1) * 128], identf[:]),
                         [b_u2f, b_idf], [b_pT[k // 4]])
                    if k % 4 == 3:
                        yield
                S.op("act", lambda e: e.activation(out=u2T[:, 0:4, :], in_=pT[0][:], func=AF.Copy), [], [b_pT[0], b_u2T])
                yield
                S.op("dve", lambda e: e.tensor_copy(out=u2T[:, 4:8, :], in_=pT[1][:]), [], [b_pT[1], b_u2T])
                yield
                for k in range(8):
                    S.op("pe", lambda e: e.matmul(plog[:, 0:NE], lhsT=u2T[:, k, :], rhs=wr[:, k, :], start=(k == 0), stop=(k == 7)),
                         [b_u2T, b_wr], [b_plog])
                yield
                S.op("dve", lambda e: e.tensor_copy(out=logit_all[:, t, :], in_=plog[:, 0:NE]), [], [b_plog, b_aff[t]])
                yield

            for kk in range(NT + 3):
                round_robin([S1(kk) if kk < NT else None,
                             S2(kk - 1) if 0 <= kk - 1 < NT else None,
                             S3(kk - 2) if 0 <= kk - 2 < NT else None,
                             S4(kk - 3) if 0 <= kk - 3 < NT else None])
            b_affall = Buf()
            S.op("act", lambda e: e.activation(out=aff_all[:], in_=logit_all[:], func=AF.Sigmoid), b_aff, [b_affall])

            RR = sb([128, 12, NG], F32); b_R = Buf()
            aff = aff_all[:].rearrange("p t e -> p (t e)")
            sel, eq, selm, ge, msk, gv, pos, val, vld, dd, cntf = (RR[:, n, :] for n in range(11))
            q3 = lambda ap: ap.rearrange("p (a j) -> p a j", j=4)
            t3 = lambda ap: ap.rearrange("p (t e) -> p t e", e=NE)
            r128 = sb([128, 4, NT * 8], F32)
            m1, m2, gsc, gone = (r128[:, n, :] for n in range(4))
            r16 = sb([128, 8, NT], F32)
            gmax, gsum, rgs, first, second, g1s, gts = (r16[:, n, :] for n in range(7))
            mk = sb([128, NG], BF16); b_mk = Buf()
            S.op("dve", lambda e: e.tensor_tensor(out=t3(sel), in0=aff_all[:], in1=br[:].unsqueeze(1).to_broadcast([128, NT, NE]), op=ALU.add),
                 [b_affall, b_br], [b_R])
            S.op("dve", lambda e: e.tensor_reduce(out=m1, in_=q3(sel), axis=AX.X, op=ALU.max), [], [b_R])
            S.op("dve", lambda e: e.tensor_tensor(out=q3(eq), in0=q3(sel), in1=m1.unsqueeze(2).to_broadcast([128, NT * 8, 4]), op=ALU.is_equal), [], [b_R])
            S.op("dve", lambda e: e.scalar_tensor_tensor(out=selm, in0=eq, scalar=-1e9, in1=sel, op0=ALU.mult, op1=ALU.add), [], [b_R])
            S.op("dve", lambda e: e.tensor_reduce(out=m2, in_=q3(selm), axis=AX.X, op=ALU.max), [], [b_R])
            S.op("dve", lambda e: e.tensor_tensor(out=gsc, in0=m1, in1=m2, op=ALU.add), [], [b_R])
            g8 = lambda ap: ap.rearrange("p (t g) -> p t g", g=8)
            S.op("dve", lambda e: e.tensor_reduce(out=gmax, in_=g8(gsc), axis=AX.X, op=ALU.max), [], [b_R])
            S.op("dve", lambda e: e.tensor_tensor(out=g8(gone), in0=g8(gsc), in1=gmax.unsqueeze(2).to_broadcast([128, NT, 8]), op=ALU.is_equal), [], [b_R])
            S.op("dve", lambda e: e.tensor_tensor(out=q3(ge), in0=q3(sel), in1=m2.unsqueeze(2).to_broadcast([128, NT * 8, 4]), op=ALU.is_ge), [], [b_R])
            S.op("dve", lambda e: e.tensor_tensor(out=q3(msk), in0=q3(ge), in1=gone.unsqueeze(2).to_broadcast([128, NT * 8, 4]), op=ALU.mult), [], [b_R])
            S.op("dve", lambda e: e.tensor_tensor(out=gv, in0=aff, in1=msk, op=ALU.mult), [b_affall], [b_R])
            S.op("dve", lambda e: e.tensor_reduce(out=gsum, in_=t3(gv), axis=AX.X, op=ALU.add), [], [b_R])
            S.op("dve", lambda e: e.reciprocal(out=rgs, in_=gsum), [], [b_R])
            S.op("dve", lambda e: e.tensor_tensor(out=t3(gv), in0=t3(gv), in1=rgs.unsqueeze(2).to_broadcast([128, NT, NE]), op=ALU.mult), [], [b_R])
            S.op("dve", lambda e: e.tensor_copy(out=mk[:], in_=msk), [b_R], [b_mk])
            S.op("pe", lambda e: e.matmul(ppre[:], lhsT=tris[:], rhs=mk[:], start=True, stop=True), [b_tris, b_mk], [b_ppre])
            S.op("pe", lambda e: e.matmul(ptot[:], lhsT=ones[:], rhs=mk[:], start=True, stop=True), [b_ones, b_mk], [b_ptot])
            S.op("dve", lambda e: e.tensor_copy(out=dd, in_=ptot[:]), [], [b_ptot, b_R])
            S.op("dve", lambda e: e.memset(cntf[:, 0:NE], 0.0), [], [b_R])
            for t in range(1, NT):
                S.op("dve", lambda e: e.tensor_tensor(out=cntf[:, t * NE:(t + 1) * NE], in0=cntf[:, (t - 1) * NE:t * NE],
                                                      in1=dd[:, (t - 1) * NE:t * NE], op=ALU.add), [], [b_R])
            S.op("dve", lambda e: e.tensor_tensor(out=pos, in0=ppre[:], in1=cntf, op=ALU.add), [], [b_ppre, b_R])
            S.op("dve", lambda e: e.tensor_scalar(out=vld, in0=pos, scalar1=float(CAP) - 0.5, scalar2=None, op0=ALU.is_lt), [], [b_R])
            S.op("dve", lambda e: e.tensor_tensor(out=vld, in0=vld, in1=msk, op=ALU.mult), [], [b_R])
            S.op("dve", lambda e: e.tensor_tensor(out=t3(val), in0=t3(pos), in1=eoff[:].unsqueeze(1).to_broadcast([128, NT, NE]), op=ALU.add),
                 [b_eoff], [b_R])
            S.op("dve", lambda e: e.scalar_tensor_tensor(out=dd, in0=val, scalar=1.0, in1=vld, op0=ALU.add, op1=ALU.mult), [], [b_R])
            S.op("dve", lambda e: e.tensor_scalar_add(out=dd, in0=dd, scalar1=-1.0), [], [b_R])
            S.op("dve", lambda e: e.tensor_reduce(out=first, in_=t3(dd), axis=AX.X, op=ALU.max), [], [b_R])
            S.op("dve", lambda e: e.tensor_tensor(out=t3(eq), in0=t3(dd), in1=first.unsqueeze(2).to_broadcast([128, NT, NE]), op=ALU.is_equal), [], [b_R])
            S.op("dve", lambda e: e.scalar_tensor_tensor(out=selm, in0=eq, scalar=-1e9, in1=dd, op0=ALU.mult, op1=ALU.add), [], [b_R])
            S.op("dve", lambda e: e.tensor_reduce(out=second, in_=t3(selm), axis=AX.X, op=ALU.max), [], [b_R])
            S.op("dve", lambda e: e.tensor_tensor(out=gv, in0=gv, in1=vld, op=ALU.mult), [], [b_R])
            S.op("dve", lambda e: e.tensor_reduce(out=gts, in_=t3(gv), axis=AX.X, op=ALU.add), [], [b_R])
            S.op("dve", lambda e: e.tensor_tensor(out=ge, in0=gv, in1=eq, op=ALU.mult), [], [b_R])
            S.op("dve", lambda e: e.tensor_reduce(out=g1s, in_=t3(ge), axis=AX.X, op=ALU.add), [], [b_R])
            S.op("dve", lambda e: e.tensor_copy(out=gate2[:, :, 0], in_=g1s), [b_R], [b_rt])
            S.op("dve", lambda e: e.tensor_tensor(out=gate2[:, :, 1], in0=gts, in1=g1s, op=ALU.subtract), [b_R], [b_rt])
            fs = r16[:, 3:5, :]
            neg = r16[:, 5:7, :]
            dS = destS[:].rearrange("p (t s) -> p s t", s=2)
            dG = destG[:].rearrange("p (t s) -> p s t", s=2)
            S.op("dve", lambda e: e.tensor_scalar(out=neg, in0=fs, scalar1=0.0, scalar2=BIGROW + 1.0, op0=ALU.is_lt, op1=ALU.mult), [b_rt], [b_R])
            S.op("dve", lambda e: e.tensor_tensor(out=neg, in0=neg, in1=fs, op=ALU.add), [], [b_R])
            S.op("dve", lambda e: e.tensor_copy(out=dS, in_=neg), [b_R], [b_rt])
            S.op("dve", lambda e: e.tensor_scalar_max(out=neg, in0=fs, scalar1=0.0), [b_rt], [b_R])
            S.op("dve", lambda e: e.tensor_copy(out=dG, in_=neg), [b_R], [b_rt])
            for t in range(NT):
                for sidx in range(2):
                    scat.append(Buf())
                    S.dma_fn("pool", lambda e: e.indirect_dma_start(
                        out=d["xbuf"][:, :], out_offset=bass.IndirectOffsetOnAxis(ap=destS[:, 2 * t + sidx:2 * t + sidx + 1], axis=0),
                        in_=u2b_all[:, t, :], in_offset=None, bounds_check=bc_reg, oob_is_err=False),
                        [b_u2b[t], b_rt] + zfill, [scat[-1]])
        S.barrier()
        if "dbg_dest" in d:
            for nm, src in (("dbg_dest", destS), ("dbg_gate", gate2)):
                outs.append(Buf())
                S.dma("sp", d[nm], src[:], reads=[b_rt], writes=[outs[-1]])
        ysc = []
        with ExitStack() as es:
            def sbn(shape, dt, n=2):
                return [cx.sb(shape, dt, es) for _ in range(n)], [Buf() for _ in range(n)]

            wg, b_wg = sbn([128, 8, DFF], BF16)
            wu, b_wu = sbn([128, 8, DFF], BF16)
            wd, b_wd = sbn([128, 4, D], BF16)
            Xe, b_Xe = sbn([128, CAP // 128, D], BF16)
            XT, b_XT = sbn([128, 8, CAP], BF16)
            hT, b_hT = sbn([128, 4, CAP], BF16)
            sg, b_sg = sbn([128, CAP], F32)
            Ye, b_Ye = sbn([128, D], F32)
            pX = cx.ps([128, 8, 128], BF16, es); b_pX = Buf()
            pg = [cx.ps([128, 512], F32, es) for _ in range(2)]
            b_pg = [Buf(), Buf()]
            pu = [cx.ps([128, 512], F32, es) for _ in range(2)]
            b_pu = [Buf(), Buf()]
            pd = [cx.ps([128, 512], F32, es) for _ in range(2)]
            b_pd = [Buf(), Buf()]
            wg_v = d["w_gate"].rearrange("e (k p) n -> e p k n", p=128)
            wu_v = d["w_up"].rearrange("e (k p) n -> e p k n", p=128)
            wd_v = d["w_down"].rearrange("e (k p) n -> e p k n", p=128)
            nst = CAP // 128
            pcnt = 0
            dcnt = 0
            for ex in range(NE):
                i = ex % 2
                S.dma("pool", wg[i][:], wg_v[ex], writes=[b_wg[i]])
                S.dma("pool", wu[i][:], wu_v[ex], writes=[b_wu[i]])
                S.dma("pool", wd[i][:], wd_v[ex], writes=[b_wd[i]])
                S.dma("sp", Xe[i][:], d["xbuf"][ex * CAP:(ex + 1) * CAP, :].rearrange("(s p) n -> p s n", p=128),
                      reads=scat, writes=[b_Xe[i]])
                for st in range(nst):
                    for k in range(8):
                        S.op("pe", lambda e: e.transpose(pX[:, k, :], Xe[i][:, st, k * 128:(k + 1) * 128], ident[:]),
                             [b_Xe[i], b_id], [b_pX])
                    if st % 2 == 0:
                        S.op("act", lambda e: e.activation(out=XT[i][:, :, st * 128:(st + 1) * 128], in_=pX[:], func=AF.Copy),
                             [], [b_pX, b_XT[i]])
                    else:
                        S.op("dve", lambda e: e.tensor_copy(out=XT[i][:, :, st * 128:(st + 1) * 128], in_=pX[:]),
                             [], [b_pX, b_XT[i]])
                for f in range(4):
                    pi = pcnt % 2
                    pcnt += 1
                    for k in range(8):
                        S.op("pe", lambda e: e.matmul(pg[pi][:, 0:CAP], lhsT=wg[i][:, k, f * 128:(f + 1) * 128], rhs=XT[i][:, k, :],
                                                      start=(k == 0), stop=(k == 7)), [b_wg[i], b_XT[i]], [b_pg[pi]])
                    for k in range(8):
                        S.op("pe", lambda e: e.matmul(pu[pi][:, 0:CAP], lhsT=wu[i][:, k, f * 128:(f + 1) * 128], rhs=XT[i][:, k, :],
                                                      start=(k == 0), stop=(k == 7)), [b_wu[i], b_XT[i]], [b_pu[pi]])
                    S.op("act", lambda e: e.activation(out=sg[pi][:], in_=pg[pi][:, 0:CAP], func=AF.Silu), [], [b_pg[pi], b_sg[pi]])
                    S.op("dve", lambda e: e.tensor_tensor(out=hT[i][:, f, :], in0=pu[pi][:, 0:CAP], in1=sg[pi][:], op=ALU.mult),
                         [b_sg[pi]], [b_pu[pi], b_hT[i]])
                for st in range(nst):
                    yi = dcnt % 2
                    dcnt += 1
                    for half in range(2):
                        hs = slice(half * 512, (half + 1) * 512)
                        for f in range(4):
                            S.op("pe", lambda e: e.matmul(pd[half][:], lhsT=hT[i][:, f, st * 128:(st + 1) * 128], rhs=wd[i][:, f, hs],
                                                          start=(f == 0), stop=(f == 3)), [b_hT[i], b_wd[i]], [b_pd[half]])
                        if half == 0:
                            S.op("act", lambda e: e.activation(out=Ye[yi][:, hs], in_=pd[half][:], func=AF.Copy),
                                 [], [b_pd[half], b_Ye[yi]])
                        else:
                            S.op("dve", lambda e: e.tensor_copy(out=Ye[yi][:, hs], in_=pd[half][:]), [], [b_pd[half], b_Ye[yi]])
                    ysc.append(Buf())
                    S.dma("act", d["ybuf"][ex * CAP + st * 128:ex * CAP + (st + 1) * 128, :], Ye[yi][:],
                          reads=[b_Ye[yi]], writes=[ysc[-1]])
            S.barrier()

        with ExitStack() as es:
            def sbn(shape, dt, n=2):
                return [cx.sb(shape, dt, es) for _ in range(n)], [Buf() for _ in range(n)]

            lnp = cx.sb([128, 2, D], F32, es); b_lnp = Buf()
            S.dma("sp", lnp[:], d["lnp"][:, 2 * D:4 * D].partition_broadcast(128).rearrange("p o (a n) -> p (o a) n", a=2), writes=[b_lnp])
            Yg, b_Yg = sbn([128, 2 * D], F32, 3)
            x1r, b_x1r = sbn([128, D], F32, 3)
            acc, b_acc = sbn([128, D], F32)
            z_, b_z_ = sbn([128, D], F32)
            xn = cx.sb([128, D], F32, es); b_xn = Buf()
            tmp = cx.sb([128, D], F32, es); b_tmp = Buf()
            xo, b_xo = sbn([128, D], F32)
            lns = LNScratch(cx, es)

            def G(t):
                i = t % 3
                S.dma("sp", x1r[i][:], d["x1s"][t * 128:(t + 1) * 128, :], reads=x1w, writes=[b_x1r[i]])
                for sidx in range(2):
                    S.dma_fn("pool", lambda e: e.indirect_dma_start(
                        out=Yg[i][:, sidx * D:(sidx + 1) * D], out_offset=None, in_=d["ybuf"][:, :],
                        in_offset=bass.IndirectOffsetOnAxis(ap=destG[:, 2 * t + sidx:2 * t + sidx + 1], axis=0),
                        bounds_check=bc_reg, oob_is_err=False), [b_rt] + ysc, [b_Yg[i]])

            lns2 = [lns, LNScratch(cx, es)]
            xn2 = [xn, cx.sb([128, D], F32, es)]
            b_xn2 = [b_xn, Buf()]
            tmp2 = [tmp, cx.sb([128, D], F32, es)]
            b_tmp2 = [b_tmp, Buf()]

            def Cmb(t):
                i = t % 3
                j = t % 2
                S.op("dve", lambda e: e.tensor_scalar_mul(out=acc[j][:], in0=Yg[i][:, 0:D], scalar1=gate2[:, t, 0:1]),
                     [b_Yg[i], b_rt], [b_acc[j]])
                yield
                S.op("dve", lambda e: e.scalar_tensor_tensor(out=acc[j][:], in0=Yg[i][:, D:2 * D], scalar=gate2[:, t, 1:2],
                                                             in1=acc[j][:], op0=ALU.mult, op1=ALU.add),
                     [b_Yg[i], b_rt], [b_acc[j]])
                yield
                S.op("pool", lambda e: e.tensor_tensor(out=acc[j][:], in0=acc[j][:], in1=g2_ap, op=ALU.mult), [b_ada], [b_acc[j]])
                yield
                S.op("dve", lambda e: e.scalar_tensor_tensor(out=z_[j][:], in0=x1r[i][:], scalar=ALPHA, in1=acc[j][:],
                                                             op0=ALU.mult, op1=ALU.add), [b_x1r[i], b_acc[j]], [b_z_[j]])
                yield

            def Fin(t):
                j = t % 2
                yield from g_ln_mod(cx, z_[j][:], b_z_[j], lns2[j], xn2[j], b_xn2[j], lnp[:, 0, :], lnp[:, 1, :], b_lnp,
                                    xo[j][:], b_xo[j], tmp2[j], b_tmp2[j])
                outs.append(Buf())
                S.dma("sp", xout_d[t * 128:(t + 1) * 128, :], xo[j][:], reads=[b_xo[j]], writes=[outs[-1]])
                yield

            G(0)
            G(1)
            for kk in range(NT + 1):
                if kk + 2 < NT:
                    G(kk + 2)
                round_robin([Cmb(kk) if kk < NT else None, Fin(kk - 1) if 0 <= kk - 1 < NT else None])
            S.barrier()
        S.barrier()
    return outs


def pack_C_inputs(inp, l, j, x_shard, yT_shard, ada_row, hc):
    lnp = np.concatenate([inp["ln_g"][l, 0], inp["ln_b"][l, 0], inp["ln_g"][l, 1], inp["ln_b"][l, 1]])[None, :]
    return {"x": x_shard, "yT": yT_shard, "ada": ada_row, "w_out": np.ascontiguousarray(inp["w_out"][l]),
            "lnp": np.ascontiguousarray(lnp), "w_router": inp["w_router"], "b_router": inp["b_router"][None, :],
            "eoff": hc["eoff"], "w_gate": np.ascontiguousarray(inp["w_gate"][l]), "w_up": np.ascontiguousarray(inp["w_up"][l]),
            "w_down": np.ascontiguousarray(inp["w_down"][l]), "ident_bf": hc["ident_bf"], "ident_f": hc["ident_f"],
            "tris_bf": hc["tris_bf"], "ones_bf": hc["ones_bf"]}


def _run(nc, in_maps):
    res = run_bass_kernel_spmd(nc, in_maps, core_ids=list(range(NCORES)))
    return res.results


def kernel_unfused(**inputs):
    inp = {k: np.asarray(v) for k, v in inputs.items()}
    hc = host_consts()
    x = np.ascontiguousarray(inp["x"][0], dtype=np.float32)
    cT = np.ascontiguousarray(inp["c"][0].reshape(8, 128).T)
    for l in range(DEPTH):
        ra = _run(build_A(), [{"x": np.ascontiguousarray(x[j * TPC:(j + 1) * TPC]), "cT": cT,
                               "w_ada": np.ascontiguousarray(inp["w_ada"][l]),
                               "b_ada": np.ascontiguousarray(inp["b_ada"][l][None, :]),
                               "ident_bf": hc["ident_bf"]} for j in range(NCORES)])
        uT = np.ascontiguousarray(np.concatenate([r["uT"] for r in ra], axis=1))
        ada = ra[0]["ada"]
        rb = _run(build_B(), [pack_B_inputs(inp, l, h, uT, hc) for h in range(NCORES)])
        yT = np.concatenate([r["yT"][0:128] for r in rb] + [r["yT"][128:256] for r in rb], axis=0)
        rc = _run(build_C(), [pack_C_inputs(inp, l, j, np.ascontiguousarray(x[j * TPC:(j + 1) * TPC]),
                                            np.ascontiguousarray(yT[:, j * TPC:(j + 1) * TPC]), ada, hc)
                              for j in range(NCORES)])
        x = np.concatenate([r["xout"] for r in rc], axis=0)
    return x[None].astype(np.float32)


def kernel(**inputs):
    return kernel_unfused(**inputs)
```
